# Optimizing a Trainium2 kernel written in Bass

```python
import jax, jax.numpy as jnp
from jax import lax
import numpy as np

D_MODEL = 2048
BATCH = 4
SEQ = 4096
DEPTH = 1

SGU_CHUNK = 128
SGU_GROUPS = 8
SGU_WIDTH = D_MODEL // 2
MOBA_HEADS = 16
MOBA_HEAD_DIM = D_MODEL // MOBA_HEADS
MOBA_WIDTH = MOBA_HEADS * MOBA_HEAD_DIM
MOBA_BLOCK = 256
MOBA_TOPK = 3
MOBA_Q_CHUNK = 128
ROPE_THETA = 10000.0
MOE_GROUPS = 4
MOE_EXPERTS_PER_GROUP = 8
MOE_EXPERTS = MOE_GROUPS * MOE_EXPERTS_PER_GROUP
MOE_TOPK = 2
MOE_D_FF = D_MODEL // 4
MOE_ROW_BLOCK = 128
EPS = 1e-6
NEG_INF = -1e30
IN_COLS = 2 * SGU_WIDTH + 3 * MOBA_WIDTH + 2 * D_MODEL

kernel_name = 'hybrid_sgu_moba_hiermoe_block'


def rms_norm(x, g):
    xf = x.astype(jnp.float32)
    y = xf * lax.rsqrt(jnp.mean(xf * xf, axis=-1, keepdims=True) + EPS)
    return (y * g.astype(jnp.float32)).astype(x.dtype)


def layer_norm(x, g, b):
    xf = x.astype(jnp.float32)
    mu = jnp.mean(xf, axis=-1, keepdims=True)
    var = jnp.mean(jnp.square(xf - mu), axis=-1, keepdims=True)
    y = (xf - mu) * lax.rsqrt(var + EPS)
    return (y * g.astype(jnp.float32) + b.astype(jnp.float32)).astype(x.dtype)


def modulate(n, shift, scale):
    return n * (1.0 + scale[:, None, :]) + shift[:, None, :]


def rotary_tables(positions, dim):
    inv_freq = ROPE_THETA ** (-jnp.arange(0, dim, 2, dtype=jnp.float32) / dim)
    ang = positions.astype(jnp.float32)[..., None] * inv_freq
    return jnp.cos(ang), jnp.sin(ang)


def apply_rotary(x, cos, sin):
    x1, x2 = jnp.split(x, 2, axis=-1)
    c = cos[:, :, None, :].astype(x.dtype)
    s = sin[:, :, None, :].astype(x.dtype)
    return jnp.concatenate([x1 * c - x2 * s, x2 * c + x1 * s], axis=-1)


def spatial_gating(z, ln_g, ln_b, w_s, b_s):
    B, S, _ = z.shape
    u, v = jnp.split(z, 2, axis=-1)
    v = layer_norm(v, ln_g, ln_b)
    nc = S // SGU_CHUNK
    cg = SGU_WIDTH // SGU_GROUPS
    v = v.reshape(B, nc, SGU_CHUNK, SGU_GROUPS, cg)
    causal = jnp.tril(jnp.ones((SGU_CHUNK, SGU_CHUNK), dtype=bool))
    w = jnp.where(causal[None], w_s, 0)
    sv = jnp.einsum('gij,bcjgd->bcigd', w, v) + b_s.T[None, None, :, :, None]
    return u * sv.reshape(B, S, SGU_WIDTH)


def moba_attention(q, k, v):
    B, S, H, dh = q.shape
    s_pad = -(-S // MOBA_BLOCK) * MOBA_BLOCK
    pad = ((0, 0), (0, s_pad - S), (0, 0), (0, 0))
    q, k, v = jnp.pad(q, pad), jnp.pad(k, pad), jnp.pad(v, pad)
    nb = s_pad // MOBA_BLOCK
    n_top = min(MOBA_TOPK, nb)
    nq = s_pad // MOBA_Q_CHUNK
    scale = dh ** -0.5

    def heads_first(t):
        return t.transpose(0, 2, 1, 3).reshape(B * H, s_pad, dh)

    qh, kh, vh = heads_first(q), heads_first(k), heads_first(v)
    kb = kh.reshape(B * H, nb, MOBA_BLOCK, dh)
    vb = vh.reshape(B * H, nb, MOBA_BLOCK, dh)
    k_mean = jnp.mean(kb.astype(jnp.float32), axis=2)
    gate = jnp.einsum('zsd,znd->zsn', qh.astype(jnp.float32), k_mean)
    q_blk = jnp.arange(s_pad) // MOBA_BLOCK
    past = jnp.arange(nb)[None, :] < q_blk[:, None]
    gate = jnp.where(past[None], gate, NEG_INF)
    _, top_idx = lax.top_k(gate, n_top)

    def one_head(args):
        q_z, idx_z, kb_z, vb_z = args

        def one_chunk(cargs):
            q_c, idx_c, ci = cargs
            start = ci * MOBA_Q_CHUNK
            q_pos = start + jnp.arange(MOBA_Q_CHUNK)
            blk = start // MOBA_BLOCK
            k_own = lax.dynamic_index_in_dim(kb_z, blk, 0, keepdims=False)
            v_own = lax.dynamic_index_in_dim(vb_z, blk, 0, keepdims=False)
            k_sel = kb_z[idx_c]
            v_sel = vb_z[idx_c]
            s_own = jnp.einsum('qd,kd->qk', q_c, k_own).astype(jnp.float32) * scale
            k_pos = blk * MOBA_BLOCK + jnp.arange(MOBA_BLOCK)
            s_own = jnp.where(k_pos[None, :] <= q_pos[:, None], s_own, NEG_INF)
            s_sel = jnp.einsum('qd,qnkd->qnk', q_c, k_sel).astype(jnp.float32) * scale
            s_sel = jnp.where((idx_c < blk)[:, :, None], s_sel, NEG_INF)
            s_all = jnp.concatenate([s_own, s_sel.reshape(MOBA_Q_CHUNK, n_top * MOBA_BLOCK)], axis=-1)
            p = jax.nn.softmax(s_all, axis=-1).astype(q_c.dtype)
            p_own = p[:, :MOBA_BLOCK]
            p_sel = p[:, MOBA_BLOCK:].reshape(MOBA_Q_CHUNK, n_top, MOBA_BLOCK)
            return jnp.einsum('qk,kd->qd', p_own, v_own) + jnp.einsum('qnk,qnkd->qd', p_sel, v_sel)

        return lax.map(one_chunk, (q_z, idx_z, jnp.arange(nq)))

    out = lax.map(one_head, (qh.reshape(B * H, nq, MOBA_Q_CHUNK, dh),
                             top_idx.reshape(B * H, nq, MOBA_Q_CHUNK, n_top), kb, vb))
    out = out.reshape(B, H, s_pad, dh)[:, :, :S].transpose(0, 2, 1, 3)
    return out.reshape(B, S, H * dh)


def hierarchical_moe(n, w_rg, b_rg, w_re, b_re, w_gate, w_up, w_down):
    B, S, D = n.shape
    T = B * S
    xt = n.reshape(T, D)
    tok = jnp.arange(T)
    g_logits = (xt @ w_rg + b_rg).astype(jnp.float32)
    g_prob = jax.nn.softmax(g_logits, axis=-1)
    g_sel = jnp.argmax(g_logits, axis=-1).astype(jnp.int32)
    p_group = g_prob[tok, g_sel][:, None]
    e_logits = ((xt @ w_re).reshape(T, MOE_GROUPS, MOE_EXPERTS_PER_GROUP) + b_re).astype(jnp.float32)
    e_logits = e_logits[tok, g_sel]
    top_val, top_loc = lax.top_k(e_logits, MOE_TOPK)
    w_tok = jax.nn.softmax(top_val, axis=-1) * p_group
    expert_id = (g_sel[:, None] * MOE_EXPERTS_PER_GROUP + top_loc).reshape(-1)
    token_id = jnp.repeat(tok, MOE_TOPK)
    weight = w_tok.reshape(-1)
    A = T * MOE_TOPK
    cap = -(-(A + MOE_EXPERTS * MOE_ROW_BLOCK) // MOE_ROW_BLOCK) * MOE_ROW_BLOCK
    n_blocks = cap // MOE_ROW_BLOCK
    order = jnp.argsort(expert_id)
    e_sorted = expert_id[order]
    counts = jnp.bincount(expert_id, length=MOE_EXPERTS)
    padded = (counts + MOE_ROW_BLOCK - 1) // MOE_ROW_BLOCK * MOE_ROW_BLOCK
    start = jnp.cumsum(counts) - counts
    end_padded = jnp.cumsum(padded)
    start_padded = end_padded - padded
    dest = start_padded[e_sorted] + (jnp.arange(A) - start[e_sorted])
    row_token = jnp.full((cap,), T, dtype=jnp.int32).at[dest].set(token_id[order].astype(jnp.int32))
    row_weight = jnp.zeros((cap,), weight.dtype).at[dest].set(weight[order])
    block_expert = jnp.minimum(
        jnp.searchsorted(end_padded, jnp.arange(n_blocks) * MOE_ROW_BLOCK, side='right'),
        MOE_EXPERTS - 1)
    x_pad = jnp.concatenate([xt, jnp.zeros((1, D), xt.dtype)], axis=0)
    rows = x_pad[row_token].reshape(n_blocks, MOE_ROW_BLOCK, D)

    def expert_block(args):
        xb, e = args
        hid = jax.nn.silu(xb @ w_gate[e]) * (xb @ w_up[e])
        return hid @ w_down[e]

    y_rows = lax.map(expert_block, (rows, block_expert)).reshape(cap, D)
    y = jnp.zeros((T + 1, D), y_rows.dtype).at[row_token].add(
        y_rows * row_weight[:, None].astype(y_rows.dtype))
    return y[:T].reshape(B, S, D)


def setup_inputs(seed: int = 0) -> dict:
    key = jax.random.key(seed)
    ks = jax.random.split(key, 32)
    f32 = jnp.float32
    L, D = DEPTH, D_MODEL

    def nrm(k, shape, scale):
        return jax.random.normal(k, shape, f32) * scale

    x = nrm(ks[0], (BATCH, SEQ, D), 1.0)
    c = nrm(ks[1], (BATCH, D), 1.0)
    offset = jax.random.randint(ks[2], (BATCH, 1), 0, 1024, dtype=jnp.int32)
    positions = offset + jnp.arange(SEQ, dtype=jnp.int32)[None, :]
    return {
        'x': x,
        'c': c,
        'positions': positions,
        'w_ada': nrm(ks[3], (L, D, 6 * D), 0.5 * D ** -0.5),
        'b_ada': nrm(ks[4], (L, 6 * D), 0.02),
        'norm_mix_g': 1.0 + nrm(ks[5], (L, D), 0.02),
        'w_in': nrm(ks[6], (L, D, IN_COLS), D ** -0.5),
        'sgu_ln_g': 1.0 + nrm(ks[7], (L, SGU_WIDTH), 0.02),
        'sgu_ln_b': nrm(ks[8], (L, SGU_WIDTH), 0.02),
        'sgu_w_s': nrm(ks[9], (L, SGU_GROUPS, SGU_CHUNK, SGU_CHUNK), SGU_CHUNK ** -0.5),
        'sgu_b_s': 1.0 + nrm(ks[10], (L, SGU_GROUPS, SGU_CHUNK), 0.02),
        'w_sgu_out': nrm(ks[11], (L, SGU_WIDTH, D), SGU_WIDTH ** -0.5),
        'w_moba_out': nrm(ks[12], (L, MOBA_WIDTH, D), MOBA_WIDTH ** -0.5),
        'w_out': nrm(ks[13], (L, D, D), D ** -0.5),
        'norm_ffn_g': 1.0 + nrm(ks[14], (L, D), 0.02),
        'w_route_group': nrm(ks[15], (L, D, MOE_GROUPS), D ** -0.5),
        'b_route_group': nrm(ks[16], (L, MOE_GROUPS), 0.01),
        'w_route_expert': nrm(ks[17], (L, D, MOE_EXPERTS), D ** -0.5),
        'b_route_expert': nrm(ks[18], (L, MOE_GROUPS, MOE_EXPERTS_PER_GROUP), 0.01),
        'w_exp_gate': nrm(ks[19], (L, MOE_EXPERTS, D, MOE_D_FF), D ** -0.5),
        'w_exp_up': nrm(ks[20], (L, MOE_EXPERTS, D, MOE_D_FF), D ** -0.5),
        'w_exp_down': nrm(ks[21], (L, MOE_EXPERTS, MOE_D_FF, D), MOE_D_FF ** -0.5),
        'norm_final_g': 1.0 + nrm(ks[22], (D,), 0.02),
    }


def reference(x, c, positions, w_ada, b_ada, norm_mix_g, w_in, sgu_ln_g, sgu_ln_b, sgu_w_s,
              sgu_b_s, w_sgu_out, w_moba_out, w_out, norm_ffn_g, w_route_group, b_route_group,
              w_route_expert, b_route_expert, w_exp_gate, w_exp_up, w_exp_down, norm_final_g):
    B, S, D = x.shape
    cos, sin = rotary_tables(positions, MOBA_HEAD_DIM)
    c_act = jax.nn.silu(c)
    splits = [2 * SGU_WIDTH, 2 * SGU_WIDTH + MOBA_WIDTH, 2 * SGU_WIDTH + 2 * MOBA_WIDTH,
              2 * SGU_WIDTH + 3 * MOBA_WIDTH, 2 * SGU_WIDTH + 3 * MOBA_WIDTH + D_MODEL]
    h = x
    for l in range(DEPTH):
        ada = c_act @ w_ada[l] + b_ada[l]
        sh_m, sc_m, g_m, sh_f, sc_f, g_f = jnp.split(ada, 6, axis=-1)
        n = modulate(rms_norm(h, norm_mix_g[l]), sh_m, sc_m)
        proj = n @ w_in[l]
        z_sgu, q, k, v, gate_a, gate_b = jnp.split(proj, splits, axis=-1)
        y_a = spatial_gating(jax.nn.gelu(z_sgu), sgu_ln_g[l], sgu_ln_b[l],
                             sgu_w_s[l], sgu_b_s[l]) @ w_sgu_out[l]
        q = apply_rotary(q.reshape(B, S, MOBA_HEADS, MOBA_HEAD_DIM), cos, sin)
        k = apply_rotary(k.reshape(B, S, MOBA_HEADS, MOBA_HEAD_DIM), cos, sin)
        v = v.reshape(B, S, MOBA_HEADS, MOBA_HEAD_DIM)
        y_b = moba_attention(q, k, v) @ w_moba_out[l]
        merged = jax.nn.sigmoid(gate_a) * y_a + jax.nn.sigmoid(gate_b) * y_b
        h = h + g_m[:, None, :] * (merged @ w_out[l])
        n2 = modulate(rms_norm(h, norm_ffn_g[l]), sh_f, sc_f)
        h = h + g_f[:, None, :] * hierarchical_moe(
            n2, w_route_group[l], b_route_group[l], w_route_expert[l], b_route_expert[l],
            w_exp_gate[l], w_exp_up[l], w_exp_down[l])
    return rms_norm(h, norm_final_g)
```

```python
import numpy as np
from contextlib import ExitStack
import concourse.bass as bass
import concourse.mybir as mybir
from concourse.bass_utils import run_bass_kernel_spmd
from concourse.alu_op_type import AluOpType as ALU

AF = mybir.ActivationFunctionType
AX = mybir.AxisListType
F32 = mybir.dt.float32
BF16 = mybir.dt.bfloat16
I32 = mybir.dt.int32
U32 = mybir.dt.uint32

D = 2048
SEQ = 4096
NOWN = 2048
NH = 16
DH = 128
EPS = 1e-6
BIG = 30000.0
NEXP = 32
DFF = 512
RB = 256
NBLK = 48
CAPROWS = NBLK * RB
PI = float(np.pi)
RELAX = False

CP_IDENT, CP_PM, CP_TRI01, CP_TRIPEN, CP_LSTRICT, CP_ONES = 0, 128, 256, 384, 512, 640
CP_INVF, CP_IOTAP, CP_SIGN = 768, 769, 770
CP_PASTPEN = 771
CP_PAST01 = CP_PASTPEN + 128
CP_W = CP_PAST01 + 128
C2_THR = 0
C2_BLKI = 1024
C2_W = 1024 + 2048


class Res:
    __slots__ = ("name", "w", "rs", "rd")

    def __init__(self, name):
        self.name = name
        self.w = None
        self.rs = {}
        self.rd = []


class Sched:
    ENGS = ("pe", "act", "dve", "pool", "sp")

    def __init__(self, nc, es, ndsem=12):
        self.nc = nc
        self.eng = {"pe": nc.tensor, "act": nc.scalar, "dve": nc.vector, "pool": nc.gpsimd, "sp": nc.sync}
        self.sem = {e: es.enter_context(nc.semaphore("sem_" + e)) for e in self.ENGS}
        self.cnt = {e: 0 for e in self.ENGS}
        self.seen = {e: {f: 0 for f in self.ENGS} for e in self.ENGS}
        self.dq = {}
        for q, nd in (("sp", 16), ("pool", 14)):
            sems = [es.enter_context(nc.semaphore(f"dsem_{q}{i}")) for i in range(nd)]
            self.dq[q] = {"sems": sems, "val": [0] * nd, "nxt": 0}
        self.seen_d = {e: {} for e in self.ENGS}

    def _wait(self, e, t):
        if t is None:
            return
        if t[0] == "dma":
            _, q, k, v = t
            key = (q, k)
            if self.seen_d[e].get(key, 0) >= v:
                return
            self.eng[e].wait_ge(self.dq[q]["sems"][k], v)
            self.seen_d[e][key] = v
        else:
            src, v = t
            if self.seen[e][src] >= v:
                return
            self.eng[e].wait_ge(self.sem[src], v)
            self.seen[e][src] = v

    def _deps(self, e, reads, writes):
        for r in reads:
            self._wait(e, r.w)
        for w in writes:
            if w.w is not None and not (w.w[0] == e and RELAX):
                self._wait(e, w.w)
            for src, v in w.rs.items():
                if src != e or not RELAX:
                    self._wait(e, (src, v))
            for t in w.rd:
                self._wait(e, t)

    def _mark(self, t, reads, writes):
        for r in reads:
            if t[0] == "dma":
                r.rd.append(t)
            else:
                if r.rs.get(t[0], 0) < t[1]:
                    r.rs[t[0]] = t[1]
        for w in writes:
            w.w = t
            w.rs = {}
            w.rd = []

    def op(self, e, fn, reads=(), writes=()):
        self._deps(e, reads, writes)
        inst = fn(self.eng[e])
        self.cnt[e] += 1
        inst.then_inc(self.sem[e], 1)
        t = (e, self.cnt[e])
        self.seen[e][e] = max(self.seen[e][e], 0)
        self._mark(t, reads, writes)
        return t

    def dma(self, q, out, in_, reads=(), writes=(), after=(), **kw):
        self._deps(q, reads, writes)
        for t_ in after:
            self._wait(q, t_)
        d = self.dq[q]
        k = d["nxt"]
        d["nxt"] = (k + 1) % len(d["sems"])
        if d["val"][k]:
            self._wait(q, ("dma", q, k, d["val"][k]))
        inst = self.eng[q].dma_start(out=out, in_=in_, **kw)
        d["val"][k] += 16
        inst.then_inc(d["sems"][k], 16)
        t = ("dma", q, k, d["val"][k])
        self._mark(t, reads, writes)
        return t

    def idma(self, fn, reads=(), writes=()):
        q = "pool"
        self._deps(q, reads, writes)
        d = self.dq[q]
        k = d["nxt"]
        d["nxt"] = (k + 1) % len(d["sems"])
        if d["val"][k]:
            self._wait(q, ("dma", q, k, d["val"][k]))
        inst = fn(self.eng[q])
        d["val"][k] += 16
        inst.then_inc(d["sems"][k], 16)
        t = ("dma", q, k, d["val"][k])
        self._mark(t, reads, writes)
        return t

    def barrier(self, engines=None):
        for e in (engines or self.ENGS):
            for f in self.ENGS:
                if f != e and self.cnt[f]:
                    self._wait(e, (f, self.cnt[f]))
            for q, d in self.dq.items():
                for k, v in enumerate(d["val"]):
                    if v:
                        self._wait(e, ("dma", q, k, v))


def dram_bcast(ap, n):
    return bass.AP(ap.tensor, ap.offset, [[0, 128], [1, n]])


def build(dbg=(), stop_after=None):
    nc = bass.Bass("TRN2", target_bir_lowering=False)
    dbg = set(dbg)

    def din(name, shape, dt=F32):
        return nc.dram_tensor(name, list(shape), dt, kind="ExternalInput").ap()

    def dscr(name, shape, dt):
        if name in dbg:
            return nc.dram_tensor(name, list(shape), dt, kind="ExternalOutput").ap()
        return nc.dram_tensor(name, list(shape), dt).ap()

    x = din("x", [SEQ, D])
    cvec = din("cvec", [128, 16])
    pos = din("pos", [1, SEQ], I32)
    w_ada = din("w_ada", [D, 6 * D])
    b_ada = din("b_ada", [1, 6 * D])
    gmix = din("gmix", [128, 16])
    w_in = din("w_in", [D, 6 * D])
    ln_g = din("sgu_ln_g", [1, 1024])
    ln_b = din("sgu_ln_b", [1, 1024])
    wsT_d = din("wsT", [128, 8 * 128])
    bs_d = din("sgu_b_s", [1, 8 * 128])
    w_sgu_out = din("w_sgu_out", [1024, D])
    w_moba_out = din("w_moba_out", [D, D])
    w_out = din("w_out", [D, D])
    nffn_g = din("norm_ffn_g", [1, D])
    nfin_g = din("norm_final_g", [1, D])
    wr_d = din("wr", [128, 16 * 36])
    br_d = din("br", [1, 36])
    weg = din("weg", [NEXP * 128 * 4, 2048])
    weu = din("weu", [NEXP * 128 * 4, 2048])
    wed = din("wed", [NEXP * 128 * 4, 2048])
    cpack = din("cpack", [128, CP_W])
    eall_d = din("eall", [16, 16 * 128])
    cpack2 = din("cpack2", [128, C2_W])

    out = nc.dram_tensor("out", [NOWN, D], F32, kind="ExternalOutput").ap()

    UT = dscr("UT", [16, 128, 8 * 128], BF16)
    VS = dscr("VS", [NOWN, 1024], BF16)
    QT = dscr("QT", [NH, 128, NOWN], BF16)
    KT = dscr("KT", [NH, 128, SEQ], BF16)
    VA = dscr("VA", [NH, 128, 32 * 128], BF16)
    GA = dscr("GA", [16, 128, NOWN], BF16)
    GB = dscr("GB", [16, 128, NOWN], BF16)
    MG = dscr("MG", [16, 128, NOWN], BF16)
    H1 = dscr("H1", [NOWN, D], F32)
    N2 = dscr("N2", [NOWN, D], BF16)
    XS = dscr("XS", [CAPROWS, D], BF16)
    YS = dscr("YS", [CAPROWS, D], BF16)
    DBG = dscr("DBG", [128, 4096], F32)
    WGB = dscr("WGB", [NEXP * 128 * 4, 2048], BF16)
    WUB = dscr("WUB", [NEXP * 128 * 4, 2048], BF16)
    WDB = dscr("WDB", [NEXP * 128 * 4, 2048], BF16)

    def dscr_out(name, shape, dt):
        return nc.dram_tensor(name, list(shape), dt, kind="ExternalOutput").ap()

    with ExitStack() as es:
        S = Sched(nc, es)

        uniq = [0]

        def sb(name, shape, dt, stack=es):
            uniq[0] += 1
            return stack.enter_context(nc.sbuf_tensor(f"{name}_{uniq[0]}", list(shape), dt))

        PB = [es.enter_context(nc.psum_tensor(f"pb{i}", [128, 512], F32)) for i in range(6)]
        PBr = [Res(f"pb{i}") for i in range(6)]
        PT = [es.enter_context(nc.psum_tensor(f"pt{i}", [128, 1024], BF16)) for i in range(2)]
        PTr = [Res(f"pt{i}") for i in range(2)]

        cp = sb("cp", [128, CP_W], F32)
        cpr = Res("cp")
        S.dma("sp", cp[:], cpack[:, :], writes=[cpr])
        identb = sb("identb", [128, 128], BF16)
        pmb = sb("pmb", [128, 128], BF16)
        tripenb = sb("tripenb", [128, 128], BF16)
        lstrb = sb("lstrb", [128, 128], BF16)
        onesb = sb("onesb", [128, 128], BF16)
        epsc = sb("epsc", [128, 1], F32)
        constr = Res("constb")
        for dst, off in ((identb, CP_IDENT), (pmb, CP_PM), (tripenb, CP_TRIPEN), (lstrb, CP_LSTRICT), (onesb, CP_ONES)):
            S.op("dve", lambda v, dst=dst, off=off: v.tensor_copy(out=dst[:], in_=cp[:, off:off + 128]),
                 reads=[cpr], writes=[constr])
        S.op("dve", lambda v: v.memset(epsc[:], EPS), writes=[constr])
        eallb = sb("eallb", [16, 2048], BF16)
        with ExitStack() as s0:
            eall_f = sb("eall_f", [16, 2048], F32, s0)
            er = Res("eall")
            S.dma("sp", eall_f[:], eall_d[:, :], writes=[er])
            S.op("dve", lambda v: v.tensor_copy(out=eallb[:], in_=eall_f[:]), reads=[er], writes=[constr])
            S.barrier()

        adaT = sb("adaT", [128, 96], F32)
        adaTr = Res("adaT")
        g1 = sb("g1", [128, 16], F32)
        g1r = Res("g1")
        gm_bc = sb("gm_bc", [128, D], F32)
        G2_bc = sb("G2_bc", [128, D], F32)
        shf_bc = sb("shf_bc", [128, D], F32)
        gf_bc = sb("gf_bc", [128, D], F32)
        bcr = Res("bc")

        with ExitStack() as sa:
            c_sb = sb("c_sb", [128, 16], F32, sa)
            c_act = sb("c_act", [128, 16], BF16, sa)
            ada_row = sb("ada_row", [1, 6 * D], F32, sa)
            bada = [sb(f"bada{i}", [1, 512], F32, sa) for i in range(2)]
            badar = [Res(f"bada{i}") for i in range(2)]
            wblk = [sb(f"wa{i}", [128, 16, 512], BF16, sa) for i in range(3)]
            wblr = [Res(f"wa{i}") for i in range(3)]
            one1 = sb("one1", [1, 128], F32, sa)
            cr, ar, br_ = Res("c"), Res("adarow"), Res("bada")
            S.dma("sp", c_sb[:], cvec[:, :], writes=[cr])
            S.op("act", lambda a: a.activation(out=c_act[:], in_=c_sb[:], func=AF.Silu), reads=[cr], writes=[cr])
            S.op("dve", lambda v: v.memset(one1[:], 1.0), writes=[ar])
            wav = w_ada.rearrange("(kc p) n -> p kc n", p=128)
            for nt in range(24):
                k = nt % 3
                S.dma("pool", wblk[k][:], wav[:, :, nt * 512:(nt + 1) * 512], writes=[wblr[k]])
                pbi = nt % 2
                S.dma("sp", bada[pbi][:], b_ada[0:1, nt * 512:(nt + 1) * 512], writes=[badar[pbi]])

                def mm(pe, k=k, pbi=pbi):
                    for kc in range(16):
                        i = pe.matmul(PB[pbi][0:1, :], lhsT=c_act[:, kc:kc + 1], rhs=wblk[k][:, kc, :],
                                      start=(kc == 0), stop=(kc == 15))
                    return i
                S.op("pe", mm, reads=[cr, wblr[k]], writes=[PBr[pbi]])
                S.op("dve", lambda v, nt=nt, pbi=pbi: v.tensor_tensor(
                    out=ada_row[0:1, nt * 512:(nt + 1) * 512], in0=PB[pbi][0:1, :],
                    in1=bada[pbi][0:1, :], op=ALU.add), reads=[PBr[pbi], badar[pbi]], writes=[ar])
            def mmT(pe):
                for j in range(96):
                    i = pe.matmul(PB[2][:, j:j + 1], lhsT=ada_row[0:1, j * 128:(j + 1) * 128], rhs=one1[0:1, 0:1],
                                  start=True, stop=True)
                return i
            S.op("pe", mmT, reads=[ar], writes=[PBr[2]])
            S.op("dve", lambda v: v.tensor_copy(out=adaT[:], in_=PB[2][:, 0:96]), reads=[PBr[2]], writes=[adaTr])
            gmx = sb("gmx", [128, 16], F32, sa)
            gr_ = Res("gmx")
            S.dma("sp", gmx[:], gmix[:, :], writes=[gr_])
            S.op("dve", lambda v: v.scalar_tensor_tensor(out=g1[:], in0=adaT[:, 16:32], scalar=1.0, in1=gmx[:],
                                                         op0=ALU.add, op1=ALU.mult), reads=[adaTr, gr_], writes=[g1r])
            S.dma("sp", G2_bc[:], dram_bcast(nffn_g[0:1, :], D), writes=[bcr])
            for which, dst in ((2, gm_bc), (3, shf_bc), (4, None), (5, gf_bc)):
                for q4 in range(4):
                    c0 = which * D + q4 * 512
                    pbi = 3 + (q4 % 2)
                    S.op("pe", lambda pe, c0=c0, pbi=pbi: pe.matmul(PB[pbi][:, :], lhsT=one1[0:1, :],
                                                                    rhs=ada_row[0:1, c0:c0 + 512], start=True, stop=True),
                         reads=[ar], writes=[PBr[pbi]])
                    if dst is not None:
                        S.op("dve", lambda v, dst=dst, q4=q4, pbi=pbi: v.tensor_copy(
                            out=dst[:, q4 * 512:(q4 + 1) * 512], in_=PB[pbi][:, :]), reads=[PBr[pbi]], writes=[bcr])
                    else:
                        S.op("dve", lambda v, q4=q4, pbi=pbi: v.scalar_tensor_tensor(
                            out=G2_bc[:, q4 * 512:(q4 + 1) * 512], in0=PB[pbi][:, :], scalar=1.0,
                            in1=G2_bc[:, q4 * 512:(q4 + 1) * 512], op0=ALU.add, op1=ALU.mult),
                            reads=[PBr[pbi], bcr], writes=[bcr])
            S.barrier()
        if "adaT" in dbg:
            S.dma("sp", DBG[:, 0:96], adaT[:], reads=[adaTr])
            S.dma("sp", DBG[:, 128:128 + 2048], G2_bc[:], reads=[bcr])
        if stop_after == "A":
            S.barrier()
            return nc

        kmT = sb("kmT", [128, 16, 16], F32)
        kmr = Res("kmT")
        d1f = sb("d1f", [128, 16], F32)
        d2f = sb("d2f", [128, 16], F32)
        w1_all = sb("w1_all", [128, 16], F32)
        w2_all = sb("w2_all", [128, 16], F32)
        idx4 = sb("idx4", [128, 256], I32)
        wallr, idxr, XSr, YSr = Res("wall"), Res("idx4"), Res("XS"), Res("YS")
        sNT = es.enter_context(ExitStack())
        NT = sb("NT", [128, 16, NOWN], BF16, sNT)
        NTr = Res("NT")
        NTr2 = Res("NT2")
        w_inv = w_in.rearrange("(kc p) n -> p kc n", p=128)

        def norm_stage(tok0):
            with ExitStack() as sb_:
                NB_ = 3
                xt = [sb(f"xt{i}", [128, D], F32, sb_) for i in range(NB_)]
                xs = [sb(f"xs{i}", [128, D], BF16, sb_) for i in range(2)]
                junk = sb("junk", [128, D], BF16, sb_)
                ssq = [sb(f"ssq{i}", [128, 1], F32, sb_) for i in range(NB_)]
                xr = [Res(f"xt{i}") for i in range(NB_)]
                xsr = [Res(f"xs{i}") for i in range(2)]
                sr = [Res(f"ssq{i}") for i in range(NB_)]
                jr = Res("junk")
                ntmp = [sb(f"ntmp{i}", [128, 8, 128], F32, sb_) for i in range(2)]
                ntr = [Res(f"ntmp{i}") for i in range(2)]

                def p1(tt):
                    k = tt % NB_
                    S.dma("sp", xt[k][:], x[tok0 + tt * 128: tok0 + (tt + 1) * 128, :], writes=[xr[k]])
                    S.op("act", lambda a, k=k: a.activation(out=junk[:], in_=xt[k][:], func=AF.Square, accum_out=ssq[k][:]),
                         reads=[xr[k]], writes=[jr, sr[k]])

                def p2(tt):
                    k = tt % NB_
                    k2 = tt % 2
                    S.op("act", lambda a, k=k: a.activation(out=ssq[k][:], in_=ssq[k][:], func=AF.Sqrt, scale=1.0 / D, bias=epsc[:, 0:1]),
                         reads=[sr[k], constr], writes=[sr[k]])
                    S.op("dve", lambda v, k=k: v.reciprocal(out=ssq[k][:], in_=ssq[k][:]), reads=[sr[k]], writes=[sr[k]])
                    S.op("act", lambda a, k=k, k2=k2: a.activation(out=xs[k2][:], in_=xt[k][:], func=AF.Copy, scale=ssq[k][:, 0:1]),
                         reads=[xr[k], sr[k]], writes=[xsr[k2]])

                def p3(tt):
                    k2 = tt % 2
                    for half in range(2):
                        def tr(pe, k2=k2, half=half):
                            for jj in range(8):
                                kc = half * 8 + jj
                                i = pe.transpose(out=PT[half][:, jj * 128:(jj + 1) * 128], in_=xs[k2][:, kc * 128:(kc + 1) * 128],
                                                 identity=identb[:])
                            return i
                        S.op("pe", tr, reads=[xsr[k2], constr], writes=[PTr[half]])
                        tb = (2 * tt + half) % 2
                        S.op("dve", lambda v, half=half, tb=tb: v.tensor_tensor(
                            out=ntmp[tb][:], in0=PT[half][:, :].rearrange("p (a b) -> p a b", b=128),
                            in1=g1[:, half * 8:(half + 1) * 8].unsqueeze(2).broadcast_to([128, 8, 128]), op=ALU.mult),
                            reads=[PTr[half], g1r], writes=[ntr[tb]])
                        S.op("pool", lambda g_, half=half, tb=tb, tt=tt: g_.tensor_tensor(
                            out=NT[:, half * 8:(half + 1) * 8, tt * 128:(tt + 1) * 128], in0=ntmp[tb][:],
                            in1=adaT[:, half * 8:(half + 1) * 8].unsqueeze(2).broadcast_to([128, 8, 128]), op=ALU.add),
                            reads=[ntr[tb], adaTr], writes=[NTr])

                p1(0)
                p1(1)
                p2(0)
                for tt in range(16):
                    if tt + 2 < 16:
                        p1(tt + 2)
                    if tt + 1 < 16:
                        p2(tt + 1)
                    p3(tt)
                S.barrier()

        def proj_stage(tok0, blocks):
            ntok = NOWN
            with ExitStack() as sc:
                wb = [sb(f"wb{i}", [128, 16, 512], BF16, sc) for i in range(3)]
                wbr = [Res(f"wb{i}") for i in range(3)]
                cosT = sb("cosT", [128, NOWN], F32, sc)
                sinT = sb("sinT", [128, NOWN], F32, sc)
                tabr = Res("tab")
                import os
                KN = os.environ.get("KNOB", "")
                need_tab = any(kd in ("q", "k") for kd, _ in blocks) or "forcetab" in KN
                with ExitStack() as st:
                    pi_ = sb("pos_i", [128, 512], I32, st)
                    ang = sb("ang", [128, 512], F32, st)
                    kf = sb("kf", [128, 512], F32, st)
                    ki = sb("ki", [128, 512], I32, st)
                    msk = sb("msk", [128, 512], F32, st)
                    r1, r2, r3, r4, r5 = Res("pos_i"), Res("ang"), Res("kf"), Res("ki"), Res("msk")
                    for tg in (range(4) if need_tab else ()):
                        t0 = tok0 + tg * 512
                        S.dma("sp", pi_[:], dram_bcast(pos[0:1, t0:t0 + 512], 512), writes=[r1])
                        for which, dst in ((0, sinT), (1, cosT)):
                            S.op("dve", lambda v: v.tensor_copy(out=ang[:], in_=pi_[:]), reads=[r1], writes=[r2])
                            S.op("dve", lambda v, which=which: v.tensor_scalar(
                                out=ang[:], in0=ang[:], scalar1=cp[:, CP_INVF:CP_INVF + 1], scalar2=which * PI / 2,
                                op0=ALU.mult, op1=ALU.add), reads=[r2, cpr], writes=[r2])
                            S.op("dve", lambda v: v.tensor_scalar(out=kf[:], in0=ang[:], scalar1=1.0 / (2 * PI), scalar2=None,
                                                                  op0=ALU.mult), reads=[r2], writes=[r3])
                            S.op("dve", lambda v: v.tensor_copy(out=ki[:], in_=kf[:]), reads=[r3], writes=[r4])
                            S.op("dve", lambda v: v.tensor_copy(out=kf[:], in_=ki[:]), reads=[r4], writes=[r3])
                            S.op("dve", lambda v: v.scalar_tensor_tensor(out=ang[:], in0=kf[:], scalar=-2 * PI, in1=ang[:],
                                                                         op0=ALU.mult, op1=ALU.add), reads=[r3, r2], writes=[r2])
                            S.op("dve", lambda v: v.tensor_scalar(out=msk[:], in0=ang[:], scalar1=PI, scalar2=-2 * PI,
                                                                  op0=ALU.is_gt, op1=ALU.mult), reads=[r2], writes=[r5])
                            S.op("dve", lambda v: v.tensor_tensor(out=ang[:], in0=ang[:], in1=msk[:], op=ALU.add),
                                 reads=[r2, r5], writes=[r2])
                            S.op("dve", lambda v: v.tensor_scalar(out=msk[:], in0=ang[:], scalar1=-PI, scalar2=2 * PI,
                                                                  op0=ALU.is_lt, op1=ALU.mult), reads=[r2], writes=[r5])
                            S.op("dve", lambda v: v.tensor_tensor(out=ang[:], in0=ang[:], in1=msk[:], op=ALU.add),
                                 reads=[r2, r5], writes=[r2])
                            S.op("dve", lambda v: v.tensor_scalar(out=ang[:], in0=ang[:], scalar1=-3.1415925, scalar2=3.1415925,
                                                                  op0=ALU.max, op1=ALU.min), reads=[r2], writes=[r2])
                            if which == 0:
                                S.op("act", lambda a, dst=dst, tg=tg: a.activation(out=dst[:, tg * 512:(tg + 1) * 512], in_=ang[:],
                                                                                 func=AF.Sin, scale=cp[:, CP_SIGN:CP_SIGN + 1]),
                                     reads=[r2, cpr], writes=[tabr])
                            else:
                                S.op("act", lambda a, dst=dst, tg=tg: a.activation(out=dst[:, tg * 512:(tg + 1) * 512], in_=ang[:],
                                                                                 func=AF.Sin), reads=[r2], writes=[tabr])
                    S.barrier()
                ev = [sb(f"ev{i}", [128, 512], BF16, sc) for i in range(3)]
                evr = [Res(f"ev{i}") for i in range(3)]
                qb = [sb(f"qb{i}", [128, 512], BF16, sc) for i in range(2)]
                qbr = [Res(f"qb{i}") for i in range(2)]
                t1 = [sb(f"t1{i}", [128, 512], F32, sc) for i in range(2)]
                t1r = [Res(f"t1{i}") for i in range(2)]
                t2 = [sb(f"t2{i}", [128, 512], F32, sc) for i in range(2)]
                t2r = [Res(f"t2{i}") for i in range(2)]
                evi = [0]
                rpi = [0]
                pbi = [0]
                for bi, (kind, col0) in enumerate(blocks):
                    k = bi % 3
                    S.dma("pool", wb[k][:], w_inv[:, :, col0:col0 + 512], writes=[wbr[k]])
                    if kind in ("vs", "va"):
                        for tt in range(16):
                            pb = pbi[0] % 6
                            pbi[0] += 1

                            def mm(pe, k=k, tt=tt, pb=pb):
                                for kc in range(16):
                                    i = pe.matmul(PB[pb][:, :], lhsT=NT[:, kc, tt * 128:(tt + 1) * 128], rhs=wb[k][:, kc, :],
                                                  start=(kc == 0), stop=(kc == 15))
                                return i
                            S.op("pe", mm, reads=[NTr, NTr2, wbr[k]], writes=[PBr[pb]])
                            e = evi[0] % 3
                            evi[0] += 1
                            if kind == "vs":
                                S.op("act", lambda a, e=e, pb=pb: a.activation(out=ev[e][:], in_=PB[pb][:, :], func=AF.Gelu_apprx_tanh),
                                     reads=[PBr[pb]], writes=[evr[e]])
                                c0 = col0 - 1024
                                S.dma("sp", VS[tt * 128:(tt + 1) * 128, c0:c0 + 512], ev[e][:], reads=[evr[e]])
                            else:
                                S.op("act", lambda a, e=e, pb=pb: a.activation(out=ev[e][:], in_=PB[pb][:, :], func=AF.Copy),
                                     reads=[PBr[pb]], writes=[evr[e]])
                                h0 = (col0 - 6144) // 128
                                tile_g = tok0 // 128 + tt
                                dst = VA.rearrange("h p (t d) -> p h t d", d=128)[:, h0:h0 + 4, tile_g, :]
                                S.dma("sp", dst, ev[e][:].rearrange("p (h d) -> p h d", d=128), reads=[evr[e]])
                        continue
                    for tg in range(4):
                        for nch in range(4):
                            pb = pbi[0] % 6
                            pbi[0] += 1

                            def mm(pe, k=k, tg=tg, nch=nch, pb=pb):
                                for kc in range(16):
                                    i = pe.matmul(PB[pb][:, :], lhsT=wb[k][:, kc, nch * 128:(nch + 1) * 128],
                                                  rhs=NT[:, kc, tg * 512:(tg + 1) * 512], start=(kc == 0), stop=(kc == 15))
                                return i
                            S.op("pe", mm, reads=[NTr, NTr2, wbr[k]], writes=[PBr[pb]])
                            e = evi[0] % 3
                            evi[0] += 1
                            tsl = slice(tg * 512, (tg + 1) * 512)
                            if kind == "u":
                                ch = col0 // 128 + nch
                                S.op("act", lambda a, e=e, pb=pb: a.activation(out=ev[e][:], in_=PB[pb][:, :], func=AF.Gelu_apprx_tanh),
                                     reads=[PBr[pb]], writes=[evr[e]])
                                dst = UT.rearrange("t p (c k) -> p t c k", k=128)[:, tg * 4:(tg + 1) * 4, ch, :]
                                S.dma("sp", dst, ev[e][:].rearrange("p (t k) -> p t k", k=128), reads=[evr[e]])
                            elif kind in ("ga", "gb"):
                                ch = (col0 - (8192 if kind == "ga" else 10240)) // 128 + nch
                                S.op("act", lambda a, e=e, pb=pb: a.activation(out=ev[e][:], in_=PB[pb][:, :], func=AF.Sigmoid),
                                     reads=[PBr[pb]], writes=[evr[e]])
                                S.dma("sp", (GA if kind == "ga" else GB)[ch, :, tsl], ev[e][:], reads=[evr[e]])
                            else:
                                h = (col0 - (2048 if kind == "q" else 4096)) // 128 + nch
                                r = rpi[0] % 2
                                rpi[0] += 1
                                S.op("dve", lambda v, r=r, pb=pb, tsl=tsl: v.tensor_tensor(out=t1[r][:], in0=PB[pb][:, :], in1=cosT[:, tsl],
                                                                                          op=ALU.mult), reads=[PBr[pb], tabr], writes=[t1r[r]])
                                S.op("dve", lambda v, r=r, pb=pb, tsl=tsl: v.tensor_tensor(out=t2[r][0:64, :], in0=PB[pb][64:128, :],
                                                                                          in1=sinT[0:64, tsl], op=ALU.mult),
                                     reads=[PBr[pb], tabr], writes=[t2r[r]])
                                S.op("dve", lambda v, r=r, pb=pb, tsl=tsl: v.tensor_tensor(out=t2[r][64:128, :], in0=PB[pb][0:64, :],
                                                                                          in1=sinT[64:128, tsl], op=ALU.mult),
                                     reads=[PBr[pb], tabr], writes=[t2r[r]])
                                S.op("pool", lambda g, r=r, e=e: g.tensor_tensor(out=ev[e][:], in0=t1[r][:], in1=t2[r][:], op=ALU.add),
                                     reads=[t1r[r], t2r[r]], writes=[evr[e]])
                                if kind == "q":
                                    S.dma("sp", QT[h, :, tsl], ev[e][:], reads=[evr[e]])
                                else:
                                    S.dma("sp", KT[h, :, tok0 + tg * 512: tok0 + (tg + 1) * 512], ev[e][:], reads=[evr[e]])
                                    kb0 = (tok0 + tg * 512) // 256
                                    S.op("dve", lambda v, e=e, h=h, kb0=kb0: v.tensor_reduce(
                                        out=kmT[:, h, kb0:kb0 + 2], in_=ev[e][:].rearrange("p (b k) -> p b k", k=256),
                                        axis=AX.X, op=ALU.add), reads=[evr[e]], writes=[kmr])
                S.barrier()

        own_blocks = ([("u", 0), ("u", 512), ("vs", 1024), ("vs", 1536)]
                      + [("q", 2048 + 512 * i) for i in range(4)] + [("k", 4096 + 512 * i) for i in range(4)]
                      + [("va", 6144 + 512 * i) for i in range(4)] + [("ga", 8192 + 512 * i) for i in range(4)]
                      + [("gb", 10240 + 512 * i) for i in range(4)])
        oth_blocks = [("k", 4096 + 512 * i) for i in range(4)] + [("va", 6144 + 512 * i) for i in range(4)]
        if stop_after and stop_after.startswith("C0"):
            allb = dict(u=("u", 0), vs=("vs", 1024), q=("q", 2048), k=("k", 4096), va=("va", 6144), ga=("ga", 8192))
            sel = stop_after.split(":")[1].split(",") if ":" in stop_after else list(allb)
            own_blocks = [allb[z] for z in sel]
            stop_after = "C0"
        norm_stage(0)
        if "NT" in dbg:
            NTd = dscr_out("NTd", [128, 16 * NOWN], BF16)
            S.dma("sp", NTd[:, :], NT[:].rearrange("p a b -> p (a b)"), reads=[NTr, NTr2])
        if stop_after == "B":
            S.barrier()
            return nc
        proj_stage(0, own_blocks)
        if stop_after in ("C0", "C1"):
            if "kmT" in dbg:
                S.dma("sp", DBG[:, 0:256], kmT[:].rearrange("p a b -> p (a b)"), reads=[kmr])
            S.barrier()
            return nc
        norm_stage(NOWN)
        proj_stage(NOWN, oth_blocks)
        if "kmT" in dbg:
            S.dma("sp", DBG[:, 0:256], kmT[:].rearrange("p a b -> p (a b)"), reads=[kmr])

        ST = sb("ST", [128, 8, NOWN], BF16, sNT)
        STr = Res("ST")
        with ExitStack() as sd:
            lng = sb("lng", [128, 1024], F32, sd)
            lnb = sb("lnb", [128, 1024], F32, sd)
            wsf = sb("wsf", [128, 1024], F32, sd)
            wsb = sb("wsb", [128, 8, 128], BF16, sd)
            bsf = sb("bsf", [1, 1024], F32, sd)
            bsh = sb("bsh", [1, 1024], BF16, sd)
            bsl = sb("bsl", [1, 1024], BF16, sd)
            bst = sb("bst", [1, 1024], F32, sd)
            dcr = Res("dconst")
            S.dma("sp", lng[:], dram_bcast(ln_g[0:1, :], 1024), writes=[dcr])
            S.dma("sp", lnb[:], dram_bcast(ln_b[0:1, :], 1024), writes=[dcr])
            S.dma("sp", wsf[:], wsT_d[:, :], writes=[dcr])
            S.dma("sp", bsf[:], bs_d[:, :], writes=[dcr])
            for g in range(8):
                S.op("dve", lambda v, g=g: v.tensor_tensor(out=wsb[:, g, :], in0=wsf[:, g * 128:(g + 1) * 128],
                                                           in1=cp[:, CP_TRI01:CP_TRI01 + 128], op=ALU.mult),
                     reads=[dcr, cpr], writes=[dcr])
            S.op("dve", lambda v: v.tensor_copy(out=bsh[:], in_=bsf[:]), reads=[dcr], writes=[dcr])
            S.op("dve", lambda v: v.tensor_tensor(out=bst[:], in0=bsf[:], in1=bsh[:], op=ALU.subtract), reads=[dcr], writes=[dcr])
            S.op("dve", lambda v: v.tensor_copy(out=bsl[:], in_=bst[:]), reads=[dcr], writes=[dcr])
            vt = [sb(f"vt{i}", [128, 1024], BF16, sd) for i in range(2)]
            ut = [sb(f"ut{i}", [128, 8, 128], BF16, sd) for i in range(2)]
            vtr = [Res(f"vt{i}") for i in range(2)]
            utr = [Res(f"ut{i}") for i in range(2)]
            st6 = sb("st6", [128, 12], F32, sd)
            mv = sb("mv", [128, 2], F32, sd)
            rs_ = sb("rs_", [128, 1], F32, sd)
            vn = sb("vn", [128, 1024], F32, sd)
            vln = [sb(f"vln{i}", [128, 1024], BF16, sd) for i in range(2)]
            smr, vnr = Res("dsmall"), Res("vn")
            vlr = [Res(f"vln{i}") for i in range(2)]
            for tt in range(16):
                k = tt % 2
                S.dma("sp", vt[k][:], VS[tt * 128:(tt + 1) * 128, :], writes=[vtr[k]])
                S.dma("sp", ut[k][:].rearrange("p c k -> p (c k)"), UT[tt, :, :], writes=[utr[k]])
                S.op("dve", lambda v, k=k: v.bn_stats(out=st6[:, 0:6], in_=vt[k][:, 0:512]), reads=[vtr[k]], writes=[smr])
                S.op("dve", lambda v, k=k: v.bn_stats(out=st6[:, 6:12], in_=vt[k][:, 512:1024]), reads=[vtr[k]], writes=[smr])
                S.op("dve", lambda v: v.bn_aggr(out=mv[:], in_=st6[:]), reads=[smr], writes=[smr])
                S.op("dve", lambda v: v.tensor_scalar(out=rs_[:], in0=mv[:, 1:2], scalar1=EPS, scalar2=None, op0=ALU.add),
                     reads=[smr], writes=[smr])
                S.op("act", lambda a: a.activation(out=rs_[:], in_=rs_[:], func=AF.Sqrt), reads=[smr], writes=[smr])
                S.op("dve", lambda v: v.reciprocal(out=rs_[:], in_=rs_[:]), reads=[smr], writes=[smr])
                S.op("dve", lambda v, k=k: v.tensor_scalar(out=vn[:], in0=vt[k][:], scalar1=mv[:, 0:1], scalar2=rs_[:, 0:1],
                                                            op0=ALU.subtract, op1=ALU.mult), reads=[vtr[k], smr], writes=[vnr])
                S.op("dve", lambda v: v.tensor_tensor(out=vn[:], in0=vn[:], in1=lng[:], op=ALU.mult), reads=[vnr, dcr], writes=[vnr])
                S.op("pool", lambda g_, k=k: g_.tensor_tensor(out=vln[k][:], in0=vn[:], in1=lnb[:], op=ALU.add),
                     reads=[vnr, dcr], writes=[vlr[k]])
                for half in range(2):
                    pb = half

                    def mm(pe, k=k, half=half, pb=pb):
                        for gg in range(4):
                            g = half * 4 + gg
                            o = PB[pb][:, gg * 128:(gg + 1) * 128]
                            pe.matmul(o, lhsT=vln[k][:, g * 128:(g + 1) * 128], rhs=wsb[:, g, :], start=True, stop=False)
                            pe.matmul(o, lhsT=onesb[0:1, :], rhs=bsh[0:1, g * 128:(g + 1) * 128], start=False, stop=False)
                            i = pe.matmul(o, lhsT=onesb[0:1, :], rhs=bsl[0:1, g * 128:(g + 1) * 128], start=False, stop=True)
                        return i
                    S.op("pe", mm, reads=[vlr[k], dcr, constr], writes=[PBr[pb]])
                    S.op("dve", lambda v, k=k, half=half, pb=pb, tt=tt: v.tensor_tensor(
                        out=ST[:, half * 4:half * 4 + 4, tt * 128:(tt + 1) * 128],
                        in0=PB[pb][:, :].rearrange("p (g i) -> p g i", i=128), in1=ut[k][:, half * 4:half * 4 + 4, :], op=ALU.mult),
                        reads=[PBr[pb], utr[k]], writes=[STr])
            S.barrier()
        if "ST" in dbg:
            STd = dscr_out("STd", [128, 8 * NOWN], BF16)
            S.dma("sp", STd[:, :], ST[:].rearrange("p a b -> p (a b)"), reads=[STr])
        if stop_after == "D":
            S.barrier()
            return nc

        AT, ATr = NT, Res("AT")
        SCALE = float(DH) ** -0.5
        with ExitStack() as se:
            kt = [sb(f"kt{i}", [128, SEQ], BF16, se) for i in range(2)]
            va = [sb(f"va{i}", [128, 32, 132], BF16, se) for i in range(2)]
            qt = [sb(f"qt{i}", [128, NOWN], BF16, se) for i in range(2)]
            hr = [Res(f"hd{i}") for i in range(2)]
            hkr = [Res(f"hk{i}") for i in range(2)]
            hqr = [Res(f"hq{i}") for i in range(2)]
            kmb = [sb(f"kmb{i}", [128, 16], BF16, se) for i in range(2)]
            kmbr = [Res(f"kmb{i}") for i in range(2)]
            for i in range(2):
                S.op("dve", lambda v, i=i: v.memset(va[i][:, :, 128:129], 1.0), writes=[hr[i]])
            gm = sb("gm", [128, 2, 16], F32, se)
            m8 = sb("m8", [128, 2, 8], F32, se)
            selt = sb("selt", [128, 2, 16], F32, se)
            Mq = sb("Mq", [128, 2, 16], BF16, se)
            MT = [sb(f"MT{i}", [16, 256], BF16, se) for i in range(2)]
            gsr = Res("gsmall")
            Mqr = Res("Mq")
            MTr = [Res(f"MT{i}") for i in range(2)]
            pt_ = [sb(f"pexp{i}", [128, 256], BF16, se) for i in range(4)]
            ptr = [Res(f"pexp{i}") for i in range(4)]
            rden = sb("rden", [128, 2], F32, se)
            atn = [sb(f"atn{i}", [128, 128], BF16, se) for i in range(2)]
            atr = [Res(f"atn{i}") for i in range(2)]
            rdr = Res("rden")
            sidx = [0]
            pidx = [0]
            units = [(h, i) for h in range(NH) for i in range(8)]

            def load_head(h):
                hb = h % 2
                S.dma("sp", qt[hb][:], QT[h, :, :], writes=[hqr[hb]])
                S.dma("sp", kt[hb][:], KT[h, :, :], writes=[hkr[hb]])
                S.dma("sp", va[hb][:, :, 0:128], VA[h, :, :].rearrange("p (t d) -> p t d", d=128), writes=[hr[hb]])
                S.op("dve", lambda v, hb=hb, h=h: v.tensor_copy(out=kmb[hb][:], in_=kmT[:, h, :]), reads=[kmr], writes=[kmbr[hb]])

            def prep_a(u):
                h, i = units[u]
                hb = h % 2

                def gmm(pe):
                    for q2 in range(2):
                        q0 = i * 256 + q2 * 128
                        ins = pe.matmul(PB[5][:, q2 * 16:(q2 + 1) * 16], lhsT=qt[hb][:, q0:q0 + 128], rhs=kmb[hb][:, :],
                                        start=True, stop=True)
                    return ins
                S.op("pe", gmm, reads=[hqr[hb], kmbr[hb]], writes=[PBr[5]])
                for q2 in range(2):
                    S.op("dve", lambda v, q2=q2: v.tensor_tensor(
                        out=gm[:, q2, :], in0=PB[5][:, q2 * 16:(q2 + 1) * 16],
                        in1=cp[:, CP_PASTPEN + i * 16:CP_PASTPEN + (i + 1) * 16], op=ALU.add), reads=[PBr[5], cpr], writes=[gsr])
                    S.op("dve", lambda v, q2=q2: v.max(out=m8[:, q2, :], in_=gm[:, q2, :]), reads=[gsr], writes=[gsr])
                    S.op("dve", lambda v, q2=q2: v.tensor_scalar(out=selt[:, q2, :], in0=gm[:, q2, :], scalar1=m8[:, q2, 2:3],
                                                                  scalar2=None, op0=ALU.is_ge), reads=[gsr], writes=[gsr])
                    S.op("dve", lambda v, q2=q2: v.tensor_tensor(
                        out=selt[:, q2, :], in0=selt[:, q2, :], in1=cp[:, CP_PAST01 + i * 16:CP_PAST01 + (i + 1) * 16],
                        op=ALU.mult), reads=[gsr, cpr], writes=[gsr])
                    S.op("dve", lambda v, q2=q2: v.tensor_scalar(out=Mq[:, q2, :], in0=selt[:, q2, :], scalar1=-1.0, scalar2=BIG,
                                                                  op0=ALU.add, op1=ALU.mult), reads=[gsr], writes=[Mqr])

            def prep_b(u):
                mb = u % 2

                def mtr(pe):
                    for q2 in range(2):
                        ins = pe.transpose(out=PT[0][0:16, q2 * 128:(q2 + 1) * 128], in_=Mq[:, q2, :], identity=identb[:])
                    return ins
                S.op("pe", mtr, reads=[Mqr, constr], writes=[PTr[0]])
                S.op("act", lambda a: a.activation(out=MT[mb][:], in_=PT[0][0:16, 0:256], func=AF.Copy),
                     reads=[PTr[0]], writes=[MTr[mb]])

            load_head(0)
            prep_a(0)
            prep_b(0)
            zt_ = sb("zfill", [128, 8192], BF16, se)
            zr_ = Res("zfill")
            S.op("pool", lambda g_: g_.memset(zt_[:], 0.0), writes=[zr_])
            XSz = XS.rearrange("(c p r) d -> c p (r d)", p=128, r=4)
            bg = []
            for (dst_, src_) in ((WGB, weg), (WUB, weu), (WDB, wed)):
                for c_ in range(32):
                    bg.append((dst_[c_ * 512:(c_ + 1) * 512, :], src_[c_ * 512:(c_ + 1) * 512, :], []))
            for c_ in range(CAPROWS // 512):
                bg.append((XSz[c_, :, :], zt_[:], [zr_]))
            LAG = 2
            for u, (h, i) in enumerate(units):
                hb = h % 2
                mb = u % 2
                if i == 0 and h + 1 < NH:
                    load_head(h + 1)
                if u + 1 < len(units):
                    prep_a(u + 1)
                tiles = []
                for kbl in list(range(0, i)) + list(range(8, 8 + i + 1)):
                    for c in range(2):
                        tiles.append(("past", kbl, c))
                tiles += [("d00", i, 0), ("d01", i, 0), ("d11", i, 1)]
                nt0 = sum(1 for t_ in tiles if t_[0] in ("past", "d00"))
                nt1 = sum(1 for t_ in tiles if t_[0] in ("past", "d01", "d11"))
                c0 = [0]
                c1 = [0]
                pend = []

                def emit_pv(pk, ktile, qlist):
                    def pvm(pe):
                        for (qq_, off) in qlist:
                            cnt_, tot = (c0, nt0) if qq_ == 0 else (c1, nt1)
                            ins = pe.matmul(PB[3 + qq_][:, 0:129], lhsT=pt_[pk][:, off:off + 128], rhs=va[hb][:, ktile, 0:129],
                                            start=(cnt_[0] == 0), stop=(cnt_[0] == tot - 1))
                            cnt_[0] += 1
                        return ins
                    S.op("pe", pvm, reads=[ptr[pk], hr[hb]], writes=[PBr[3], PBr[4]])

                prepb_at = min(len(tiles) - 1, max(2, len(tiles) // 2))
                for ti, (typ, kbl, c) in enumerate(tiles):
                    sbk = sidx[0] % 3
                    sidx[0] += 1
                    pk = pidx[0] % 4
                    pidx[0] += 1
                    k0 = kbl * 256 + c * 128
                    ktile = kbl * 2 + c
                    if typ == "past":
                        def smm(pe, k0=k0, kbl=kbl, sbk=sbk):
                            pe.matmul(PB[sbk][:, 0:256], lhsT=kt[hb][:, k0:k0 + 128], rhs=qt[hb][:, i * 256:(i + 1) * 256],
                                      start=True, stop=False)
                            return pe.matmul(PB[sbk][:, 0:256], lhsT=eallb[0:16, kbl * 128:(kbl + 1) * 128], rhs=MT[mb][0:16, :],
                                             start=False, stop=True)
                        S.op("pe", smm, reads=[hkr[hb], hqr[hb], MTr[mb], constr], writes=[PBr[sbk]])
                        S.op("act", lambda a, sbk=sbk, pk=pk: a.activation(out=pt_[pk][:, :], in_=PB[sbk][:, 0:256], func=AF.Exp,
                                                                          scale=SCALE), reads=[PBr[sbk]], writes=[ptr[pk]])
                        qlist = ((0, 0), (1, 128))
                    else:
                        qq = 0 if typ == "d00" else 1
                        q0 = i * 256 + qq * 128
                        tri = typ in ("d00", "d11")

                        def smm(pe, k0=k0, q0=q0, sbk=sbk, tri=tri):
                            ins = pe.matmul(PB[sbk][:, 0:128], lhsT=kt[hb][:, k0:k0 + 128], rhs=qt[hb][:, q0:q0 + 128],
                                            start=True, stop=not tri)
                            if tri:
                                ins = pe.matmul(PB[sbk][:, 0:128], lhsT=identb[:], rhs=tripenb[:], start=False, stop=True)
                            return ins
                        S.op("pe", smm, reads=[hkr[hb], hqr[hb], constr], writes=[PBr[sbk]])
                        S.op("act", lambda a, sbk=sbk, pk=pk: a.activation(out=pt_[pk][:, 0:128], in_=PB[sbk][:, 0:128], func=AF.Exp,
                                                                          scale=SCALE), reads=[PBr[sbk]], writes=[ptr[pk]])
                        qlist = ((qq, 0),)
                    pend.append((pk, ktile, qlist))
                    if len(pend) > LAG:
                        emit_pv(*pend.pop(0))
                    if ti == prepb_at and u + 1 < len(units):
                        prep_b(u + 1)
                while pend:
                    emit_pv(*pend.pop(0))
                for q2 in range(2):
                    S.op("dve", lambda v, q2=q2: v.reciprocal(out=rden[:, q2:q2 + 1], in_=PB[3 + q2][:, 128:129]),
                         reads=[PBr[3 + q2]], writes=[rdr])
                    S.op("dve", lambda v, q2=q2: v.tensor_scalar(out=atn[q2][:], in0=PB[3 + q2][:, 0:128], scalar1=rden[:, q2:q2 + 1],
                                                                  scalar2=None, op0=ALU.mult), reads=[PBr[3 + q2], rdr], writes=[atr[q2]])
                    S.op("pe", lambda pe, q2=q2: pe.transpose(out=PT[1][:, q2 * 128:(q2 + 1) * 128], in_=atn[q2][:], identity=identb[:]),
                         reads=[atr[q2], constr], writes=[PTr[1]])
                S.op("act", lambda a, h=h, i=i: a.activation(out=AT[:, h, i * 256:(i + 1) * 256], in_=PT[1][:, 0:256], func=AF.Copy),
                     reads=[PTr[1]], writes=[ATr])
                if bg:
                    o_, i_, rd_ = bg.pop(0)
                    S.dma("pool", o_, i_, reads=rd_, after=[ATr.w])
            while bg:
                o_, i_, rd_ = bg.pop(0)
                S.dma("pool", o_, i_, reads=rd_)
            S.barrier()
        if "AT" in dbg:
            ATd = dscr_out("ATd", [128, 16 * NOWN], BF16)
            S.dma("sp", ATd[:, :], AT[:].rearrange("p a b -> p (a b)"), reads=[ATr])
        if stop_after == "E":
            S.barrier()
            return nc
        wso_v = w_sgu_out.rearrange("(kc p) n -> p kc n", p=128)
        wmo_v = w_moba_out.rearrange("(kc p) n -> p kc n", p=128)
        wo_v = w_out.rearrange("(kc p) n -> p kc n", p=128)
        with ExitStack() as sf:
            wso = [sb(f"wso{i}", [128, 8, 512], BF16, sf) for i in range(2)]
            wmo = [sb(f"wmo{i}", [128, 16, 512], BF16, sf) for i in range(2)]
            wfr = [Res(f"wf{i}") for i in range(2)]
            wfr2 = [Res(f"wfb{i}") for i in range(2)]
            gat = [sb(f"gat{i}", [128, 512], BF16, sf) for i in range(2)]
            gbt = [sb(f"gbt{i}", [128, 512], BF16, sf) for i in range(2)]
            ggr = [Res(f"gg{i}") for i in range(2)]
            m1 = [sb(f"m1{i}", [128, 512], F32, sf) for i in range(2)]
            m2 = [sb(f"m2{i}", [128, 512], F32, sf) for i in range(2)]
            mr = [Res(f"m{i}") for i in range(2)]
            mgt = [sb(f"mgt{i}", [128, 512], BF16, sf) for i in range(2)]
            mgr = [Res(f"mgt{i}") for i in range(2)]
            it = 0
            for nb in range(4):
                wk = nb % 2
                S.dma("pool", wso[wk][:], wso_v[:, :, nb * 512:(nb + 1) * 512], writes=[wfr[wk]])
                S.dma("pool", wmo[wk][:], wmo_v[:, :, nb * 512:(nb + 1) * 512], writes=[wfr2[wk]])
                for tg in range(4):
                    tsl = slice(tg * 512, (tg + 1) * 512)
                    for nch in range(4):
                        k = it % 2
                        it += 1
                        ch = nb * 4 + nch
                        pa, pb2 = 2 * ((it - 1) % 3), 2 * ((it - 1) % 3) + 1
                        S.dma("sp", gat[k][:], GA[ch, :, tsl], writes=[ggr[k]])
                        S.dma("sp", gbt[k][:], GB[ch, :, tsl], writes=[ggr[k]])

                        def mma(pe, wk=wk, nch=nch, tsl=tsl, pa=pa):
                            for kc in range(8):
                                ins = pe.matmul(PB[pa][:, :], lhsT=wso[wk][:, kc, nch * 128:(nch + 1) * 128], rhs=ST[:, kc, tsl],
                                                start=(kc == 0), stop=(kc == 7))
                            return ins

                        def mmb(pe, wk=wk, nch=nch, tsl=tsl, pb2=pb2):
                            for kc in range(16):
                                ins = pe.matmul(PB[pb2][:, :], lhsT=wmo[wk][:, kc, nch * 128:(nch + 1) * 128], rhs=AT[:, kc, tsl],
                                                start=(kc == 0), stop=(kc == 15))
                            return ins
                        S.op("pe", mma, reads=[wfr[wk], STr], writes=[PBr[pa]])
                        S.op("pe", mmb, reads=[wfr2[wk], ATr], writes=[PBr[pb2]])
                        S.op("dve", lambda v, k=k, pa=pa: v.tensor_tensor(out=m1[k][:], in0=PB[pa][:, :], in1=gat[k][:], op=ALU.mult),
                             reads=[PBr[pa], ggr[k]], writes=[mr[k]])
                        S.op("dve", lambda v, k=k, pb2=pb2: v.tensor_tensor(out=m2[k][:], in0=PB[pb2][:, :], in1=gbt[k][:], op=ALU.mult),
                             reads=[PBr[pb2], ggr[k]], writes=[mr[k]])
                        S.op("pool", lambda g_, k=k: g_.tensor_tensor(out=mgt[k][:], in0=m1[k][:], in1=m2[k][:], op=ALU.add),
                             reads=[mr[k]], writes=[mgr[k]])
                        S.dma("pool", MG[ch, :, tsl], mgt[k][:], reads=[mgr[k]])
            S.barrier()
        MGs, MGr = NT, [Res(f"MGs{i}") for i in range(16)]
        with ExitStack() as sg:
            for ch in range(16):
                S.dma("sp", MGs[:, ch, :], MG[ch, :, :], writes=[MGr[ch]])
            wo = [sb(f"wo{i}", [128, 16, 512], BF16, sg) for i in range(2)]
            wor = [Res(f"wo{i}") for i in range(2)]
            xp = [sb(f"xp{i}", [128, 512], F32, sg) for i in range(2)]
            xpr = [Res(f"xp{i}") for i in range(2)]
            hp = [sb(f"hp{i}", [128, 512], F32, sg) for i in range(2)]
            hpr = [Res(f"hp{i}") for i in range(2)]
            ho = [sb(f"ho{i}", [128, 512], F32, sg) for i in range(2)]
            hor = [Res(f"ho{i}") for i in range(2)]
            it = 0
            for nb in range(4):
                wk = nb % 2
                csl = slice(nb * 512, (nb + 1) * 512)
                S.dma("pool", wo[wk][:], wo_v[:, :, csl], writes=[wor[wk]])
                for tt in range(16):
                    k = it % 2
                    pb = it % 6
                    it += 1
                    S.dma("sp", xp[k][:], x[tt * 128:(tt + 1) * 128, csl], writes=[xpr[k]])

                    def mmo(pe, wk=wk, tt=tt, pb=pb):
                        for kc in range(16):
                            ins = pe.matmul(PB[pb][:, :], lhsT=MGs[:, kc, tt * 128:(tt + 1) * 128], rhs=wo[wk][:, kc, :],
                                            start=(kc == 0), stop=(kc == 15))
                        return ins
                    S.op("pe", mmo, reads=MGr + [wor[wk]], writes=[PBr[pb]])
                    S.op("dve", lambda v, k=k, pb=pb, csl=csl: v.tensor_tensor(out=hp[k][:], in0=PB[pb][:, :], in1=gm_bc[:, csl], op=ALU.mult),
                         reads=[PBr[pb], bcr], writes=[hpr[k]])
                    S.op("pool", lambda g_, k=k: g_.tensor_tensor(out=ho[k][:], in0=hp[k][:], in1=xp[k][:], op=ALU.add),
                         reads=[hpr[k], xpr[k]], writes=[hor[k]])
                    S.dma("pool", H1[tt * 128:(tt + 1) * 128, csl], ho[k][:], reads=[hor[k]])
            S.barrier()
        if stop_after == "G":
            return nc
        n2b_all, n2r = NT, Res("n2b")
        weg_v, weu_v, wed_v = WGB, WUB, WDB
        with ExitStack() as sh:
            c2 = sb("c2", [128, C2_W], F32, sh)
            c2r = Res("c2")
            S.dma("sp", c2[:], cpack2[:, :], writes=[c2r])
            wrf = sb("wrf", [128, 16, 36], F32, sh)
            brb = sb("brb", [128, 36], F32, sh)
            S.dma("sp", wrf[:].rearrange("p a b -> p (a b)"), wr_d[:, :], writes=[c2r])
            S.dma("sp", brb[:], dram_bcast(br_d[0:1, :], 36), writes=[c2r])
            oh1_all = sb("oh1", [128, 16, 32], F32, sh)
            oh2_all = sb("oh2", [128, 16, 32], F32, sh)
            cnt_all = sb("cnta", [128, 16, 32], BF16, sh)
            allr = Res("routeall")
            lg_all = sb("lg_all", [128, 16, 36], F32, sh)
            gmx16 = sb("gmx16", [128, 16], F32, sh)
            ohg16 = sb("ohg16", [128, 16, 4], F32, sh)
            d416 = sb("d416", [128, 16, 4], F32, sh)
            pg16 = sb("pg16", [128, 16], F32, sh)
            elm16 = sb("elm16", [128, 16, 32], F32, sh)
            m816 = sb("m816", [128, 16, 8], F32, sh)
            e16 = sb("e16", [128, 16], F32, sh)
            sm = sb("sm", [128, 64], F32, sh)
            elm = sb("elm", [128, 32], F32, sh)
            m8r = sb("m8r", [128, 8], F32, sh)
            ex4 = sb("ex4", [128, 4], F32, sh)
            rr = Res("rsmall")
            sh1 = sh.enter_context(ExitStack())
            h1t = [sb(f"h1t{i}", [128, D], F32, sh1) for i in range(2)]
            h1r = [Res(f"h1t{i}") for i in range(2)]
            n2f = sb("n2f", [128, D], F32, sh1)
            n2fr = Res("n2f")
            junk = sb("hjunk", [128, D], BF16, sh1)
            jr = Res("hjunk")
            ssq = sb("hssq", [128, 1], F32, sh1)
            sr = Res("hssq")
            n2T = sb("n2T", [128, 16, 128], F32, sh1)
            n2Tr = Res("n2T")
            for tt in range(16):
                k = tt % 2
                S.dma("sp", h1t[k][:], H1[tt * 128:(tt + 1) * 128, :], writes=[h1r[k]])
                S.op("act", lambda a, k=k: a.activation(out=junk[:], in_=h1t[k][:], func=AF.Square, accum_out=ssq[:]),
                     reads=[h1r[k]], writes=[jr, sr])
                S.op("act", lambda a: a.activation(out=ssq[:], in_=ssq[:], func=AF.Sqrt, scale=1.0 / D, bias=epsc[:, 0:1]),
                     reads=[sr, constr], writes=[sr])
                S.op("dve", lambda v: v.reciprocal(out=ssq[:], in_=ssq[:]), reads=[sr], writes=[sr])
                S.op("dve", lambda v, k=k: v.scalar_tensor_tensor(out=n2f[:], in0=h1t[k][:], scalar=ssq[:, 0:1], in1=G2_bc[:],
                                                                   op0=ALU.mult, op1=ALU.mult), reads=[h1r[k], sr, bcr], writes=[n2fr])
                S.op("pool", lambda g_: g_.tensor_tensor(out=n2f[:], in0=n2f[:], in1=shf_bc[:], op=ALU.add), reads=[n2fr, bcr], writes=[n2fr])
                S.op("act", lambda a, tt=tt: a.activation(out=n2b_all[:, tt, :], in_=n2f[:], func=AF.Copy), reads=[n2fr], writes=[n2r])
                for b4 in range(4):
                    def trf(pe, b4=b4):
                        for jj in range(4):
                            kc = b4 * 4 + jj
                            ins = pe.transpose(out=PB[b4][:, jj * 128:(jj + 1) * 128], in_=n2f[:, kc * 128:(kc + 1) * 128],
                                               identity=cp[:, CP_IDENT:CP_IDENT + 128])
                        return ins
                    S.op("pe", trf, reads=[n2fr, cpr], writes=[PBr[b4]])
                    if b4 % 2 == 0:
                        S.op("dve", lambda v, b4=b4: v.tensor_copy(out=n2T[:, b4 * 4:(b4 + 1) * 4, :].rearrange("p a b -> p (a b)"),
                                                                   in_=PB[b4][:, :]), reads=[PBr[b4]], writes=[n2Tr])
                    else:
                        S.op("act", lambda a, b4=b4: a.activation(out=n2T[:, b4 * 4:(b4 + 1) * 4, :].rearrange("p a b -> p (a b)"),
                                                                  in_=PB[b4][:, :], func=AF.Copy), reads=[PBr[b4]], writes=[n2Tr])

                def lgm(pe):
                    for kc in range(16):
                        ins = pe.matmul(PB[4][:, 0:36], lhsT=n2T[:, kc, :], rhs=wrf[:, kc, :], start=(kc == 0), stop=(kc == 15))
                    return ins
                S.op("pe", lgm, reads=[n2Tr, c2r], writes=[PBr[4]])
                S.op("dve", lambda v, tt=tt: v.tensor_tensor(out=lg_all[:, tt, :], in0=PB[4][:, 0:36], in1=brb[:], op=ALU.add),
                     reads=[PBr[4], c2r], writes=[rr])
            gl = lg_all[:, :, 0:4]
            S.op("dve", lambda v: v.tensor_reduce(out=gmx16[:], in_=gl, axis=AX.X, op=ALU.max), reads=[rr], writes=[rr])
            S.op("dve", lambda v: v.tensor_tensor(out=ohg16[:], in0=gl, in1=gmx16[:].unsqueeze(2).broadcast_to([128, 16, 4]), op=ALU.is_ge),
                 reads=[rr], writes=[rr])
            S.op("dve", lambda v: v.tensor_tensor(out=d416[:], in0=gl, in1=gmx16[:].unsqueeze(2).broadcast_to([128, 16, 4]), op=ALU.subtract),
                 reads=[rr], writes=[rr])
            S.op("act", lambda a: a.activation(out=d416[:], in_=d416[:], func=AF.Exp), reads=[rr], writes=[rr])
            S.op("dve", lambda v: v.tensor_reduce(out=pg16[:], in_=d416[:], axis=AX.X, op=ALU.add), reads=[rr], writes=[rr])
            S.op("dve", lambda v: v.reciprocal(out=pg16[:], in_=pg16[:]), reads=[rr], writes=[rr])
            S.op("dve", lambda v: v.tensor_scalar(out=ohg16[:], in0=ohg16[:], scalar1=-1.0, scalar2=1e9, op0=ALU.add, op1=ALU.mult),
                 reads=[rr], writes=[rr])
            S.op("dve", lambda v: v.tensor_tensor(out=elm16[:].rearrange("p t (g e) -> p t g e", e=8),
                                                  in0=lg_all[:, :, 4:36].rearrange("p t (g e) -> p t g e", e=8),
                                                  in1=ohg16[:].unsqueeze(3).broadcast_to([128, 16, 4, 8]), op=ALU.add), reads=[rr], writes=[rr])
            for tt in range(16):
                S.op("dve", lambda v, tt=tt: v.max(out=m816[:, tt, :], in_=elm16[:, tt, :]), reads=[rr], writes=[rr])
            S.op("dve", lambda v: v.tensor_tensor(out=oh1_all[:], in0=elm16[:], in1=m816[:, :, 0:1].broadcast_to([128, 16, 32]), op=ALU.is_equal),
                 reads=[rr], writes=[allr])
            S.op("dve", lambda v: v.tensor_tensor(out=oh2_all[:], in0=elm16[:], in1=m816[:, :, 1:2].broadcast_to([128, 16, 32]), op=ALU.is_equal),
                 reads=[rr], writes=[allr])
            S.op("dve", lambda v: v.tensor_tensor(out=cnt_all[:], in0=oh1_all[:], in1=oh2_all[:], op=ALU.add), reads=[allr], writes=[allr])
            S.op("dve", lambda v: v.tensor_tensor(out=e16[:], in0=m816[:, :, 1], in1=m816[:, :, 0], op=ALU.subtract), reads=[rr], writes=[rr])
            S.op("act", lambda a: a.activation(out=e16[:], in_=e16[:], func=AF.Exp), reads=[rr], writes=[rr])
            S.op("dve", lambda v: v.tensor_scalar(out=w1_all[:], in0=e16[:], scalar1=1.0, scalar2=None, op0=ALU.add), reads=[rr], writes=[wallr])
            S.op("dve", lambda v: v.reciprocal(out=w1_all[:], in_=w1_all[:]), reads=[wallr], writes=[wallr])
            S.op("dve", lambda v: v.tensor_tensor(out=w1_all[:], in0=w1_all[:], in1=pg16[:], op=ALU.mult), reads=[wallr, rr], writes=[wallr])
            S.op("dve", lambda v: v.tensor_tensor(out=w2_all[:], in0=w1_all[:], in1=e16[:], op=ALU.mult), reads=[wallr, rr], writes=[wallr])
            S.barrier()
            sh1.close()
            cntf = sb("cntf", [128, 32], F32, sh)
            nblk = sb("nblk", [128, 32], F32, sh)
            endb = sb("endb", [128, 32], F32, sh)
            endt = sb("endt", [128, 32], F32, sh)
            startp = sb("startp", [128, 32], F32, sh)
            cmp1 = sb("cmp1", [128, 32, 32], F32, sh)
            cmp2 = sb("cmp2", [128, 64, 32], F32, sh)
            bef = sb("bef", [128, 64], F32, sh)
            idxf = sb("idxf", [128, 64, 4], F32, sh)
            p2r = Res("pass2")

            def cmm(pe):
                for tt in range(16):
                    ins = pe.matmul(PB[5][:, 0:32], lhsT=onesb[:], rhs=cnt_all[:, tt, :], start=(tt == 0), stop=(tt == 15))
                return ins
            S.op("pe", cmm, reads=[allr, constr], writes=[PBr[5]])
            S.op("dve", lambda v: v.tensor_copy(out=cntf[:], in_=PB[5][:, 0:32]), reads=[PBr[5]], writes=[p2r])
            S.op("dve", lambda v: v.tensor_tensor(out=cmp1[:], in0=cntf[:].unsqueeze(2).broadcast_to([128, 32, 32]),
                                                  in1=c2[:, C2_THR:C2_THR + 1024].rearrange("p (a b) -> p a b", b=32), op=ALU.is_gt),
                 reads=[p2r, c2r], writes=[p2r])
            S.op("dve", lambda v: v.tensor_reduce(out=nblk[:], in_=cmp1[:], axis=AX.X, op=ALU.add), reads=[p2r], writes=[p2r])
            S.op("dve", lambda v: v.tensor_copy(out=endb[:], in_=nblk[:]), reads=[p2r], writes=[p2r])
            for sft in (1, 2, 4, 8, 16):
                S.op("dve", lambda v: v.tensor_copy(out=endt[:], in_=endb[:]), reads=[p2r], writes=[p2r])
                S.op("dve", lambda v, sft=sft: v.tensor_tensor(out=endb[:, sft:32], in0=endt[:, sft:32], in1=endt[:, 0:32 - sft], op=ALU.add),
                     reads=[p2r], writes=[p2r])
            S.op("dve", lambda v: v.tensor_tensor(out=startp[:], in0=endb[:], in1=nblk[:], op=ALU.subtract), reads=[p2r], writes=[p2r])
            S.op("dve", lambda v: v.tensor_scalar(out=startp[:], in0=startp[:], scalar1=float(RB), scalar2=None, op0=ALU.mult), reads=[p2r], writes=[p2r])
            S.op("dve", lambda v: v.tensor_tensor(out=cmp2[:], in0=endb[:].unsqueeze(1).broadcast_to([128, 64, 32]),
                                                  in1=c2[:, C2_BLKI:C2_BLKI + 2048].rearrange("p (a b) -> p a b", b=32), op=ALU.is_le),
                 reads=[p2r, c2r], writes=[p2r])
            S.op("dve", lambda v: v.tensor_reduce(out=bef[:], in_=cmp2[:], axis=AX.X, op=ALU.add), reads=[p2r], writes=[p2r])
            S.op("dve", lambda v: v.tensor_scalar(out=bef[:], in0=bef[:], scalar1=31.0, scalar2=512.0, op0=ALU.min, op1=ALU.mult),
                 reads=[p2r], writes=[p2r])
            S.op("dve", lambda v: v.tensor_scalar(out=sm[:, 32:33], in0=cp[:, CP_IOTAP:CP_IOTAP + 1], scalar1=4.0, scalar2=None, op0=ALU.mult),
                 reads=[cpr, rr], writes=[rr])
            for q in range(4):
                S.op("dve", lambda v, q=q: v.tensor_scalar(out=idxf[:, :, q], in0=bef[:], scalar1=sm[:, 32:33], scalar2=float(q),
                                                           op0=ALU.add, op1=ALU.add), reads=[p2r, rr], writes=[p2r])
            S.op("dve", lambda v: v.tensor_scalar(out=bef[:], in0=c2[:, C2_BLKI:C2_BLKI + 2048].rearrange("p (a b) -> p a b", b=32)[:, :, 0],
                                                  scalar1=endb[:, 31:32], scalar2=1.0e6, op0=ALU.is_ge, op1=ALU.mult), reads=[p2r, c2r], writes=[p2r])
            for q in range(4):
                S.op("dve", lambda v, q=q: v.tensor_tensor(out=idxf[:, :, q], in0=idxf[:, :, q], in1=bef[:], op=ALU.add), reads=[p2r], writes=[p2r])
            S.op("dve", lambda v: v.tensor_copy(out=idx4[:], in_=idxf[:].rearrange("p a b -> p (a b)")), reads=[p2r], writes=[idxr])
            if "ROUTE" in dbg:
                S.dma("sp", DBG[:, 0:64], bef[:], reads=[p2r])
                S.dma("sp", DBG[:, 64:96], cntf[:], reads=[p2r])
                S.dma("sp", DBG[:, 96:128], startp[:], reads=[p2r])
            dest = sb("dest", [128, 32], F32, sh)
            tmp32 = sb("tmp32", [128, 32], F32, sh)
            dr = Res("dest")
            cap_reg = nc.gpsimd.to_reg(CAPROWS - 1)
            dcol = [sb(f"dcol{i}", [128, 1], I32, sh) for i in range(4)]
            dcr2 = [Res(f"dcol{i}") for i in range(4)]
            di = 0
            for tt in range(16):
                def rmm(pe, tt=tt):
                    for t2_ in range(tt):
                        pe.matmul(PB[5][:, 0:32], lhsT=onesb[:], rhs=cnt_all[:, t2_, :], start=(t2_ == 0), stop=False)
                    return pe.matmul(PB[5][:, 0:32], lhsT=lstrb[:], rhs=cnt_all[:, tt, :], start=(tt == 0), stop=True)
                S.op("pe", rmm, reads=[allr, constr], writes=[PBr[5]])
                S.op("dve", lambda v: v.tensor_tensor(out=dest[:], in0=PB[5][:, 0:32], in1=startp[:], op=ALU.add), reads=[PBr[5], p2r], writes=[dr])
                for kk, (oh, dall) in enumerate(((oh1_all, d1f), (oh2_all, d2f))):
                    S.op("dve", lambda v, oh=oh, tt=tt: v.tensor_tensor(out=tmp32[:], in0=oh[:, tt, :], in1=dest[:], op=ALU.mult),
                         reads=[allr, dr], writes=[dr])
                    S.op("dve", lambda v, dall=dall, tt=tt: v.tensor_reduce(out=dall[:, tt:tt + 1], in_=tmp32[:], axis=AX.X, op=ALU.add),
                         reads=[dr], writes=[wallr])
                    dc = di % 4
                    di += 1
                    S.op("dve", lambda v, dall=dall, tt=tt, dc=dc: v.tensor_copy(out=dcol[dc][:], in_=dall[:, tt:tt + 1]),
                         reads=[wallr], writes=[dcr2[dc]])
                    S.idma(lambda g_, dc=dc, tt=tt: g_.indirect_dma_start(
                        out=XS[:, :], out_offset=bass.IndirectOffsetOnAxis(ap=dcol[dc][:, 0:1], axis=0),
                        in_=n2b_all[:, tt, :], in_offset=None, bounds_check=cap_reg, oob_is_err=False), reads=[dcr2[dc], n2r])
            if "ROUTE" in dbg:
                S.dma("sp", DBG[:, 128:144], d1f[:], reads=[wallr])
                S.dma("sp", DBG[:, 144:160], d2f[:], reads=[wallr])
                S.dma("sp", DBG[:, 160:176], w1_all[:], reads=[wallr])
                S.dma("sp", DBG[:, 176:192], w2_all[:], reads=[wallr])
            S.barrier()
        if stop_after == "H":
            return nc
        sNT.close()

        with ExitStack() as si:
            wg = [sb(f"wg{i}", [128, 16, 512], BF16, si) for i in range(2)]
            wu = [sb(f"wu{i}", [128, 16, 512], BF16, si) for i in range(2)]
            wd = [sb(f"wd{i}", [128, 4, 2048], BF16, si) for i in range(2)]
            wgr = [[Res(f"wg{i}_{q}") for q in range(4)] for i in range(2)]
            wur = [[Res(f"wu{i}_{q}") for q in range(4)] for i in range(2)]
            wdr = [[Res(f"wd{i}_{q}") for q in range(4)] for i in range(2)]
            iq = [[sb(f"iq{i}_{q}", [128, 1], I32, si) for q in range(4)] for i in range(NBLK)]
            iqr = [Res(f"iq{i}") for i in range(NBLK)]
            for blk in range(NBLK):
                for q in range(4):
                    S.op("dve", lambda v, q=q, blk=blk: v.tensor_copy(out=iq[blk][q][:], in_=idx4[:, blk * 4 + q:blk * 4 + q + 1]),
                         reads=[idxr], writes=[iqr[blk]])
            xrow = [sb(f"xrow{i}", [128, D], BF16, si) for i in range(4)]
            xrr = [Res(f"xrow{i}") for i in range(4)]
            xT = [sb(f"xT{i}", [128, 16, RB], BF16, si) for i in range(2)]
            xTr = [[Res(f"xT{i}_{r}") for r in range(2)] for i in range(2)]
            sg = [sb(f"sg{i}", [128, RB], F32, si) for i in range(2)]
            sgr = [Res(f"sg{i}") for i in range(2)]
            hT = [sb(f"hT{i}", [128, 4, RB], BF16, si) for i in range(2)]
            hTr = [Res(f"hT{i}") for i in range(2)]
            yt = [sb(f"yt{i}", [128, D], BF16, si) for i in range(2)]
            ytr = [Res(f"yt{i}") for i in range(2)]
            fi = 0
            xi = 0
            yi = 0
            bc_reg = nc.gpsimd.to_reg(NEXP * 128 * 4 - 1)
            xks = {}

            def emit_gathers(blk):
                k = blk % 2
                for (wt_, src, nkc, wrs) in ((wg, weg_v, 4, wgr), (wu, weu_v, 4, wur), (wd, wed_v, 1, wdr)):
                    for q in range(4):
                        if nkc == 4:
                            o = wt_[k][:, 4 * q:4 * q + 4, :].rearrange("p a b -> p (a b)")
                        else:
                            o = wt_[k][:, q, :]
                        S.idma(lambda g_, o=o, src=src, blk=blk, q=q: g_.indirect_dma_start(
                            out=o, out_offset=None, in_=src[:, :], in_offset=bass.IndirectOffsetOnAxis(ap=iq[blk][q][:, 0:1], axis=0),
                            bounds_check=bc_reg, oob_is_err=False),
                            reads=[iqr[blk]], writes=[wrs[k][q]])

            def emit_xload(blk):
                for r in range(RB // 128):
                    xk = (blk * 2 + r) % 4
                    r0 = blk * RB + r * 128
                    S.dma("sp", xrow[xk][:], XS[r0:r0 + 128, :], reads=[XSr], writes=[xrr[xk]])

            def emit_xT(blk):
                k = blk % 2
                for r in range(RB // 128):
                    xk = (blk * 2 + r) % 4
                    for half in range(2):
                        def trx(pe, xk=xk, half=half):
                            for jj in range(8):
                                kc = half * 8 + jj
                                ins = pe.transpose(out=PT[half][:, jj * 128:(jj + 1) * 128], in_=xrow[xk][:, kc * 128:(kc + 1) * 128],
                                                   identity=identb[:])
                            return ins
                        S.op("pe", trx, reads=[xrr[xk], constr], writes=[PTr[half]])
                        if half == 0:
                            S.op("act", lambda a, k=k, r=r: a.activation(out=xT[k][:, 0:8, r * 128:(r + 1) * 128],
                                                                         in_=PT[0][:, :].rearrange("p (a b) -> p a b", b=128), func=AF.Copy),
                                 reads=[PTr[0]], writes=[xTr[k][r]])
                        else:
                            S.op("dve", lambda v, k=k, r=r: v.tensor_copy(out=xT[k][:, 8:16, r * 128:(r + 1) * 128],
                                                                          in_=PT[1][:, :].rearrange("p (a b) -> p a b", b=128)),
                                 reads=[PTr[1]], writes=[xTr[k][r]])

            def emit_gateup(blk):
                nonlocal_fi = fi_box
                k = blk % 2
                for fc in range(4):
                    f2 = nonlocal_fi[0] % 2
                    nonlocal_fi[0] += 1
                    pg, pu = 2 * f2, 2 * f2 + 1

                    def gmm_(pe, k=k, fc=fc, pg=pg):
                        for kc in range(16):
                            ins = pe.matmul(PB[pg][:, 0:RB], lhsT=wg[k][:, kc, fc * 128:(fc + 1) * 128], rhs=xT[k][:, kc, :],
                                            start=(kc == 0), stop=(kc == 15))
                        return ins

                    def umm_(pe, k=k, fc=fc, pu=pu):
                        for kc in range(16):
                            ins = pe.matmul(PB[pu][:, 0:RB], lhsT=wu[k][:, kc, fc * 128:(fc + 1) * 128], rhs=xT[k][:, kc, :],
                                            start=(kc == 0), stop=(kc == 15))
                        return ins
                    S.op("pe", gmm_, reads=wgr[k] + xTr[k], writes=[PBr[pg]])
                    S.op("pe", umm_, reads=wur[k] + xTr[k], writes=[PBr[pu]])
                    S.op("act", lambda a, f2=f2, pg=pg: a.activation(out=sg[f2][:], in_=PB[pg][:, 0:RB], func=AF.Silu),
                         reads=[PBr[pg]], writes=[sgr[f2]])
                    S.op("dve", lambda v, k=k, fc=fc, f2=f2, pu=pu: v.tensor_tensor(out=hT[k][:, fc, :], in0=PB[pu][:, 0:RB], in1=sg[f2][:],
                                                                                   op=ALU.mult), reads=[PBr[pu], sgr[f2]], writes=[hTr[k]])

            def emit_down(blk):
                k = blk % 2
                for r in range(RB // 128):
                    yk = (blk * 2 + r) % 2
                    for nt in range(4):
                        py = 4 + nt % 2

                        def dmm(pe, k=k, nt=nt, py=py, r=r):
                            for fc in range(4):
                                ins = pe.matmul(PB[py][:, :], lhsT=hT[k][:, fc, r * 128:(r + 1) * 128], rhs=wd[k][:, fc, nt * 512:(nt + 1) * 512],
                                                start=(fc == 0), stop=(fc == 3))
                            return ins
                        S.op("pe", dmm, reads=[hTr[k]] + wdr[k], writes=[PBr[py]])
                        if nt % 2 == 0:
                            S.op("act", lambda a, yk=yk, nt=nt, py=py: a.activation(out=yt[yk][:, nt * 512:(nt + 1) * 512], in_=PB[py][:, :],
                                                                                   func=AF.Copy), reads=[PBr[py]], writes=[ytr[yk]])
                        else:
                            S.op("dve", lambda v, yk=yk, nt=nt, py=py: v.tensor_copy(out=yt[yk][:, nt * 512:(nt + 1) * 512], in_=PB[py][:, :]),
                                 reads=[PBr[py]], writes=[ytr[yk]])
                    r0 = blk * RB + r * 128
                    S.dma("sp", YS[r0:r0 + 128, :], yt[yk][:], reads=[ytr[yk]])

            fi_box = [0]
            emit_gathers(0)
            emit_xload(0)
            emit_xT(0)
            emit_gathers(1)
            emit_xload(1)
            for blk in range(NBLK):
                emit_gateup(blk)
                if blk + 1 < NBLK:
                    emit_xT(blk + 1)
                if blk + 2 < NBLK:
                    emit_xload(blk + 2)
                emit_down(blk)
                if blk + 2 < NBLK:
                    emit_gathers(blk + 2)
            S.barrier()

        with ExitStack() as sj:
            nfin_bc = sb("nfin_bc", [128, D], F32, sj)
            nfr = Res("nfin")
            S.dma("sp", nfin_bc[:], dram_bcast(nfin_g[0:1, :], D), writes=[nfr])
            y1 = [sb(f"y1{i}", [128, D], BF16, sj) for i in range(2)]
            y2 = [sb(f"y2{i}", [128, D], BF16, sj) for i in range(2)]
            acc = [sb(f"jacc{i}", [128, D], F32, sj) for i in range(2)]
            accr = [Res(f"jacc{i}") for i in range(2)]
            yr = [Res(f"y{i}") for i in range(2)]
            yr2 = [Res(f"yb{i}") for i in range(2)]
            h1j = [sb(f"h1j{i}", [128, D], F32, sj) for i in range(2)]
            h1jr = [Res(f"h1j{i}") for i in range(2)]
            ot = [sb(f"jo{i}", [128, D], F32, sj) for i in range(2)]
            orr = [Res(f"jo{i}") for i in range(2)]
            junk = sb("jjunk", [128, D], BF16, sj)
            jr = Res("jjunk")
            ssq = [sb(f"jssq{i}", [128, 1], F32, sj) for i in range(2)]
            sr = [Res(f"jssq{i}") for i in range(2)]
            jc = [[sb(f"jc{i}_{z}", [128, 1], I32, sj) for z in range(2)] for i in range(16)]
            jcr = [Res(f"jc{i}") for i in range(16)]
            jcr2 = [Res(f"jcb{i}") for i in range(16)]
            for tt in range(16):
                S.op("dve", lambda v, tt=tt: v.tensor_copy(out=jc[tt][0][:], in_=d1f[:, tt:tt + 1]), reads=[wallr], writes=[jcr[tt]])
                S.op("dve", lambda v, tt=tt: v.tensor_copy(out=jc[tt][1][:], in_=d2f[:, tt:tt + 1]), reads=[wallr], writes=[jcr2[tt]])
            def j_loads(tt):
                k = tt % 2
                S.idma(lambda g_, k=k: g_.indirect_dma_start(out=y1[k][:], out_offset=None, in_=YS[:, :],
                                                            in_offset=bass.IndirectOffsetOnAxis(ap=jc[tt][0][:, 0:1], axis=0),
                                                            bounds_check=cap_reg, oob_is_err=False),
                       reads=[jcr[tt], YSr], writes=[yr[k]])
                S.idma(lambda g_, k=k: g_.indirect_dma_start(out=y2[k][:], out_offset=None, in_=YS[:, :],
                                                            in_offset=bass.IndirectOffsetOnAxis(ap=jc[tt][1][:, 0:1], axis=0),
                                                            bounds_check=cap_reg, oob_is_err=False),
                       reads=[jcr2[tt], YSr], writes=[yr2[k]])
                S.dma("sp", h1j[k][:], H1[tt * 128:(tt + 1) * 128, :], writes=[h1jr[k]])

            j_loads(0)
            for tt in range(16):
                k = tt % 2
                if tt + 1 < 16:
                    j_loads(tt + 1)
                S.op("dve", lambda v, k=k, tt=tt: v.tensor_scalar(out=acc[k][:], in0=y1[k][:], scalar1=w1_all[:, tt:tt + 1], scalar2=None,
                                                                  op0=ALU.mult), reads=[yr[k], wallr], writes=[accr[k]])
                S.op("dve", lambda v, k=k, tt=tt: v.scalar_tensor_tensor(out=acc[k][:], in0=y2[k][:], scalar=w2_all[:, tt:tt + 1], in1=acc[k][:],
                                                                         op0=ALU.mult, op1=ALU.add), reads=[accr[k], yr2[k], wallr], writes=[accr[k]])
                S.op("pool", lambda g_, k=k: g_.tensor_tensor(out=acc[k][:], in0=acc[k][:], in1=gf_bc[:], op=ALU.mult), reads=[accr[k], bcr], writes=[accr[k]])
                S.op("dve", lambda v, k=k: v.tensor_tensor(out=h1j[k][:], in0=h1j[k][:], in1=acc[k][:], op=ALU.add), reads=[accr[k], h1jr[k]], writes=[h1jr[k]])
                S.op("act", lambda a, k=k: a.activation(out=junk[:], in_=h1j[k][:], func=AF.Square, accum_out=ssq[k][:]),
                     reads=[h1jr[k]], writes=[jr, sr[k]])
                S.op("act", lambda a, k=k: a.activation(out=ssq[k][:], in_=ssq[k][:], func=AF.Sqrt, scale=1.0 / D, bias=epsc[:, 0:1]),
                     reads=[sr[k], constr], writes=[sr[k]])
                S.op("dve", lambda v, k=k: v.reciprocal(out=ssq[k][:], in_=ssq[k][:]), reads=[sr[k]], writes=[sr[k]])
                S.op("dve", lambda v, k=k: v.scalar_tensor_tensor(out=ot[k][:], in0=h1j[k][:], scalar=ssq[k][:, 0:1], in1=nfin_bc[:],
                                                                   op0=ALU.mult, op1=ALU.mult), reads=[h1jr[k], sr[k], nfr], writes=[orr[k]])
                S.dma("sp", out[tt * 128:(tt + 1) * 128, :], ot[k][:], reads=[orr[k]])
            S.barrier()
    return nc


def _local_order(j):
    own = [2 * i + j for i in range(8)]
    oth = [2 * i + 1 - j for i in range(8)]
    return own + oth


def _const_pack(j):
    cpk = np.zeros((128, CP_W), np.float32)
    p = np.arange(128)
    cpk[:, CP_IDENT:CP_IDENT + 128] = np.eye(128)
    pm = np.zeros((128, 128), np.float32)
    for pp in range(64):
        pm[pp + 64, pp] = -1.0
        pm[pp, pp + 64] = 1.0
    cpk[:, CP_PM:CP_PM + 128] = pm
    cpk[:, CP_TRI01:CP_TRI01 + 128] = (p[:, None] <= p[None, :])
    cpk[:, CP_TRIPEN:CP_TRIPEN + 128] = np.where(p[:, None] <= p[None, :], 0.0, -BIG)
    cpk[:, CP_LSTRICT:CP_LSTRICT + 128] = (p[:, None] < p[None, :])
    cpk[:, CP_ONES:CP_ONES + 128] = 1.0
    invf = (10000.0 ** (-np.arange(0, 128, 2, dtype=np.float32) / 128)).astype(np.float32)
    cpk[:, CP_INVF] = np.concatenate([invf, invf])
    cpk[:, CP_IOTAP] = p
    cpk[:, CP_SIGN] = np.where(p < 64, -1.0, 1.0)
    order = _local_order(j)
    pp_ = np.zeros((8, 16), np.float32)
    p01 = np.zeros((8, 16), np.float32)
    for i in range(8):
        gq = 2 * i + j
        for kb in range(16):
            past = order[kb] < gq
            pp_[i, kb] = 0.0 if past else -1e30
            p01[i, kb] = 1.0 if past else 0.0
    cpk[:, CP_PASTPEN:CP_PASTPEN + 128] = pp_.reshape(1, 128)
    cpk[:, CP_PAST01:CP_PAST01 + 128] = p01.reshape(1, 128)
    return cpk


def _const_pack2():
    c2 = np.zeros((128, C2_W), np.float32)
    c2[:, C2_THR:C2_THR + 1024] = np.tile(float(RB) * np.arange(32, dtype=np.float32), 32)[None, :]
    c2[:, C2_BLKI:C2_BLKI + 2048] = np.repeat(np.arange(64, dtype=np.float32), 32)[None, :]
    return c2


def _prep_shared(inp):
    f = np.float32
    sh = {}
    sh["w_ada"] = np.ascontiguousarray(inp["w_ada"][0], f)
    sh["b_ada"] = np.ascontiguousarray(inp["b_ada"][0].reshape(1, -1), f)
    sh["gmix"] = np.ascontiguousarray(inp["norm_mix_g"][0].reshape(16, 128).T, f)
    sh["w_in"] = np.ascontiguousarray(inp["w_in"][0], f)
    sh["sgu_ln_g"] = np.ascontiguousarray(inp["sgu_ln_g"][0].reshape(1, -1), f)
    sh["sgu_ln_b"] = np.ascontiguousarray(inp["sgu_ln_b"][0].reshape(1, -1), f)
    sh["wsT"] = np.ascontiguousarray(np.transpose(inp["sgu_w_s"][0], (2, 0, 1)).reshape(128, 1024), f)
    sh["sgu_b_s"] = np.ascontiguousarray(inp["sgu_b_s"][0].reshape(1, -1), f)
    sh["w_sgu_out"] = np.ascontiguousarray(inp["w_sgu_out"][0], f)
    sh["w_moba_out"] = np.ascontiguousarray(inp["w_moba_out"][0], f)
    sh["w_out"] = np.ascontiguousarray(inp["w_out"][0], f)
    sh["norm_ffn_g"] = np.ascontiguousarray(inp["norm_ffn_g"][0].reshape(1, -1), f)
    sh["norm_final_g"] = np.ascontiguousarray(inp["norm_final_g"].reshape(1, -1), f)
    wr = np.concatenate([inp["w_route_group"][0], inp["w_route_expert"][0]], axis=1)
    sh["wr"] = np.ascontiguousarray(wr.reshape(16, 128, 36).transpose(1, 0, 2).reshape(128, 16 * 36), f)
    sh["br"] = np.ascontiguousarray(
        np.concatenate([inp["b_route_group"][0].reshape(-1), inp["b_route_expert"][0].reshape(-1)]).reshape(1, 36), f)
    sh["weg"] = np.ascontiguousarray(
        inp["w_exp_gate"][0].reshape(NEXP, 16, 128, DFF).transpose(0, 2, 1, 3).reshape(NEXP * 128 * 4, 2048), f)
    sh["weu"] = np.ascontiguousarray(
        inp["w_exp_up"][0].reshape(NEXP, 16, 128, DFF).transpose(0, 2, 1, 3).reshape(NEXP * 128 * 4, 2048), f)
    sh["wed"] = np.ascontiguousarray(
        inp["w_exp_down"][0].reshape(NEXP, 4, 128, D).transpose(0, 2, 1, 3).reshape(NEXP * 128 * 4, 2048), f)
    ea = np.zeros((16, 16 * 128), np.float32)
    for kb in range(16):
        ea[kb, kb * 128:(kb + 1) * 128] = 1.0
    sh["eall"] = ea
    sh["cpack2"] = _const_pack2()
    return sh


def _prep_core(inp, sh, c):
    b, j = c // 2, c % 2
    order = _local_order(j)
    xb = np.asarray(inp["x"][b], np.float32).reshape(16, 256, D)
    pb = np.asarray(inp["positions"][b], np.int32).reshape(16, 256)
    m = dict(sh)
    m["x"] = np.ascontiguousarray(xb[order].reshape(SEQ, D))
    m["pos"] = np.ascontiguousarray(pb[order].reshape(1, SEQ))
    m["cvec"] = np.ascontiguousarray(np.asarray(inp["c"][b], np.float32).reshape(16, 128).T)
    m["cpack"] = _const_pack(j)
    return m


_NC_CACHE = {}


def kernel(**inputs):
    inp = {k: np.asarray(v) for k, v in inputs.items()}
    sh = _prep_shared(inp)
    in_maps = [_prep_core(inp, sh, c) for c in range(8)]
    if "nc" not in _NC_CACHE:
        _NC_CACHE["nc"] = build()
    res = run_bass_kernel_spmd(_NC_CACHE["nc"], in_maps, core_ids=list(range(8)))
    outp = np.zeros((4, 16, 256, D), np.float32)
    for c in range(8):
        b, j = c // 2, c % 2
        o = np.asarray(res.results[c]["out"]).reshape(8, 256, D)
        for i in range(8):
            outp[b, 2 * i + j] = o[i]
    return outp.reshape(4, SEQ, D)
```

```python
import numpy as np
from contextlib import ExitStack
import concourse.bass as bass
import concourse.mybir as mybir
from concourse.bass_utils import run_bass_kernel_spmd
from concourse.alu_op_type import AluOpType as ALU

AF = mybir.ActivationFunctionType
AX = mybir.AxisListType
F32 = mybir.dt.float32
BF16 = mybir.dt.bfloat16
I32 = mybir.dt.int32
U32 = mybir.dt.uint32

D = 2048
SEQ = 4096
NOWN = 2048
NH = 16
DH = 128
EPS = 1e-6
BIG = 30000.0
NEXP = 32
DFF = 512
RB = 256
NBLK = 48
CAPROWS = NBLK * RB
PI = float(np.pi)
RELAX = False

CP_IDENT, CP_PM, CP_TRI01, CP_TRIPEN, CP_LSTRICT, CP_ONES = 0, 128, 256, 384, 512, 640
CP_INVF, CP_IOTAP, CP_SIGN = 768, 769, 770
CP_PASTPEN = 771
CP_PAST01 = CP_PASTPEN + 128
CP_W = CP_PAST01 + 128
C2_THR = 0
C2_BLKI = 1024
C2_W = 1024 + 2048


class Res:
    __slots__ = ("name", "w", "rs", "rd")

    def __init__(self, name):
        self.name = name
        self.w = None
        self.rs = {}
        self.rd = []


class Sched:
    ENGS = ("pe", "act", "dve", "pool", "sp")

    def __init__(self, nc, es, ndsem=12):
        self.nc = nc
        self.eng = {"pe": nc.tensor, "act": nc.scalar, "dve": nc.vector, "pool": nc.gpsimd, "sp": nc.sync}
        self.sem = {e: es.enter_context(nc.semaphore("sem_" + e)) for e in self.ENGS}
        self.cnt = {e: 0 for e in self.ENGS}
        self.seen = {e: {f: 0 for f in self.ENGS} for e in self.ENGS}
        self.dq = {}
        for q, nd in (("sp", 16), ("pool", 14)):
            sems = [es.enter_context(nc.semaphore(f"dsem_{q}{i}")) for i in range(nd)]
            self.dq[q] = {"sems": sems, "val": [0] * nd, "nxt": 0}
        self.seen_d = {e: {} for e in self.ENGS}

    def _wait(self, e, t):
        if t is None:
            return
        if t[0] == "dma":
            _, q, k, v = t
            key = (q, k)
            if self.seen_d[e].get(key, 0) >= v:
                return
            self.eng[e].wait_ge(self.dq[q]["sems"][k], v)
            self.seen_d[e][key] = v
        else:
            src, v = t
            if self.seen[e][src] >= v:
                return
            self.eng[e].wait_ge(self.sem[src], v)
            self.seen[e][src] = v

    def _deps(self, e, reads, writes):
        for r in reads:
            self._wait(e, r.w)
        for w in writes:
            if w.w is not None and not (w.w[0] == e and RELAX):
                self._wait(e, w.w)
            for src, v in w.rs.items():
                if src != e or not RELAX:
                    self._wait(e, (src, v))
            for t in w.rd:
                self._wait(e, t)

    def _mark(self, t, reads, writes):
        for r in reads:
            if t[0] == "dma":
                r.rd.append(t)
            else:
                if r.rs.get(t[0], 0) < t[1]:
                    r.rs[t[0]] = t[1]
        for w in writes:
            w.w = t
            w.rs = {}
            w.rd = []

    def op(self, e, fn, reads=(), writes=()):
        self._deps(e, reads, writes)
        inst = fn(self.eng[e])
        self.cnt[e] += 1
        inst.then_inc(self.sem[e], 1)
        t = (e, self.cnt[e])
        self.seen[e][e] = max(self.seen[e][e], 0)
        self._mark(t, reads, writes)
        return t

    def dma(self, q, out, in_, reads=(), writes=(), after=(), **kw):
        self._deps(q, reads, writes)
        for t_ in after:
            self._wait(q, t_)
        d = self.dq[q]
        k = d["nxt"]
        d["nxt"] = (k + 1) % len(d["sems"])
        if d["val"][k]:
            self._wait(q, ("dma", q, k, d["val"][k]))
        inst = self.eng[q].dma_start(out=out, in_=in_, **kw)
        d["val"][k] += 16
        inst.then_inc(d["sems"][k], 16)
        t = ("dma", q, k, d["val"][k])
        self._mark(t, reads, writes)
        return t

    def idma(self, fn, reads=(), writes=()):
        q = "pool"
        self._deps(q, reads, writes)
        d = self.dq[q]
        k = d["nxt"]
        d["nxt"] = (k + 1) % len(d["sems"])
        if d["val"][k]:
            self._wait(q, ("dma", q, k, d["val"][k]))
        inst = fn(self.eng[q])
        d["val"][k] += 16
        inst.then_inc(d["sems"][k], 16)
        t = ("dma", q, k, d["val"][k])
        self._mark(t, reads, writes)
        return t

    def barrier(self, engines=None):
        for e in (engines or self.ENGS):
            for f in self.ENGS:
                if f != e and self.cnt[f]:
                    self._wait(e, (f, self.cnt[f]))
            for q, d in self.dq.items():
                for k, v in enumerate(d["val"]):
                    if v:
                        self._wait(e, ("dma", q, k, v))


def dram_bcast(ap, n):
    return bass.AP(ap.tensor, ap.offset, [[0, 128], [1, n]])


def build(dbg=(), stop_after=None):
    nc = bass.Bass("TRN2", target_bir_lowering=False)
    dbg = set(dbg)

    def din(name, shape, dt=F32):
        return nc.dram_tensor(name, list(shape), dt, kind="ExternalInput").ap()

    def dscr(name, shape, dt):
        if name in dbg:
            return nc.dram_tensor(name, list(shape), dt, kind="ExternalOutput").ap()
        return nc.dram_tensor(name, list(shape), dt).ap()

    x = din("x", [SEQ, D])
    cvec = din("cvec", [128, 16])
    pos = din("pos", [1, SEQ], I32)
    w_ada = din("w_ada", [D, 6 * D])
    b_ada = din("b_ada", [1, 6 * D])
    gmix = din("gmix", [128, 16])
    w_in = din("w_in", [D, 6 * D])
    ln_g = din("sgu_ln_g", [1, 1024])
    ln_b = din("sgu_ln_b", [1, 1024])
    wsT_d = din("wsT", [128, 8 * 128])
    bs_d = din("sgu_b_s", [1, 8 * 128])
    w_sgu_out = din("w_sgu_out", [1024, D])
    w_moba_out = din("w_moba_out", [D, D])
    w_out = din("w_out", [D, D])
    nffn_g = din("norm_ffn_g", [1, D])
    nfin_g = din("norm_final_g", [1, D])
    wr_d = din("wr", [128, 16 * 36])
    br_d = din("br", [1, 36])
    weg = din("weg", [NEXP * 128 * 4, 2048])
    weu = din("weu", [NEXP * 128 * 4, 2048])
    wed = din("wed", [NEXP * 128 * 4, 2048])
    cpack = din("cpack", [128, CP_W])
    eall_d = din("eall", [16, 16 * 128])
    cpack2 = din("cpack2", [128, C2_W])

    out = nc.dram_tensor("out", [NOWN, D], F32, kind="ExternalOutput").ap()

    UT = dscr("UT", [16, 128, 8 * 128], BF16)
    VS = dscr("VS", [NOWN, 1024], BF16)
    QT = dscr("QT", [NH, 128, NOWN], BF16)
    KT = dscr("KT", [NH, 128, SEQ], BF16)
    VA = dscr("VA", [NH, 128, 32 * 128], BF16)
    GA = dscr("GA", [16, 128, NOWN], BF16)
    GB = dscr("GB", [16, 128, NOWN], BF16)
    MG = dscr("MG", [16, 128, NOWN], BF16)
    H1 = dscr("H1", [NOWN, D], F32)
    N2 = dscr("N2", [NOWN, D], BF16)
    XS = dscr("XS", [CAPROWS, D], BF16)
    YS = dscr("YS", [CAPROWS, D], BF16)
    DBG = dscr("DBG", [128, 4096], F32)
    WGB = dscr("WGB", [NEXP * 128 * 4, 2048], BF16)
    WUB = dscr("WUB", [NEXP * 128 * 4, 2048], BF16)
    WDB = dscr("WDB", [NEXP * 128 * 4, 2048], BF16)

    def dscr_out(name, shape, dt):
        return nc.dram_tensor(name, list(shape), dt, kind="ExternalOutput").ap()

    with ExitStack() as es:
        S = Sched(nc, es)

        uniq = [0]

        def sb(name, shape, dt, stack=es):
            uniq[0] += 1
            return stack.enter_context(nc.sbuf_tensor(f"{name}_{uniq[0]}", list(shape), dt))

        PB = [es.enter_context(nc.psum_tensor(f"pb{i}", [128, 512], F32)) for i in range(6)]
        PBr = [Res(f"pb{i}") for i in range(6)]
        PT = [es.enter_context(nc.psum_tensor(f"pt{i}", [128, 1024], BF16)) for i in range(2)]
        PTr = [Res(f"pt{i}") for i in range(2)]

        cp = sb("cp", [128, CP_W], F32)
        cpr = Res("cp")
        S.dma("sp", cp[:], cpack[:, :], writes=[cpr])
        identb = sb("identb", [128, 128], BF16)
        pmb = sb("pmb", [128, 128], BF16)
        tripenb = sb("tripenb", [128, 128], BF16)
        lstrb = sb("lstrb", [128, 128], BF16)
        onesb = sb("onesb", [128, 128], BF16)
        epsc = sb("epsc", [128, 1], F32)
        constr = Res("constb")
        for dst, off in ((identb, CP_IDENT), (pmb, CP_PM), (tripenb, CP_TRIPEN), (lstrb, CP_LSTRICT), (onesb, CP_ONES)):
            S.op("dve", lambda v, dst=dst, off=off: v.tensor_copy(out=dst[:], in_=cp[:, off:off + 128]),
                 reads=[cpr], writes=[constr])
        S.op("dve", lambda v: v.memset(epsc[:], EPS), writes=[constr])
        eallb = sb("eallb", [16, 2048], BF16)
        with ExitStack() as s0:
            eall_f = sb("eall_f", [16, 2048], F32, s0)
            er = Res("eall")
            S.dma("sp", eall_f[:], eall_d[:, :], writes=[er])
            S.op("dve", lambda v: v.tensor_copy(out=eallb[:], in_=eall_f[:]), reads=[er], writes=[constr])
            S.barrier()

        adaT = sb("adaT", [128, 96], F32)
        adaTr = Res("adaT")
        g1 = sb("g1", [128, 16], F32)
        g1r = Res("g1")
        gm_bc = sb("gm_bc", [128, D], F32)
        G2_bc = sb("G2_bc", [128, D], F32)
        shf_bc = sb("shf_bc", [128, D], F32)
        gf_bc = sb("gf_bc", [128, D], F32)
        bcr = Res("bc")

        with ExitStack() as sa:
            c_sb = sb("c_sb", [128, 16], F32, sa)
            c_act = sb("c_act", [128, 16], BF16, sa)
            ada_row = sb("ada_row", [1, 6 * D], F32, sa)
            bada = [sb(f"bada{i}", [1, 512], F32, sa) for i in range(2)]
            badar = [Res(f"bada{i}") for i in range(2)]
            wblk = [sb(f"wa{i}", [128, 16, 512], BF16, sa) for i in range(3)]
            wblr = [Res(f"wa{i}") for i in range(3)]
            one1 = sb("one1", [1, 128], F32, sa)
            cr, ar, br_ = Res("c"), Res("adarow"), Res("bada")
            S.dma("sp", c_sb[:], cvec[:, :], writes=[cr])
            S.op("act", lambda a: a.activation(out=c_act[:], in_=c_sb[:], func=AF.Silu), reads=[cr], writes=[cr])
            S.op("dve", lambda v: v.memset(one1[:], 1.0), writes=[ar])
            wav = w_ada.rearrange("(kc p) n -> p kc n", p=128)
            for nt in range(24):
                k = nt % 3
                S.dma("pool", wblk[k][:], wav[:, :, nt * 512:(nt + 1) * 512], writes=[wblr[k]])
                pbi = nt % 2
                S.dma("sp", bada[pbi][:], b_ada[0:1, nt * 512:(nt + 1) * 512], writes=[badar[pbi]])

                def mm(pe, k=k, pbi=pbi):
                    for kc in range(16):
                        i = pe.matmul(PB[pbi][0:1, :], lhsT=c_act[:, kc:kc + 1], rhs=wblk[k][:, kc, :],
                                      start=(kc == 0), stop=(kc == 15))
                    return i
                S.op("pe", mm, reads=[cr, wblr[k]], writes=[PBr[pbi]])
                S.op("dve", lambda v, nt=nt, pbi=pbi: v.tensor_tensor(
                    out=ada_row[0:1, nt * 512:(nt + 1) * 512], in0=PB[pbi][0:1, :],
                    in1=bada[pbi][0:1, :], op=ALU.add), reads=[PBr[pbi], badar[pbi]], writes=[ar])
            def mmT(pe):
                for j in range(96):
                    i = pe.matmul(PB[2][:, j:j + 1], lhsT=ada_row[0:1, j * 128:(j + 1) * 128], rhs=one1[0:1, 0:1],
                                  start=True, stop=True)
                return i
            S.op("pe", mmT, reads=[ar], writes=[PBr[2]])
            S.op("dve", lambda v: v.tensor_copy(out=adaT[:], in_=PB[2][:, 0:96]), reads=[PBr[2]], writes=[adaTr])
            gmx = sb("gmx", [128, 16], F32, sa)
            gr_ = Res("gmx")
            S.dma("sp", gmx[:], gmix[:, :], writes=[gr_])
            S.op("dve", lambda v: v.scalar_tensor_tensor(out=g1[:], in0=adaT[:, 16:32], scalar=1.0, in1=gmx[:],
                                                         op0=ALU.add, op1=ALU.mult), reads=[adaTr, gr_], writes=[g1r])
            S.dma("sp", G2_bc[:], dram_bcast(nffn_g[0:1, :], D), writes=[bcr])
            for which, dst in ((2, gm_bc), (3, shf_bc), (4, None), (5, gf_bc)):
                for q4 in range(4):
                    c0 = which * D + q4 * 512
                    pbi = 3 + (q4 % 2)
                    S.op("pe", lambda pe, c0=c0, pbi=pbi: pe.matmul(PB[pbi][:, :], lhsT=one1[0:1, :],
                                                                    rhs=ada_row[0:1, c0:c0 + 512], start=True, stop=True),
                         reads=[ar], writes=[PBr[pbi]])
                    if dst is not None:
                        S.op("dve", lambda v, dst=dst, q4=q4, pbi=pbi: v.tensor_copy(
                            out=dst[:, q4 * 512:(q4 + 1) * 512], in_=PB[pbi][:, :]), reads=[PBr[pbi]], writes=[bcr])
                    else:
                        S.op("dve", lambda v, q4=q4, pbi=pbi: v.scalar_tensor_tensor(
                            out=G2_bc[:, q4 * 512:(q4 + 1) * 512], in0=PB[pbi][:, :], scalar=1.0,
                            in1=G2_bc[:, q4 * 512:(q4 + 1) * 512], op0=ALU.add, op1=ALU.mult),
                            reads=[PBr[pbi], bcr], writes=[bcr])
            S.barrier()
        if "adaT" in dbg:
            S.dma("sp", DBG[:, 0:96], adaT[:], reads=[adaTr])
            S.dma("sp", DBG[:, 128:128 + 2048], G2_bc[:], reads=[bcr])
        if stop_after == "A":
            S.barrier()
            return nc

        kmT = sb("kmT", [128, 16, 16], F32)
        kmr = Res("kmT")
        d1f = sb("d1f", [128, 16], F32)
        d2f = sb("d2f", [128, 16], F32)
        w1_all = sb("w1_all", [128, 16], F32)
        w2_all = sb("w2_all", [128, 16], F32)
        idx4 = sb("idx4", [128, 256], I32)
        wallr, idxr, XSr, YSr = Res("wall"), Res("idx4"), Res("XS"), Res("YS")
        sNT = es.enter_context(ExitStack())
        NT = sb("NT", [128, 16, NOWN], BF16, sNT)
        NTr = Res("NT")
        NTr2 = Res("NT2")
        w_inv = w_in.rearrange("(kc p) n -> p kc n", p=128)

        def norm_stage(tok0):
            with ExitStack() as sb_:
                NB_ = 3
                xt = [sb(f"xt{i}", [128, D], F32, sb_) for i in range(NB_)]
                xs = [sb(f"xs{i}", [128, D], BF16, sb_) for i in range(2)]
                junk = sb("junk", [128, D], BF16, sb_)
                ssq = [sb(f"ssq{i}", [128, 1], F32, sb_) for i in range(NB_)]
                xr = [Res(f"xt{i}") for i in range(NB_)]
                xsr = [Res(f"xs{i}") for i in range(2)]
                sr = [Res(f"ssq{i}") for i in range(NB_)]
                jr = Res("junk")
                ntmp = [sb(f"ntmp{i}", [128, 8, 128], F32, sb_) for i in range(2)]
                ntr = [Res(f"ntmp{i}") for i in range(2)]

                def p1(tt):
                    k = tt % NB_
                    S.dma("sp", xt[k][:], x[tok0 + tt * 128: tok0 + (tt + 1) * 128, :], writes=[xr[k]])
                    S.op("act", lambda a, k=k: a.activation(out=junk[:], in_=xt[k][:], func=AF.Square, accum_out=ssq[k][:]),
                         reads=[xr[k]], writes=[jr, sr[k]])

                def p2(tt):
                    k = tt % NB_
                    k2 = tt % 2
                    S.op("act", lambda a, k=k: a.activation(out=ssq[k][:], in_=ssq[k][:], func=AF.Sqrt, scale=1.0 / D, bias=epsc[:, 0:1]),
                         reads=[sr[k], constr], writes=[sr[k]])
                    S.op("dve", lambda v, k=k: v.reciprocal(out=ssq[k][:], in_=ssq[k][:]), reads=[sr[k]], writes=[sr[k]])
                    S.op("act", lambda a, k=k, k2=k2: a.activation(out=xs[k2][:], in_=xt[k][:], func=AF.Copy, scale=ssq[k][:, 0:1]),
                         reads=[xr[k], sr[k]], writes=[xsr[k2]])

                def p3(tt):
                    k2 = tt % 2
                    for half in range(2):
                        def tr(pe, k2=k2, half=half):
                            for jj in range(8):
                                kc = half * 8 + jj
                                i = pe.transpose(out=PT[half][:, jj * 128:(jj + 1) * 128], in_=xs[k2][:, kc * 128:(kc + 1) * 128],
                                                 identity=identb[:])
                            return i
                        S.op("pe", tr, reads=[xsr[k2], constr], writes=[PTr[half]])
                        tb = (2 * tt + half) % 2
                        S.op("dve", lambda v, half=half, tb=tb: v.tensor_tensor(
                            out=ntmp[tb][:], in0=PT[half][:, :].rearrange("p (a b) -> p a b", b=128),
                            in1=g1[:, half * 8:(half + 1) * 8].unsqueeze(2).broadcast_to([128, 8, 128]), op=ALU.mult),
                            reads=[PTr[half], g1r], writes=[ntr[tb]])
                        S.op("pool", lambda g_, half=half, tb=tb, tt=tt: g_.tensor_tensor(
                            out=NT[:, half * 8:(half + 1) * 8, tt * 128:(tt + 1) * 128], in0=ntmp[tb][:],
                            in1=adaT[:, half * 8:(half + 1) * 8].unsqueeze(2).broadcast_to([128, 8, 128]), op=ALU.add),
                            reads=[ntr[tb], adaTr], writes=[NTr])

                p1(0)
                p1(1)
                p2(0)
                for tt in range(16):
                    if tt + 2 < 16:
                        p1(tt + 2)
                    if tt + 1 < 16:
                        p2(tt + 1)
                    p3(tt)
                S.barrier()

        def proj_stage(tok0, blocks):
            ntok = NOWN
            with ExitStack() as sc:
                wb = [sb(f"wb{i}", [128, 16, 512], BF16, sc) for i in range(3)]
                wbr = [Res(f"wb{i}") for i in range(3)]
                cosT = sb("cosT", [128, NOWN], F32, sc)
                sinT = sb("sinT", [128, NOWN], F32, sc)
                tabr = Res("tab")
                import os
                KN = os.environ.get("KNOB", "")
                need_tab = any(kd in ("q", "k") for kd, _ in blocks) or "forcetab" in KN
                with ExitStack() as st:
                    pi_ = sb("pos_i", [128, 512], I32, st)
                    ang = sb("ang", [128, 512], F32, st)
                    kf = sb("kf", [128, 512], F32, st)
                    ki = sb("ki", [128, 512], I32, st)
                    msk = sb("msk", [128, 512], F32, st)
                    r1, r2, r3, r4, r5 = Res("pos_i"), Res("ang"), Res("kf"), Res("ki"), Res("msk")
                    for tg in (range(4) if need_tab else ()):
                        t0 = tok0 + tg * 512
                        S.dma("sp", pi_[:], dram_bcast(pos[0:1, t0:t0 + 512], 512), writes=[r1])
                        for which, dst in ((0, sinT), (1, cosT)):
                            S.op("dve", lambda v: v.tensor_copy(out=ang[:], in_=pi_[:]), reads=[r1], writes=[r2])
                            S.op("dve", lambda v, which=which: v.tensor_scalar(
                                out=ang[:], in0=ang[:], scalar1=cp[:, CP_INVF:CP_INVF + 1], scalar2=which * PI / 2,
                                op0=ALU.mult, op1=ALU.add), reads=[r2, cpr], writes=[r2])
                            S.op("dve", lambda v: v.tensor_scalar(out=kf[:], in0=ang[:], scalar1=1.0 / (2 * PI), scalar2=None,
                                                                  op0=ALU.mult), reads=[r2], writes=[r3])
                            S.op("dve", lambda v: v.tensor_copy(out=ki[:], in_=kf[:]), reads=[r3], writes=[r4])
                            S.op("dve", lambda v: v.tensor_copy(out=kf[:], in_=ki[:]), reads=[r4], writes=[r3])
                            S.op("dve", lambda v: v.scalar_tensor_tensor(out=ang[:], in0=kf[:], scalar=-2 * PI, in1=ang[:],
                                                                         op0=ALU.mult, op1=ALU.add), reads=[r3, r2], writes=[r2])
                            S.op("dve", lambda v: v.tensor_scalar(out=msk[:], in0=ang[:], scalar1=PI, scalar2=-2 * PI,
                                                                  op0=ALU.is_gt, op1=ALU.mult), reads=[r2], writes=[r5])
                            S.op("dve", lambda v: v.tensor_tensor(out=ang[:], in0=ang[:], in1=msk[:], op=ALU.add),
                                 reads=[r2, r5], writes=[r2])
                            S.op("dve", lambda v: v.tensor_scalar(out=msk[:], in0=ang[:], scalar1=-PI, scalar2=2 * PI,
                                                                  op0=ALU.is_lt, op1=ALU.mult), reads=[r2], writes=[r5])
                            S.op("dve", lambda v: v.tensor_tensor(out=ang[:], in0=ang[:], in1=msk[:], op=ALU.add),
                                 reads=[r2, r5], writes=[r2])
                            S.op("dve", lambda v: v.tensor_scalar(out=ang[:], in0=ang[:], scalar1=-3.1415925, scalar2=3.1415925,
                                                                  op0=ALU.max, op1=ALU.min), reads=[r2], writes=[r2])
                            if which == 0:
                                S.op("act", lambda a, dst=dst, tg=tg: a.activation(out=dst[:, tg * 512:(tg + 1) * 512], in_=ang[:],
                                                                                 func=AF.Sin, scale=cp[:, CP_SIGN:CP_SIGN + 1]),
                                     reads=[r2, cpr], writes=[tabr])
                            else:
                                S.op("act", lambda a, dst=dst, tg=tg: a.activation(out=dst[:, tg * 512:(tg + 1) * 512], in_=ang[:],
                                                                                 func=AF.Sin), reads=[r2], writes=[tabr])
                    S.barrier()
                ev = [sb(f"ev{i}", [128, 512], BF16, sc) for i in range(3)]
                evr = [Res(f"ev{i}") for i in range(3)]
                qb = [sb(f"qb{i}", [128, 512], BF16, sc) for i in range(2)]
                qbr = [Res(f"qb{i}") for i in range(2)]
                t1 = [sb(f"t1{i}", [128, 512], F32, sc) for i in range(2)]
                t1r = [Res(f"t1{i}") for i in range(2)]
                t2 = [sb(f"t2{i}", [128, 512], F32, sc) for i in range(2)]
                t2r = [Res(f"t2{i}") for i in range(2)]
                evi = [0]
                rpi = [0]
                pbi = [0]
                def issue_w(bj):
                    S.dma("pool", wb[bj % 3][:], w_inv[:, :, blocks[bj][1]:blocks[bj][1] + 512], writes=[wbr[bj % 3]])

                issue_w(0)
                for bi, (kind, col0) in enumerate(blocks):
                    k = bi % 3
                    if bi + 1 < len(blocks):
                        issue_w(bi + 1)
                    if kind in ("vs", "va"):
                        for tt in range(16):
                            pb = pbi[0] % 6
                            pbi[0] += 1

                            def mm(pe, k=k, tt=tt, pb=pb):
                                for kc in range(16):
                                    i = pe.matmul(PB[pb][:, :], lhsT=NT[:, kc, tt * 128:(tt + 1) * 128], rhs=wb[k][:, kc, :],
                                                  start=(kc == 0), stop=(kc == 15))
                                return i
                            S.op("pe", mm, reads=[NTr, NTr2, wbr[k]], writes=[PBr[pb]])
                            e = evi[0] % 3
                            evi[0] += 1
                            if kind == "vs":
                                S.op("act", lambda a, e=e, pb=pb: a.activation(out=ev[e][:], in_=PB[pb][:, :], func=AF.Gelu_apprx_tanh),
                                     reads=[PBr[pb]], writes=[evr[e]])
                                c0 = col0 - 1024
                                S.dma("sp", VS[tt * 128:(tt + 1) * 128, c0:c0 + 512], ev[e][:], reads=[evr[e]])
                            else:
                                S.op("act", lambda a, e=e, pb=pb: a.activation(out=ev[e][:], in_=PB[pb][:, :], func=AF.Copy),
                                     reads=[PBr[pb]], writes=[evr[e]])
                                h0 = (col0 - 6144) // 128
                                tile_g = tok0 // 128 + tt
                                dst = VA.rearrange("h p (t d) -> p h t d", d=128)[:, h0:h0 + 4, tile_g, :]
                                S.dma("sp", dst, ev[e][:].rearrange("p (h d) -> p h d", d=128), reads=[evr[e]])
                        continue
                    for tg in range(4):
                        for nch in range(4):
                            pb = pbi[0] % 6
                            pbi[0] += 1

                            def mm(pe, k=k, tg=tg, nch=nch, pb=pb):
                                for kc in range(16):
                                    i = pe.matmul(PB[pb][:, :], lhsT=wb[k][:, kc, nch * 128:(nch + 1) * 128],
                                                  rhs=NT[:, kc, tg * 512:(tg + 1) * 512], start=(kc == 0), stop=(kc == 15))
                                return i
                            S.op("pe", mm, reads=[NTr, NTr2, wbr[k]], writes=[PBr[pb]])
                            e = evi[0] % 3
                            evi[0] += 1
                            tsl = slice(tg * 512, (tg + 1) * 512)
                            if kind == "u":
                                ch = col0 // 128 + nch
                                S.op("act", lambda a, e=e, pb=pb: a.activation(out=ev[e][:], in_=PB[pb][:, :], func=AF.Gelu_apprx_tanh),
                                     reads=[PBr[pb]], writes=[evr[e]])
                                dst = UT.rearrange("t p (c k) -> p t c k", k=128)[:, tg * 4:(tg + 1) * 4, ch, :]
                                S.dma("sp", dst, ev[e][:].rearrange("p (t k) -> p t k", k=128), reads=[evr[e]])
                            elif kind in ("ga", "gb"):
                                ch = (col0 - (8192 if kind == "ga" else 10240)) // 128 + nch
                                S.op("act", lambda a, e=e, pb=pb: a.activation(out=ev[e][:], in_=PB[pb][:, :], func=AF.Sigmoid),
                                     reads=[PBr[pb]], writes=[evr[e]])
                                S.dma("sp", (GA if kind == "ga" else GB)[ch, :, tsl], ev[e][:], reads=[evr[e]])
                            else:
                                h = (col0 - (2048 if kind == "q" else 4096)) // 128 + nch
                                r = rpi[0] % 2
                                rpi[0] += 1
                                S.op("dve", lambda v, r=r, pb=pb, tsl=tsl: v.tensor_tensor(out=t1[r][:], in0=PB[pb][:, :], in1=cosT[:, tsl],
                                                                                          op=ALU.mult), reads=[PBr[pb], tabr], writes=[t1r[r]])
                                S.op("dve", lambda v, r=r, pb=pb, tsl=tsl: v.tensor_tensor(out=t2[r][0:64, :], in0=PB[pb][64:128, :],
                                                                                          in1=sinT[0:64, tsl], op=ALU.mult),
                                     reads=[PBr[pb], tabr], writes=[t2r[r]])
                                S.op("dve", lambda v, r=r, pb=pb, tsl=tsl: v.tensor_tensor(out=t2[r][64:128, :], in0=PB[pb][0:64, :],
                                                                                          in1=sinT[64:128, tsl], op=ALU.mult),
                                     reads=[PBr[pb], tabr], writes=[t2r[r]])
                                S.op("pool", lambda g, r=r, e=e: g.tensor_tensor(out=ev[e][:], in0=t1[r][:], in1=t2[r][:], op=ALU.add),
                                     reads=[t1r[r], t2r[r]], writes=[evr[e]])
                                if kind == "q":
                                    S.dma("sp", QT[h, :, tsl], ev[e][:], reads=[evr[e]])
                                else:
                                    S.dma("sp", KT[h, :, tok0 + tg * 512: tok0 + (tg + 1) * 512], ev[e][:], reads=[evr[e]])
                                    kb0 = (tok0 + tg * 512) // 256
                                    S.op("dve", lambda v, e=e, h=h, kb0=kb0: v.tensor_reduce(
                                        out=kmT[:, h, kb0:kb0 + 2], in_=ev[e][:].rearrange("p (b k) -> p b k", k=256),
                                        axis=AX.X, op=ALU.add), reads=[evr[e]], writes=[kmr])
                S.barrier()

        own_blocks = ([("u", 0), ("u", 512), ("vs", 1024), ("vs", 1536)]
                      + [("q", 2048 + 512 * i) for i in range(4)] + [("k", 4096 + 512 * i) for i in range(4)]
                      + [("va", 6144 + 512 * i) for i in range(4)] + [("ga", 8192 + 512 * i) for i in range(4)]
                      + [("gb", 10240 + 512 * i) for i in range(4)])
        oth_blocks = [("k", 4096 + 512 * i) for i in range(4)] + [("va", 6144 + 512 * i) for i in range(4)]
        if stop_after and stop_after.startswith("C0"):
            allb = dict(u=("u", 0), vs=("vs", 1024), q=("q", 2048), k=("k", 4096), va=("va", 6144), ga=("ga", 8192))
            sel = stop_after.split(":")[1].split(",") if ":" in stop_after else list(allb)
            own_blocks = [allb[z] for z in sel]
            stop_after = "C0"
        norm_stage(0)
        if "NT" in dbg:
            NTd = dscr_out("NTd", [128, 16 * NOWN], BF16)
            S.dma("sp", NTd[:, :], NT[:].rearrange("p a b -> p (a b)"), reads=[NTr, NTr2])
        if stop_after == "B":
            S.barrier()
            return nc
        proj_stage(0, own_blocks)
        if stop_after in ("C0", "C1"):
            if "kmT" in dbg:
                S.dma("sp", DBG[:, 0:256], kmT[:].rearrange("p a b -> p (a b)"), reads=[kmr])
            S.barrier()
            return nc
        norm_stage(NOWN)
        proj_stage(NOWN, oth_blocks)
        if "kmT" in dbg:
            S.dma("sp", DBG[:, 0:256], kmT[:].rearrange("p a b -> p (a b)"), reads=[kmr])

        ST = sb("ST", [128, 8, NOWN], BF16, sNT)
        STr = Res("ST")
        with ExitStack() as sd:
            lng = sb("lng", [128, 1024], F32, sd)
            lnb = sb("lnb", [128, 1024], F32, sd)
            wsf = sb("wsf", [128, 1024], F32, sd)
            wsb = sb("wsb", [128, 8, 128], BF16, sd)
            bsf = sb("bsf", [1, 1024], F32, sd)
            bsh = sb("bsh", [1, 1024], BF16, sd)
            bsl = sb("bsl", [1, 1024], BF16, sd)
            bst = sb("bst", [1, 1024], F32, sd)
            dcr = Res("dconst")
            S.dma("sp", lng[:], dram_bcast(ln_g[0:1, :], 1024), writes=[dcr])
            S.dma("sp", lnb[:], dram_bcast(ln_b[0:1, :], 1024), writes=[dcr])
            S.dma("sp", wsf[:], wsT_d[:, :], writes=[dcr])
            S.dma("sp", bsf[:], bs_d[:, :], writes=[dcr])
            for g in range(8):
                S.op("dve", lambda v, g=g: v.tensor_tensor(out=wsb[:, g, :], in0=wsf[:, g * 128:(g + 1) * 128],
                                                           in1=cp[:, CP_TRI01:CP_TRI01 + 128], op=ALU.mult),
                     reads=[dcr, cpr], writes=[dcr])
            S.op("dve", lambda v: v.tensor_copy(out=bsh[:], in_=bsf[:]), reads=[dcr], writes=[dcr])
            S.op("dve", lambda v: v.tensor_tensor(out=bst[:], in0=bsf[:], in1=bsh[:], op=ALU.subtract), reads=[dcr], writes=[dcr])
            S.op("dve", lambda v: v.tensor_copy(out=bsl[:], in_=bst[:]), reads=[dcr], writes=[dcr])
            vt = [sb(f"vt{i}", [128, 1024], BF16, sd) for i in range(2)]
            ut = [sb(f"ut{i}", [128, 8, 128], BF16, sd) for i in range(2)]
            vtr = [Res(f"vt{i}") for i in range(2)]
            utr = [Res(f"ut{i}") for i in range(2)]
            st6 = sb("st6", [128, 12], F32, sd)
            mv = sb("mv", [128, 2], F32, sd)
            rs_ = sb("rs_", [128, 1], F32, sd)
            vn = sb("vn", [128, 1024], F32, sd)
            vln = [sb(f"vln{i}", [128, 1024], BF16, sd) for i in range(2)]
            smr, vnr = Res("dsmall"), Res("vn")
            vlr = [Res(f"vln{i}") for i in range(2)]
            for tt in range(16):
                k = tt % 2
                S.dma("sp", vt[k][:], VS[tt * 128:(tt + 1) * 128, :], writes=[vtr[k]])
                S.dma("sp", ut[k][:].rearrange("p c k -> p (c k)"), UT[tt, :, :], writes=[utr[k]])
                S.op("dve", lambda v, k=k: v.bn_stats(out=st6[:, 0:6], in_=vt[k][:, 0:512]), reads=[vtr[k]], writes=[smr])
                S.op("dve", lambda v, k=k: v.bn_stats(out=st6[:, 6:12], in_=vt[k][:, 512:1024]), reads=[vtr[k]], writes=[smr])
                S.op("dve", lambda v: v.bn_aggr(out=mv[:], in_=st6[:]), reads=[smr], writes=[smr])
                S.op("dve", lambda v: v.tensor_scalar(out=rs_[:], in0=mv[:, 1:2], scalar1=EPS, scalar2=None, op0=ALU.add),
                     reads=[smr], writes=[smr])
                S.op("act", lambda a: a.activation(out=rs_[:], in_=rs_[:], func=AF.Sqrt), reads=[smr], writes=[smr])
                S.op("dve", lambda v: v.reciprocal(out=rs_[:], in_=rs_[:]), reads=[smr], writes=[smr])
                S.op("dve", lambda v, k=k: v.tensor_scalar(out=vn[:], in0=vt[k][:], scalar1=mv[:, 0:1], scalar2=rs_[:, 0:1],
                                                            op0=ALU.subtract, op1=ALU.mult), reads=[vtr[k], smr], writes=[vnr])
                S.op("dve", lambda v: v.tensor_tensor(out=vn[:], in0=vn[:], in1=lng[:], op=ALU.mult), reads=[vnr, dcr], writes=[vnr])
                S.op("pool", lambda g_, k=k: g_.tensor_tensor(out=vln[k][:], in0=vn[:], in1=lnb[:], op=ALU.add),
                     reads=[vnr, dcr], writes=[vlr[k]])
                for half in range(2):
                    pb = half

                    def mm(pe, k=k, half=half, pb=pb):
                        for gg in range(4):
                            g = half * 4 + gg
                            o = PB[pb][:, gg * 128:(gg + 1) * 128]
                            pe.matmul(o, lhsT=vln[k][:, g * 128:(g + 1) * 128], rhs=wsb[:, g, :], start=True, stop=False)
                            pe.matmul(o, lhsT=onesb[0:1, :], rhs=bsh[0:1, g * 128:(g + 1) * 128], start=False, stop=False)
                            i = pe.matmul(o, lhsT=onesb[0:1, :], rhs=bsl[0:1, g * 128:(g + 1) * 128], start=False, stop=True)
                        return i
                    S.op("pe", mm, reads=[vlr[k], dcr, constr], writes=[PBr[pb]])
                    S.op("dve", lambda v, k=k, half=half, pb=pb, tt=tt: v.tensor_tensor(
                        out=ST[:, half * 4:half * 4 + 4, tt * 128:(tt + 1) * 128],
                        in0=PB[pb][:, :].rearrange("p (g i) -> p g i", i=128), in1=ut[k][:, half * 4:half * 4 + 4, :], op=ALU.mult),
                        reads=[PBr[pb], utr[k]], writes=[STr])
            S.barrier()
        if "ST" in dbg:
            STd = dscr_out("STd", [128, 8 * NOWN], BF16)
            S.dma("sp", STd[:, :], ST[:].rearrange("p a b -> p (a b)"), reads=[STr])
        if stop_after == "D":
            S.barrier()
            return nc

        AT, ATr = NT, Res("AT")
        SCALE = float(DH) ** -0.5
        with ExitStack() as se:
            kt = [sb(f"kt{i}", [128, SEQ], BF16, se) for i in range(2)]
            va = [sb(f"va{i}", [128, 32, 132], BF16, se) for i in range(2)]
            qt = [sb(f"qt{i}", [128, NOWN], BF16, se) for i in range(2)]
            hr = [Res(f"hd{i}") for i in range(2)]
            hkr = [Res(f"hk{i}") for i in range(2)]
            hqr = [Res(f"hq{i}") for i in range(2)]
            kmb = [sb(f"kmb{i}", [128, 16], BF16, se) for i in range(2)]
            kmbr = [Res(f"kmb{i}") for i in range(2)]
            for i in range(2):
                S.op("dve", lambda v, i=i: v.memset(va[i][:, :, 128:129], 1.0), writes=[hr[i]])
            gm = sb("gm", [128, 2, 16], F32, se)
            m8 = sb("m8", [128, 2, 8], F32, se)
            selt = sb("selt", [128, 2, 16], F32, se)
            Mq = sb("Mq", [128, 2, 16], BF16, se)
            MT = [sb(f"MT{i}", [16, 256], BF16, se) for i in range(2)]
            gsr = Res("gsmall")
            Mqr = Res("Mq")
            MTr = [Res(f"MT{i}") for i in range(2)]
            pt_ = [sb(f"pexp{i}", [128, 256], BF16, se) for i in range(4)]
            ptr = [Res(f"pexp{i}") for i in range(4)]
            rden = sb("rden", [128, 2], F32, se)
            atn = [sb(f"atn{i}", [128, 128], BF16, se) for i in range(2)]
            atr = [Res(f"atn{i}") for i in range(2)]
            rdr = Res("rden")
            sidx = [0]
            pidx = [0]
            units = [(h, i) for h in range(NH) for i in range(8)]

            def load_head(h):
                hb = h % 2
                S.dma("sp", qt[hb][:], QT[h, :, :], writes=[hqr[hb]])
                S.dma("sp", kt[hb][:], KT[h, :, :], writes=[hkr[hb]])
                S.dma("sp", va[hb][:, :, 0:128], VA[h, :, :].rearrange("p (t d) -> p t d", d=128), writes=[hr[hb]])
                S.op("dve", lambda v, hb=hb, h=h: v.tensor_copy(out=kmb[hb][:], in_=kmT[:, h, :]), reads=[kmr], writes=[kmbr[hb]])

            def prep_a(u):
                h, i = units[u]
                hb = h % 2

                def gmm(pe):
                    for q2 in range(2):
                        q0 = i * 256 + q2 * 128
                        ins = pe.matmul(PB[5][:, q2 * 16:(q2 + 1) * 16], lhsT=qt[hb][:, q0:q0 + 128], rhs=kmb[hb][:, :],
                                        start=True, stop=True)
                    return ins
                S.op("pe", gmm, reads=[hqr[hb], kmbr[hb]], writes=[PBr[5]])
                for q2 in range(2):
                    S.op("dve", lambda v, q2=q2: v.tensor_tensor(
                        out=gm[:, q2, :], in0=PB[5][:, q2 * 16:(q2 + 1) * 16],
                        in1=cp[:, CP_PASTPEN + i * 16:CP_PASTPEN + (i + 1) * 16], op=ALU.add), reads=[PBr[5], cpr], writes=[gsr])
                    S.op("dve", lambda v, q2=q2: v.max(out=m8[:, q2, :], in_=gm[:, q2, :]), reads=[gsr], writes=[gsr])
                    S.op("dve", lambda v, q2=q2: v.tensor_scalar(out=selt[:, q2, :], in0=gm[:, q2, :], scalar1=m8[:, q2, 2:3],
                                                                  scalar2=None, op0=ALU.is_ge), reads=[gsr], writes=[gsr])
                    S.op("dve", lambda v, q2=q2: v.tensor_tensor(
                        out=selt[:, q2, :], in0=selt[:, q2, :], in1=cp[:, CP_PAST01 + i * 16:CP_PAST01 + (i + 1) * 16],
                        op=ALU.mult), reads=[gsr, cpr], writes=[gsr])
                    S.op("dve", lambda v, q2=q2: v.tensor_scalar(out=Mq[:, q2, :], in0=selt[:, q2, :], scalar1=-1.0, scalar2=BIG,
                                                                  op0=ALU.add, op1=ALU.mult), reads=[gsr], writes=[Mqr])

            def prep_b(u):
                mb = u % 2

                def mtr(pe):
                    for q2 in range(2):
                        ins = pe.transpose(out=PT[0][0:16, q2 * 128:(q2 + 1) * 128], in_=Mq[:, q2, :], identity=identb[:])
                    return ins
                S.op("pe", mtr, reads=[Mqr, constr], writes=[PTr[0]])
                S.op("act", lambda a: a.activation(out=MT[mb][:], in_=PT[0][0:16, 0:256], func=AF.Copy),
                     reads=[PTr[0]], writes=[MTr[mb]])

            load_head(0)
            prep_a(0)
            prep_b(0)
            zt_ = sb("zfill", [128, 8192], BF16, se)
            zr_ = Res("zfill")
            S.op("pool", lambda g_: g_.memset(zt_[:], 0.0), writes=[zr_])
            XSz = XS.rearrange("(c p r) d -> c p (r d)", p=128, r=4)
            bg = []
            for (dst_, src_) in ((WGB, weg), (WUB, weu), (WDB, wed)):
                for c_ in range(32):
                    bg.append((dst_[c_ * 512:(c_ + 1) * 512, :], src_[c_ * 512:(c_ + 1) * 512, :], []))
            for c_ in range(CAPROWS // 512):
                bg.append((XSz[c_, :, :], zt_[:], [zr_]))
            LAG = 2
            for u, (h, i) in enumerate(units):
                hb = h % 2
                mb = u % 2
                if i == 0 and h + 1 < NH:
                    load_head(h + 1)
                if u + 1 < len(units):
                    prep_a(u + 1)
                tiles = []
                for kbl in list(range(0, i)) + list(range(8, 8 + i + 1)):
                    for c in range(2):
                        tiles.append(("past", kbl, c))
                tiles += [("d00", i, 0), ("d01", i, 0), ("d11", i, 1)]
                nt0 = sum(1 for t_ in tiles if t_[0] in ("past", "d00"))
                nt1 = sum(1 for t_ in tiles if t_[0] in ("past", "d01", "d11"))
                c0 = [0]
                c1 = [0]
                pend = []

                def emit_pv(pk, ktile, qlist):
                    def pvm(pe):
                        for (qq_, off) in qlist:
                            cnt_, tot = (c0, nt0) if qq_ == 0 else (c1, nt1)
                            ins = pe.matmul(PB[3 + qq_][:, 0:129], lhsT=pt_[pk][:, off:off + 128], rhs=va[hb][:, ktile, 0:129],
                                            start=(cnt_[0] == 0), stop=(cnt_[0] == tot - 1))
                            cnt_[0] += 1
                        return ins
                    S.op("pe", pvm, reads=[ptr[pk], hr[hb]], writes=[PBr[3], PBr[4]])

                prepb_at = min(len(tiles) - 1, max(2, len(tiles) // 2))
                for ti, (typ, kbl, c) in enumerate(tiles):
                    sbk = sidx[0] % 3
                    sidx[0] += 1
                    pk = pidx[0] % 4
                    pidx[0] += 1
                    k0 = kbl * 256 + c * 128
                    ktile = kbl * 2 + c
                    if typ == "past":
                        def smm(pe, k0=k0, kbl=kbl, sbk=sbk):
                            pe.matmul(PB[sbk][:, 0:256], lhsT=kt[hb][:, k0:k0 + 128], rhs=qt[hb][:, i * 256:(i + 1) * 256],
                                      start=True, stop=False)
                            return pe.matmul(PB[sbk][:, 0:256], lhsT=eallb[0:16, kbl * 128:(kbl + 1) * 128], rhs=MT[mb][0:16, :],
                                             start=False, stop=True)
                        S.op("pe", smm, reads=[hkr[hb], hqr[hb], MTr[mb], constr], writes=[PBr[sbk]])
                        S.op("act", lambda a, sbk=sbk, pk=pk: a.activation(out=pt_[pk][:, :], in_=PB[sbk][:, 0:256], func=AF.Exp,
                                                                          scale=SCALE), reads=[PBr[sbk]], writes=[ptr[pk]])
                        qlist = ((0, 0), (1, 128))
                    else:
                        qq = 0 if typ == "d00" else 1
                        q0 = i * 256 + qq * 128
                        tri = typ in ("d00", "d11")

                        def smm(pe, k0=k0, q0=q0, sbk=sbk, tri=tri):
                            ins = pe.matmul(PB[sbk][:, 0:128], lhsT=kt[hb][:, k0:k0 + 128], rhs=qt[hb][:, q0:q0 + 128],
                                            start=True, stop=not tri)
                            if tri:
                                ins = pe.matmul(PB[sbk][:, 0:128], lhsT=identb[:], rhs=tripenb[:], start=False, stop=True)
                            return ins
                        S.op("pe", smm, reads=[hkr[hb], hqr[hb], constr], writes=[PBr[sbk]])
                        S.op("act", lambda a, sbk=sbk, pk=pk: a.activation(out=pt_[pk][:, 0:128], in_=PB[sbk][:, 0:128], func=AF.Exp,
                                                                          scale=SCALE), reads=[PBr[sbk]], writes=[ptr[pk]])
                        qlist = ((qq, 0),)
                    pend.append((pk, ktile, qlist))
                    if len(pend) > LAG:
                        emit_pv(*pend.pop(0))
                    if ti == prepb_at and u + 1 < len(units):
                        prep_b(u + 1)
                while pend:
                    emit_pv(*pend.pop(0))
                for q2 in range(2):
                    S.op("dve", lambda v, q2=q2: v.reciprocal(out=rden[:, q2:q2 + 1], in_=PB[3 + q2][:, 128:129]),
                         reads=[PBr[3 + q2]], writes=[rdr])
                    S.op("dve", lambda v, q2=q2: v.tensor_scalar(out=atn[q2][:], in0=PB[3 + q2][:, 0:128], scalar1=rden[:, q2:q2 + 1],
                                                                  scalar2=None, op0=ALU.mult), reads=[PBr[3 + q2], rdr], writes=[atr[q2]])
                    S.op("pe", lambda pe, q2=q2: pe.transpose(out=PT[1][:, q2 * 128:(q2 + 1) * 128], in_=atn[q2][:], identity=identb[:]),
                         reads=[atr[q2], constr], writes=[PTr[1]])
                S.op("act", lambda a, h=h, i=i: a.activation(out=AT[:, h, i * 256:(i + 1) * 256], in_=PT[1][:, 0:256], func=AF.Copy),
                     reads=[PTr[1]], writes=[ATr])
                if bg:
                    o_, i_, rd_ = bg.pop(0)
                    S.dma("pool", o_, i_, reads=rd_, after=[ATr.w])
            while bg:
                o_, i_, rd_ = bg.pop(0)
                S.dma("pool", o_, i_, reads=rd_)
            S.barrier()
        if "AT" in dbg:
            ATd = dscr_out("ATd", [128, 16 * NOWN], BF16)
            S.dma("sp", ATd[:, :], AT[:].rearrange("p a b -> p (a b)"), reads=[ATr])
        if stop_after == "E":
            S.barrier()
            return nc
        wso_v = w_sgu_out.rearrange("(kc p) n -> p kc n", p=128)
        wmo_v = w_moba_out.rearrange("(kc p) n -> p kc n", p=128)
        wo_v = w_out.rearrange("(kc p) n -> p kc n", p=128)
        with ExitStack() as sf:
            wso = [sb(f"wso{i}", [128, 8, 512], BF16, sf) for i in range(2)]
            wmo = [sb(f"wmo{i}", [128, 16, 512], BF16, sf) for i in range(2)]
            wfr = [Res(f"wf{i}") for i in range(2)]
            wfr2 = [Res(f"wfb{i}") for i in range(2)]
            gat = [sb(f"gat{i}", [128, 512], BF16, sf) for i in range(2)]
            gbt = [sb(f"gbt{i}", [128, 512], BF16, sf) for i in range(2)]
            ggr = [Res(f"gg{i}") for i in range(2)]
            m1 = [sb(f"m1{i}", [128, 512], F32, sf) for i in range(2)]
            m2 = [sb(f"m2{i}", [128, 512], F32, sf) for i in range(2)]
            mr = [Res(f"m{i}") for i in range(2)]
            mgt = [sb(f"mgt{i}", [128, 512], BF16, sf) for i in range(2)]
            mgr = [Res(f"mgt{i}") for i in range(2)]
            it = 0
            def issue_f(nb_):
                S.dma("pool", wso[nb_ % 2][:], wso_v[:, :, nb_ * 512:(nb_ + 1) * 512], writes=[wfr[nb_ % 2]])
                S.dma("pool", wmo[nb_ % 2][:], wmo_v[:, :, nb_ * 512:(nb_ + 1) * 512], writes=[wfr2[nb_ % 2]])

            issue_f(0)
            for nb in range(4):
                wk = nb % 2
                if nb + 1 < 4:
                    issue_f(nb + 1)
                for tg in range(4):
                    tsl = slice(tg * 512, (tg + 1) * 512)
                    for nch in range(4):
                        k = it % 2
                        it += 1
                        ch = nb * 4 + nch
                        pa, pb2 = 2 * ((it - 1) % 3), 2 * ((it - 1) % 3) + 1
                        S.dma("sp", gat[k][:], GA[ch, :, tsl], writes=[ggr[k]])
                        S.dma("sp", gbt[k][:], GB[ch, :, tsl], writes=[ggr[k]])

                        def mma(pe, wk=wk, nch=nch, tsl=tsl, pa=pa):
                            for kc in range(8):
                                ins = pe.matmul(PB[pa][:, :], lhsT=wso[wk][:, kc, nch * 128:(nch + 1) * 128], rhs=ST[:, kc, tsl],
                                                start=(kc == 0), stop=(kc == 7))
                            return ins

                        def mmb(pe, wk=wk, nch=nch, tsl=tsl, pb2=pb2):
                            for kc in range(16):
                                ins = pe.matmul(PB[pb2][:, :], lhsT=wmo[wk][:, kc, nch * 128:(nch + 1) * 128], rhs=AT[:, kc, tsl],
                                                start=(kc == 0), stop=(kc == 15))
                            return ins
                        S.op("pe", mma, reads=[wfr[wk], STr], writes=[PBr[pa]])
                        S.op("pe", mmb, reads=[wfr2[wk], ATr], writes=[PBr[pb2]])
                        S.op("dve", lambda v, k=k, pa=pa: v.tensor_tensor(out=m1[k][:], in0=PB[pa][:, :], in1=gat[k][:], op=ALU.mult),
                             reads=[PBr[pa], ggr[k]], writes=[mr[k]])
                        S.op("dve", lambda v, k=k, pb2=pb2: v.tensor_tensor(out=m2[k][:], in0=PB[pb2][:, :], in1=gbt[k][:], op=ALU.mult),
                             reads=[PBr[pb2], ggr[k]], writes=[mr[k]])
                        S.op("pool", lambda g_, k=k: g_.tensor_tensor(out=mgt[k][:], in0=m1[k][:], in1=m2[k][:], op=ALU.add),
                             reads=[mr[k]], writes=[mgr[k]])
                        S.dma("pool", MG[ch, :, tsl], mgt[k][:], reads=[mgr[k]])
            S.barrier()
        MGs, MGr = NT, [Res(f"MGs{i}") for i in range(16)]
        with ExitStack() as sg:
            for ch in range(16):
                S.dma("sp", MGs[:, ch, :], MG[ch, :, :], writes=[MGr[ch]])
            wo = [sb(f"wo{i}", [128, 16, 512], BF16, sg) for i in range(2)]
            wor = [Res(f"wo{i}") for i in range(2)]
            xp = [sb(f"xp{i}", [128, 512], F32, sg) for i in range(2)]
            xpr = [Res(f"xp{i}") for i in range(2)]
            hp = [sb(f"hp{i}", [128, 512], F32, sg) for i in range(2)]
            hpr = [Res(f"hp{i}") for i in range(2)]
            ho = [sb(f"ho{i}", [128, 512], F32, sg) for i in range(2)]
            hor = [Res(f"ho{i}") for i in range(2)]
            it = 0
            def issue_g(nb_):
                S.dma("pool", wo[nb_ % 2][:], wo_v[:, :, nb_ * 512:(nb_ + 1) * 512], writes=[wor[nb_ % 2]])

            issue_g(0)
            for nb in range(4):
                wk = nb % 2
                csl = slice(nb * 512, (nb + 1) * 512)
                if nb + 1 < 4:
                    issue_g(nb + 1)
                for tt in range(16):
                    k = it % 2
                    pb = it % 6
                    it += 1
                    S.dma("sp", xp[k][:], x[tt * 128:(tt + 1) * 128, csl], writes=[xpr[k]])

                    def mmo(pe, wk=wk, tt=tt, pb=pb):
                        for kc in range(16):
                            ins = pe.matmul(PB[pb][:, :], lhsT=MGs[:, kc, tt * 128:(tt + 1) * 128], rhs=wo[wk][:, kc, :],
                                            start=(kc == 0), stop=(kc == 15))
                        return ins
                    S.op("pe", mmo, reads=MGr + [wor[wk]], writes=[PBr[pb]])
                    S.op("dve", lambda v, k=k, pb=pb, csl=csl: v.tensor_tensor(out=hp[k][:], in0=PB[pb][:, :], in1=gm_bc[:, csl], op=ALU.mult),
                         reads=[PBr[pb], bcr], writes=[hpr[k]])
                    S.op("pool", lambda g_, k=k: g_.tensor_tensor(out=ho[k][:], in0=hp[k][:], in1=xp[k][:], op=ALU.add),
                         reads=[hpr[k], xpr[k]], writes=[hor[k]])
                    S.dma("pool", H1[tt * 128:(tt + 1) * 128, csl], ho[k][:], reads=[hor[k]])
            S.barrier()
        if stop_after == "G":
            return nc
        n2b_all, n2r = NT, Res("n2b")
        weg_v, weu_v, wed_v = WGB, WUB, WDB
        with ExitStack() as sh:
            c2 = sb("c2", [128, C2_W], F32, sh)
            c2r = Res("c2")
            S.dma("sp", c2[:], cpack2[:, :], writes=[c2r])
            wrf = sb("wrf", [128, 16, 36], F32, sh)
            brb = sb("brb", [128, 36], F32, sh)
            S.dma("sp", wrf[:].rearrange("p a b -> p (a b)"), wr_d[:, :], writes=[c2r])
            S.dma("sp", brb[:], dram_bcast(br_d[0:1, :], 36), writes=[c2r])
            oh1_all = sb("oh1", [128, 16, 32], F32, sh)
            oh2_all = sb("oh2", [128, 16, 32], F32, sh)
            cnt_all = sb("cnta", [128, 16, 32], BF16, sh)
            allr = Res("routeall")
            lg_all = sb("lg_all", [128, 16, 36], F32, sh)
            gmx16 = sb("gmx16", [128, 16], F32, sh)
            ohg16 = sb("ohg16", [128, 16, 4], F32, sh)
            d416 = sb("d416", [128, 16, 4], F32, sh)
            pg16 = sb("pg16", [128, 16], F32, sh)
            elm16 = sb("elm16", [128, 16, 32], F32, sh)
            m816 = sb("m816", [128, 16, 8], F32, sh)
            e16 = sb("e16", [128, 16], F32, sh)
            sm = sb("sm", [128, 64], F32, sh)
            elm = sb("elm", [128, 32], F32, sh)
            m8r = sb("m8r", [128, 8], F32, sh)
            ex4 = sb("ex4", [128, 4], F32, sh)
            rr = Res("rsmall")
            sh1 = sh.enter_context(ExitStack())
            h1t = [sb(f"h1t{i}", [128, D], F32, sh1) for i in range(2)]
            h1r = [Res(f"h1t{i}") for i in range(2)]
            n2f = sb("n2f", [128, D], F32, sh1)
            n2fr = Res("n2f")
            junk = sb("hjunk", [128, D], BF16, sh1)
            jr = Res("hjunk")
            ssq = sb("hssq", [128, 1], F32, sh1)
            sr = Res("hssq")
            n2T = sb("n2T", [128, 16, 128], F32, sh1)
            n2Tr = Res("n2T")
            for tt in range(16):
                k = tt % 2
                S.dma("sp", h1t[k][:], H1[tt * 128:(tt + 1) * 128, :], writes=[h1r[k]])
                S.op("act", lambda a, k=k: a.activation(out=junk[:], in_=h1t[k][:], func=AF.Square, accum_out=ssq[:]),
                     reads=[h1r[k]], writes=[jr, sr])
                S.op("act", lambda a: a.activation(out=ssq[:], in_=ssq[:], func=AF.Sqrt, scale=1.0 / D, bias=epsc[:, 0:1]),
                     reads=[sr, constr], writes=[sr])
                S.op("dve", lambda v: v.reciprocal(out=ssq[:], in_=ssq[:]), reads=[sr], writes=[sr])
                S.op("dve", lambda v, k=k: v.scalar_tensor_tensor(out=n2f[:], in0=h1t[k][:], scalar=ssq[:, 0:1], in1=G2_bc[:],
                                                                   op0=ALU.mult, op1=ALU.mult), reads=[h1r[k], sr, bcr], writes=[n2fr])
                S.op("pool", lambda g_: g_.tensor_tensor(out=n2f[:], in0=n2f[:], in1=shf_bc[:], op=ALU.add), reads=[n2fr, bcr], writes=[n2fr])
                S.op("act", lambda a, tt=tt: a.activation(out=n2b_all[:, tt, :], in_=n2f[:], func=AF.Copy), reads=[n2fr], writes=[n2r])
                for b4 in range(4):
                    def trf(pe, b4=b4):
                        for jj in range(4):
                            kc = b4 * 4 + jj
                            ins = pe.transpose(out=PB[b4][:, jj * 128:(jj + 1) * 128], in_=n2f[:, kc * 128:(kc + 1) * 128],
                                               identity=cp[:, CP_IDENT:CP_IDENT + 128])
                        return ins
                    S.op("pe", trf, reads=[n2fr, cpr], writes=[PBr[b4]])
                    if b4 % 2 == 0:
                        S.op("dve", lambda v, b4=b4: v.tensor_copy(out=n2T[:, b4 * 4:(b4 + 1) * 4, :].rearrange("p a b -> p (a b)"),
                                                                   in_=PB[b4][:, :]), reads=[PBr[b4]], writes=[n2Tr])
                    else:
                        S.op("act", lambda a, b4=b4: a.activation(out=n2T[:, b4 * 4:(b4 + 1) * 4, :].rearrange("p a b -> p (a b)"),
                                                                  in_=PB[b4][:, :], func=AF.Copy), reads=[PBr[b4]], writes=[n2Tr])

                def lgm(pe):
                    for kc in range(16):
                        ins = pe.matmul(PB[4][:, 0:36], lhsT=n2T[:, kc, :], rhs=wrf[:, kc, :], start=(kc == 0), stop=(kc == 15))
                    return ins
                S.op("pe", lgm, reads=[n2Tr, c2r], writes=[PBr[4]])
                S.op("dve", lambda v, tt=tt: v.tensor_tensor(out=lg_all[:, tt, :], in0=PB[4][:, 0:36], in1=brb[:], op=ALU.add),
                     reads=[PBr[4], c2r], writes=[rr])
            gl = lg_all[:, :, 0:4]
            S.op("dve", lambda v: v.tensor_reduce(out=gmx16[:], in_=gl, axis=AX.X, op=ALU.max), reads=[rr], writes=[rr])
            S.op("dve", lambda v: v.tensor_tensor(out=ohg16[:], in0=gl, in1=gmx16[:].unsqueeze(2).broadcast_to([128, 16, 4]), op=ALU.is_ge),
                 reads=[rr], writes=[rr])
            S.op("dve", lambda v: v.tensor_tensor(out=d416[:], in0=gl, in1=gmx16[:].unsqueeze(2).broadcast_to([128, 16, 4]), op=ALU.subtract),
                 reads=[rr], writes=[rr])
            S.op("act", lambda a: a.activation(out=d416[:], in_=d416[:], func=AF.Exp), reads=[rr], writes=[rr])
            S.op("dve", lambda v: v.tensor_reduce(out=pg16[:], in_=d416[:], axis=AX.X, op=ALU.add), reads=[rr], writes=[rr])
            S.op("dve", lambda v: v.reciprocal(out=pg16[:], in_=pg16[:]), reads=[rr], writes=[rr])
            S.op("dve", lambda v: v.tensor_scalar(out=ohg16[:], in0=ohg16[:], scalar1=-1.0, scalar2=1e9, op0=ALU.add, op1=ALU.mult),
                 reads=[rr], writes=[rr])
            S.op("dve", lambda v: v.tensor_tensor(out=elm16[:].rearrange("p t (g e) -> p t g e", e=8),
                                                  in0=lg_all[:, :, 4:36].rearrange("p t (g e) -> p t g e", e=8),
                                                  in1=ohg16[:].unsqueeze(3).broadcast_to([128, 16, 4, 8]), op=ALU.add), reads=[rr], writes=[rr])
            for tt in range(16):
                S.op("dve", lambda v, tt=tt: v.max(out=m816[:, tt, :], in_=elm16[:, tt, :]), reads=[rr], writes=[rr])
            S.op("dve", lambda v: v.tensor_tensor(out=oh1_all[:], in0=elm16[:], in1=m816[:, :, 0:1].broadcast_to([128, 16, 32]), op=ALU.is_equal),
                 reads=[rr], writes=[allr])
            S.op("dve", lambda v: v.tensor_tensor(out=oh2_all[:], in0=elm16[:], in1=m816[:, :, 1:2].broadcast_to([128, 16, 32]), op=ALU.is_equal),
                 reads=[rr], writes=[allr])
            S.op("dve", lambda v: v.tensor_tensor(out=cnt_all[:], in0=oh1_all[:], in1=oh2_all[:], op=ALU.add), reads=[allr], writes=[allr])
            S.op("dve", lambda v: v.tensor_tensor(out=e16[:], in0=m816[:, :, 1], in1=m816[:, :, 0], op=ALU.subtract), reads=[rr], writes=[rr])
            S.op("act", lambda a: a.activation(out=e16[:], in_=e16[:], func=AF.Exp), reads=[rr], writes=[rr])
            S.op("dve", lambda v: v.tensor_scalar(out=w1_all[:], in0=e16[:], scalar1=1.0, scalar2=None, op0=ALU.add), reads=[rr], writes=[wallr])
            S.op("dve", lambda v: v.reciprocal(out=w1_all[:], in_=w1_all[:]), reads=[wallr], writes=[wallr])
            S.op("dve", lambda v: v.tensor_tensor(out=w1_all[:], in0=w1_all[:], in1=pg16[:], op=ALU.mult), reads=[wallr, rr], writes=[wallr])
            S.op("dve", lambda v: v.tensor_tensor(out=w2_all[:], in0=w1_all[:], in1=e16[:], op=ALU.mult), reads=[wallr, rr], writes=[wallr])
            S.barrier()
            sh1.close()
            cntf = sb("cntf", [128, 32], F32, sh)
            nblk = sb("nblk", [128, 32], F32, sh)
            endb = sb("endb", [128, 32], F32, sh)
            endt = sb("endt", [128, 32], F32, sh)
            startp = sb("startp", [128, 32], F32, sh)
            cmp1 = sb("cmp1", [128, 32, 32], F32, sh)
            cmp2 = sb("cmp2", [128, 64, 32], F32, sh)
            bef = sb("bef", [128, 64], F32, sh)
            idxf = sb("idxf", [128, 64, 4], F32, sh)
            p2r = Res("pass2")

            def cmm(pe):
                for tt in range(16):
                    ins = pe.matmul(PB[5][:, 0:32], lhsT=onesb[:], rhs=cnt_all[:, tt, :], start=(tt == 0), stop=(tt == 15))
                return ins
            S.op("pe", cmm, reads=[allr, constr], writes=[PBr[5]])
            S.op("dve", lambda v: v.tensor_copy(out=cntf[:], in_=PB[5][:, 0:32]), reads=[PBr[5]], writes=[p2r])
            S.op("dve", lambda v: v.tensor_tensor(out=cmp1[:], in0=cntf[:].unsqueeze(2).broadcast_to([128, 32, 32]),
                                                  in1=c2[:, C2_THR:C2_THR + 1024].rearrange("p (a b) -> p a b", b=32), op=ALU.is_gt),
                 reads=[p2r, c2r], writes=[p2r])
            S.op("dve", lambda v: v.tensor_reduce(out=nblk[:], in_=cmp1[:], axis=AX.X, op=ALU.add), reads=[p2r], writes=[p2r])
            S.op("dve", lambda v: v.tensor_copy(out=endb[:], in_=nblk[:]), reads=[p2r], writes=[p2r])
            for sft in (1, 2, 4, 8, 16):
                S.op("dve", lambda v: v.tensor_copy(out=endt[:], in_=endb[:]), reads=[p2r], writes=[p2r])
                S.op("dve", lambda v, sft=sft: v.tensor_tensor(out=endb[:, sft:32], in0=endt[:, sft:32], in1=endt[:, 0:32 - sft], op=ALU.add),
                     reads=[p2r], writes=[p2r])
            S.op("dve", lambda v: v.tensor_tensor(out=startp[:], in0=endb[:], in1=nblk[:], op=ALU.subtract), reads=[p2r], writes=[p2r])
            S.op("dve", lambda v: v.tensor_scalar(out=startp[:], in0=startp[:], scalar1=float(RB), scalar2=None, op0=ALU.mult), reads=[p2r], writes=[p2r])
            S.op("dve", lambda v: v.tensor_tensor(out=cmp2[:], in0=endb[:].unsqueeze(1).broadcast_to([128, 64, 32]),
                                                  in1=c2[:, C2_BLKI:C2_BLKI + 2048].rearrange("p (a b) -> p a b", b=32), op=ALU.is_le),
                 reads=[p2r, c2r], writes=[p2r])
            S.op("dve", lambda v: v.tensor_reduce(out=bef[:], in_=cmp2[:], axis=AX.X, op=ALU.add), reads=[p2r], writes=[p2r])
            S.op("dve", lambda v: v.tensor_scalar(out=bef[:], in0=bef[:], scalar1=31.0, scalar2=512.0, op0=ALU.min, op1=ALU.mult),
                 reads=[p2r], writes=[p2r])
            S.op("dve", lambda v: v.tensor_scalar(out=sm[:, 32:33], in0=cp[:, CP_IOTAP:CP_IOTAP + 1], scalar1=4.0, scalar2=None, op0=ALU.mult),
                 reads=[cpr, rr], writes=[rr])
            for q in range(4):
                S.op("dve", lambda v, q=q: v.tensor_scalar(out=idxf[:, :, q], in0=bef[:], scalar1=sm[:, 32:33], scalar2=float(q),
                                                           op0=ALU.add, op1=ALU.add), reads=[p2r, rr], writes=[p2r])
            S.op("dve", lambda v: v.tensor_scalar(out=bef[:], in0=c2[:, C2_BLKI:C2_BLKI + 2048].rearrange("p (a b) -> p a b", b=32)[:, :, 0],
                                                  scalar1=endb[:, 31:32], scalar2=1.0e6, op0=ALU.is_ge, op1=ALU.mult), reads=[p2r, c2r], writes=[p2r])
            for q in range(4):
                S.op("dve", lambda v, q=q: v.tensor_tensor(out=idxf[:, :, q], in0=idxf[:, :, q], in1=bef[:], op=ALU.add), reads=[p2r], writes=[p2r])
            S.op("dve", lambda v: v.tensor_copy(out=idx4[:], in_=idxf[:].rearrange("p a b -> p (a b)")), reads=[p2r], writes=[idxr])
            if "ROUTE" in dbg:
                S.dma("sp", DBG[:, 0:64], bef[:], reads=[p2r])
                S.dma("sp", DBG[:, 64:96], cntf[:], reads=[p2r])
                S.dma("sp", DBG[:, 96:128], startp[:], reads=[p2r])
            dest = sb("dest", [128, 32], F32, sh)
            tmp32 = sb("tmp32", [128, 32], F32, sh)
            dr = Res("dest")
            cap_reg = nc.gpsimd.to_reg(CAPROWS - 1)
            dcol = [sb(f"dcol{i}", [128, 1], I32, sh) for i in range(4)]
            dcr2 = [Res(f"dcol{i}") for i in range(4)]
            di = 0
            for tt in range(16):
                def rmm(pe, tt=tt):
                    for t2_ in range(tt):
                        pe.matmul(PB[5][:, 0:32], lhsT=onesb[:], rhs=cnt_all[:, t2_, :], start=(t2_ == 0), stop=False)
                    return pe.matmul(PB[5][:, 0:32], lhsT=lstrb[:], rhs=cnt_all[:, tt, :], start=(tt == 0), stop=True)
                S.op("pe", rmm, reads=[allr, constr], writes=[PBr[5]])
                S.op("dve", lambda v: v.tensor_tensor(out=dest[:], in0=PB[5][:, 0:32], in1=startp[:], op=ALU.add), reads=[PBr[5], p2r], writes=[dr])
                for kk, (oh, dall) in enumerate(((oh1_all, d1f), (oh2_all, d2f))):
                    S.op("dve", lambda v, oh=oh, tt=tt: v.tensor_tensor(out=tmp32[:], in0=oh[:, tt, :], in1=dest[:], op=ALU.mult),
                         reads=[allr, dr], writes=[dr])
                    S.op("dve", lambda v, dall=dall, tt=tt: v.tensor_reduce(out=dall[:, tt:tt + 1], in_=tmp32[:], axis=AX.X, op=ALU.add),
                         reads=[dr], writes=[wallr])
                    dc = di % 4
                    di += 1
                    S.op("dve", lambda v, dall=dall, tt=tt, dc=dc: v.tensor_copy(out=dcol[dc][:], in_=dall[:, tt:tt + 1]),
                         reads=[wallr], writes=[dcr2[dc]])
                    S.idma(lambda g_, dc=dc, tt=tt: g_.indirect_dma_start(
                        out=XS[:, :], out_offset=bass.IndirectOffsetOnAxis(ap=dcol[dc][:, 0:1], axis=0),
                        in_=n2b_all[:, tt, :], in_offset=None, bounds_check=cap_reg, oob_is_err=False), reads=[dcr2[dc], n2r])
            if "ROUTE" in dbg:
                S.dma("sp", DBG[:, 128:144], d1f[:], reads=[wallr])
                S.dma("sp", DBG[:, 144:160], d2f[:], reads=[wallr])
                S.dma("sp", DBG[:, 160:176], w1_all[:], reads=[wallr])
                S.dma("sp", DBG[:, 176:192], w2_all[:], reads=[wallr])
            S.barrier()
        if stop_after == "H":
            return nc
        sNT.close()

        with ExitStack() as si:
            wg = [sb(f"wg{i}", [128, 16, 512], BF16, si) for i in range(2)]
            wu = [sb(f"wu{i}", [128, 16, 512], BF16, si) for i in range(2)]
            wd = [sb(f"wd{i}", [128, 4, 2048], BF16, si) for i in range(2)]
            wgr = [[Res(f"wg{i}_{q}") for q in range(4)] for i in range(2)]
            wur = [[Res(f"wu{i}_{q}") for q in range(4)] for i in range(2)]
            wdr = [[Res(f"wd{i}_{q}") for q in range(4)] for i in range(2)]
            iq = [[sb(f"iq{i}_{q}", [128, 1], I32, si) for q in range(4)] for i in range(NBLK)]
            iqr = [Res(f"iq{i}") for i in range(NBLK)]
            for blk in range(NBLK):
                for q in range(4):
                    S.op("dve", lambda v, q=q, blk=blk: v.tensor_copy(out=iq[blk][q][:], in_=idx4[:, blk * 4 + q:blk * 4 + q + 1]),
                         reads=[idxr], writes=[iqr[blk]])
            xrow = [sb(f"xrow{i}", [128, D], BF16, si) for i in range(4)]
            xrr = [Res(f"xrow{i}") for i in range(4)]
            xT = [sb(f"xT{i}", [128, 16, RB], BF16, si) for i in range(2)]
            xTr = [[Res(f"xT{i}_{r}") for r in range(2)] for i in range(2)]
            sg = [sb(f"sg{i}", [128, RB], F32, si) for i in range(2)]
            sgr = [Res(f"sg{i}") for i in range(2)]
            hT = [sb(f"hT{i}", [128, 4, RB], BF16, si) for i in range(2)]
            hTr = [Res(f"hT{i}") for i in range(2)]
            yt = [sb(f"yt{i}", [128, D], BF16, si) for i in range(2)]
            ytr = [Res(f"yt{i}") for i in range(2)]
            fi = 0
            xi = 0
            yi = 0
            bc_reg = nc.gpsimd.to_reg(NEXP * 128 * 4 - 1)
            xks = {}

            def emit_gathers(blk):
                k = blk % 2
                for (wt_, src, nkc, wrs) in ((wg, weg_v, 4, wgr), (wu, weu_v, 4, wur), (wd, wed_v, 1, wdr)):
                    for q in range(4):
                        if nkc == 4:
                            o = wt_[k][:, 4 * q:4 * q + 4, :].rearrange("p a b -> p (a b)")
                        else:
                            o = wt_[k][:, q, :]
                        S.idma(lambda g_, o=o, src=src, blk=blk, q=q: g_.indirect_dma_start(
                            out=o, out_offset=None, in_=src[:, :], in_offset=bass.IndirectOffsetOnAxis(ap=iq[blk][q][:, 0:1], axis=0),
                            bounds_check=bc_reg, oob_is_err=False),
                            reads=[iqr[blk]], writes=[wrs[k][q]])

            def emit_xload(blk):
                for r in range(RB // 128):
                    xk = (blk * 2 + r) % 4
                    r0 = blk * RB + r * 128
                    S.dma("sp", xrow[xk][:], XS[r0:r0 + 128, :], reads=[XSr], writes=[xrr[xk]])

            def emit_xT(blk):
                k = blk % 2
                for r in range(RB // 128):
                    xk = (blk * 2 + r) % 4
                    for half in range(2):
                        def trx(pe, xk=xk, half=half):
                            for jj in range(8):
                                kc = half * 8 + jj
                                ins = pe.transpose(out=PT[half][:, jj * 128:(jj + 1) * 128], in_=xrow[xk][:, kc * 128:(kc + 1) * 128],
                                                   identity=identb[:])
                            return ins
                        S.op("pe", trx, reads=[xrr[xk], constr], writes=[PTr[half]])
                        if half == 0:
                            S.op("act", lambda a, k=k, r=r: a.activation(out=xT[k][:, 0:8, r * 128:(r + 1) * 128],
                                                                         in_=PT[0][:, :].rearrange("p (a b) -> p a b", b=128), func=AF.Copy),
                                 reads=[PTr[0]], writes=[xTr[k][r]])
                        else:
                            S.op("dve", lambda v, k=k, r=r: v.tensor_copy(out=xT[k][:, 8:16, r * 128:(r + 1) * 128],
                                                                          in_=PT[1][:, :].rearrange("p (a b) -> p a b", b=128)),
                                 reads=[PTr[1]], writes=[xTr[k][r]])

            def emit_gateup(blk):
                nonlocal_fi = fi_box
                k = blk % 2
                for fc in range(4):
                    f2 = nonlocal_fi[0] % 2
                    nonlocal_fi[0] += 1
                    pg, pu = 2 * f2, 2 * f2 + 1

                    def gmm_(pe, k=k, fc=fc, pg=pg):
                        for kc in range(16):
                            ins = pe.matmul(PB[pg][:, 0:RB], lhsT=wg[k][:, kc, fc * 128:(fc + 1) * 128], rhs=xT[k][:, kc, :],
                                            start=(kc == 0), stop=(kc == 15))
                        return ins

                    def umm_(pe, k=k, fc=fc, pu=pu):
                        for kc in range(16):
                            ins = pe.matmul(PB[pu][:, 0:RB], lhsT=wu[k][:, kc, fc * 128:(fc + 1) * 128], rhs=xT[k][:, kc, :],
                                            start=(kc == 0), stop=(kc == 15))
                        return ins
                    S.op("pe", gmm_, reads=wgr[k] + xTr[k], writes=[PBr[pg]])
                    S.op("pe", umm_, reads=wur[k] + xTr[k], writes=[PBr[pu]])
                    S.op("act", lambda a, f2=f2, pg=pg: a.activation(out=sg[f2][:], in_=PB[pg][:, 0:RB], func=AF.Silu),
                         reads=[PBr[pg]], writes=[sgr[f2]])
                    S.op("dve", lambda v, k=k, fc=fc, f2=f2, pu=pu: v.tensor_tensor(out=hT[k][:, fc, :], in0=PB[pu][:, 0:RB], in1=sg[f2][:],
                                                                                   op=ALU.mult), reads=[PBr[pu], sgr[f2]], writes=[hTr[k]])

            def emit_down(blk):
                k = blk % 2
                for r in range(RB // 128):
                    yk = (blk * 2 + r) % 2
                    for nt in range(4):
                        py = 4 + nt % 2

                        def dmm(pe, k=k, nt=nt, py=py, r=r):
                            for fc in range(4):
                                ins = pe.matmul(PB[py][:, :], lhsT=hT[k][:, fc, r * 128:(r + 1) * 128], rhs=wd[k][:, fc, nt * 512:(nt + 1) * 512],
                                                start=(fc == 0), stop=(fc == 3))
                            return ins
                        S.op("pe", dmm, reads=[hTr[k]] + wdr[k], writes=[PBr[py]])
                        if nt % 2 == 0:
                            S.op("act", lambda a, yk=yk, nt=nt, py=py: a.activation(out=yt[yk][:, nt * 512:(nt + 1) * 512], in_=PB[py][:, :],
                                                                                   func=AF.Copy), reads=[PBr[py]], writes=[ytr[yk]])
                        else:
                            S.op("dve", lambda v, yk=yk, nt=nt, py=py: v.tensor_copy(out=yt[yk][:, nt * 512:(nt + 1) * 512], in_=PB[py][:, :]),
                                 reads=[PBr[py]], writes=[ytr[yk]])
                    r0 = blk * RB + r * 128
                    S.dma("sp", YS[r0:r0 + 128, :], yt[yk][:], reads=[ytr[yk]])

            fi_box = [0]
            emit_gathers(0)
            emit_xload(0)
            emit_xT(0)
            emit_gathers(1)
            emit_xload(1)
            for blk in range(NBLK):
                emit_gateup(blk)
                if blk + 1 < NBLK:
                    emit_xT(blk + 1)
                if blk + 2 < NBLK:
                    emit_xload(blk + 2)
                emit_down(blk)
                if blk + 2 < NBLK:
                    emit_gathers(blk + 2)
            S.barrier()

        with ExitStack() as sj:
            nfin_bc = sb("nfin_bc", [128, D], F32, sj)
            nfr = Res("nfin")
            S.dma("sp", nfin_bc[:], dram_bcast(nfin_g[0:1, :], D), writes=[nfr])
            y1 = [sb(f"y1{i}", [128, D], BF16, sj) for i in range(2)]
            y2 = [sb(f"y2{i}", [128, D], BF16, sj) for i in range(2)]
            acc = [sb(f"jacc{i}", [128, D], F32, sj) for i in range(2)]
            accr = [Res(f"jacc{i}") for i in range(2)]
            yr = [Res(f"y{i}") for i in range(2)]
            yr2 = [Res(f"yb{i}") for i in range(2)]
            h1j = [sb(f"h1j{i}", [128, D], F32, sj) for i in range(2)]
            h1jr = [Res(f"h1j{i}") for i in range(2)]
            ot = [sb(f"jo{i}", [128, D], F32, sj) for i in range(2)]
            orr = [Res(f"jo{i}") for i in range(2)]
            junk = sb("jjunk", [128, D], BF16, sj)
            jr = Res("jjunk")
            ssq = [sb(f"jssq{i}", [128, 1], F32, sj) for i in range(2)]
            sr = [Res(f"jssq{i}") for i in range(2)]
            jc = [[sb(f"jc{i}_{z}", [128, 1], I32, sj) for z in range(2)] for i in range(16)]
            jcr = [Res(f"jc{i}") for i in range(16)]
            jcr2 = [Res(f"jcb{i}") for i in range(16)]
            for tt in range(16):
                S.op("dve", lambda v, tt=tt: v.tensor_copy(out=jc[tt][0][:], in_=d1f[:, tt:tt + 1]), reads=[wallr], writes=[jcr[tt]])
                S.op("dve", lambda v, tt=tt: v.tensor_copy(out=jc[tt][1][:], in_=d2f[:, tt:tt + 1]), reads=[wallr], writes=[jcr2[tt]])
            def j_loads(tt):
                k = tt % 2
                S.idma(lambda g_, k=k: g_.indirect_dma_start(out=y1[k][:], out_offset=None, in_=YS[:, :],
                                                            in_offset=bass.IndirectOffsetOnAxis(ap=jc[tt][0][:, 0:1], axis=0),
                                                            bounds_check=cap_reg, oob_is_err=False),
                       reads=[jcr[tt], YSr], writes=[yr[k]])
                S.idma(lambda g_, k=k: g_.indirect_dma_start(out=y2[k][:], out_offset=None, in_=YS[:, :],
                                                            in_offset=bass.IndirectOffsetOnAxis(ap=jc[tt][1][:, 0:1], axis=0),
                                                            bounds_check=cap_reg, oob_is_err=False),
                       reads=[jcr2[tt], YSr], writes=[yr2[k]])
                S.dma("sp", h1j[k][:], H1[tt * 128:(tt + 1) * 128, :], writes=[h1jr[k]])

            j_loads(0)
            for tt in range(16):
                k = tt % 2
                if tt + 1 < 16:
                    j_loads(tt + 1)
                S.op("dve", lambda v, k=k, tt=tt: v.tensor_scalar(out=acc[k][:], in0=y1[k][:], scalar1=w1_all[:, tt:tt + 1], scalar2=None,
                                                                  op0=ALU.mult), reads=[yr[k], wallr], writes=[accr[k]])
                S.op("dve", lambda v, k=k, tt=tt: v.scalar_tensor_tensor(out=acc[k][:], in0=y2[k][:], scalar=w2_all[:, tt:tt + 1], in1=acc[k][:],
                                                                         op0=ALU.mult, op1=ALU.add), reads=[accr[k], yr2[k], wallr], writes=[accr[k]])
                S.op("pool", lambda g_, k=k: g_.tensor_tensor(out=acc[k][:], in0=acc[k][:], in1=gf_bc[:], op=ALU.mult), reads=[accr[k], bcr], writes=[accr[k]])
                S.op("dve", lambda v, k=k: v.tensor_tensor(out=h1j[k][:], in0=h1j[k][:], in1=acc[k][:], op=ALU.add), reads=[accr[k], h1jr[k]], writes=[h1jr[k]])
                S.op("act", lambda a, k=k: a.activation(out=junk[:], in_=h1j[k][:], func=AF.Square, accum_out=ssq[k][:]),
                     reads=[h1jr[k]], writes=[jr, sr[k]])
                S.op("act", lambda a, k=k: a.activation(out=ssq[k][:], in_=ssq[k][:], func=AF.Sqrt, scale=1.0 / D, bias=epsc[:, 0:1]),
                     reads=[sr[k], constr], writes=[sr[k]])
                S.op("dve", lambda v, k=k: v.reciprocal(out=ssq[k][:], in_=ssq[k][:]), reads=[sr[k]], writes=[sr[k]])
                S.op("dve", lambda v, k=k: v.scalar_tensor_tensor(out=ot[k][:], in0=h1j[k][:], scalar=ssq[k][:, 0:1], in1=nfin_bc[:],
                                                                   op0=ALU.mult, op1=ALU.mult), reads=[h1jr[k], sr[k], nfr], writes=[orr[k]])
                S.dma("sp", out[tt * 128:(tt + 1) * 128, :], ot[k][:], reads=[orr[k]])
            S.barrier()
    return nc


def _local_order(j):
    own = [2 * i + j for i in range(8)]
    oth = [2 * i + 1 - j for i in range(8)]
    return own + oth


def _const_pack(j):
    cpk = np.zeros((128, CP_W), np.float32)
    p = np.arange(128)
    cpk[:, CP_IDENT:CP_IDENT + 128] = np.eye(128)
    pm = np.zeros((128, 128), np.float32)
    for pp in range(64):
        pm[pp + 64, pp] = -1.0
        pm[pp, pp + 64] = 1.0
    cpk[:, CP_PM:CP_PM + 128] = pm
    cpk[:, CP_TRI01:CP_TRI01 + 128] = (p[:, None] <= p[None, :])
    cpk[:, CP_TRIPEN:CP_TRIPEN + 128] = np.where(p[:, None] <= p[None, :], 0.0, -BIG)
    cpk[:, CP_LSTRICT:CP_LSTRICT + 128] = (p[:, None] < p[None, :])
    cpk[:, CP_ONES:CP_ONES + 128] = 1.0
    invf = (10000.0 ** (-np.arange(0, 128, 2, dtype=np.float32) / 128)).astype(np.float32)
    cpk[:, CP_INVF] = np.concatenate([invf, invf])
    cpk[:, CP_IOTAP] = p
    cpk[:, CP_SIGN] = np.where(p < 64, -1.0, 1.0)
    order = _local_order(j)
    pp_ = np.zeros((8, 16), np.float32)
    p01 = np.zeros((8, 16), np.float32)
    for i in range(8):
        gq = 2 * i + j
        for kb in range(16):
            past = order[kb] < gq
            pp_[i, kb] = 0.0 if past else -1e30
            p01[i, kb] = 1.0 if past else 0.0
    cpk[:, CP_PASTPEN:CP_PASTPEN + 128] = pp_.reshape(1, 128)
    cpk[:, CP_PAST01:CP_PAST01 + 128] = p01.reshape(1, 128)
    return cpk


def _const_pack2():
    c2 = np.zeros((128, C2_W), np.float32)
    c2[:, C2_THR:C2_THR + 1024] = np.tile(float(RB) * np.arange(32, dtype=np.float32), 32)[None, :]
    c2[:, C2_BLKI:C2_BLKI + 2048] = np.repeat(np.arange(64, dtype=np.float32), 32)[None, :]
    return c2


def _prep_shared(inp):
    f = np.float32
    sh = {}
    sh["w_ada"] = np.ascontiguousarray(inp["w_ada"][0], f)
    sh["b_ada"] = np.ascontiguousarray(inp["b_ada"][0].reshape(1, -1), f)
    sh["gmix"] = np.ascontiguousarray(inp["norm_mix_g"][0].reshape(16, 128).T, f)
    sh["w_in"] = np.ascontiguousarray(inp["w_in"][0], f)
    sh["sgu_ln_g"] = np.ascontiguousarray(inp["sgu_ln_g"][0].reshape(1, -1), f)
    sh["sgu_ln_b"] = np.ascontiguousarray(inp["sgu_ln_b"][0].reshape(1, -1), f)
    sh["wsT"] = np.ascontiguousarray(np.transpose(inp["sgu_w_s"][0], (2, 0, 1)).reshape(128, 1024), f)
    sh["sgu_b_s"] = np.ascontiguousarray(inp["sgu_b_s"][0].reshape(1, -1), f)
    sh["w_sgu_out"] = np.ascontiguousarray(inp["w_sgu_out"][0], f)
    sh["w_moba_out"] = np.ascontiguousarray(inp["w_moba_out"][0], f)
    sh["w_out"] = np.ascontiguousarray(inp["w_out"][0], f)
    sh["norm_ffn_g"] = np.ascontiguousarray(inp["norm_ffn_g"][0].reshape(1, -1), f)
    sh["norm_final_g"] = np.ascontiguousarray(inp["norm_final_g"].reshape(1, -1), f)
    wr = np.concatenate([inp["w_route_group"][0], inp["w_route_expert"][0]], axis=1)
    sh["wr"] = np.ascontiguousarray(wr.reshape(16, 128, 36).transpose(1, 0, 2).reshape(128, 16 * 36), f)
    sh["br"] = np.ascontiguousarray(
        np.concatenate([inp["b_route_group"][0].reshape(-1), inp["b_route_expert"][0].reshape(-1)]).reshape(1, 36), f)
    sh["weg"] = np.ascontiguousarray(
        inp["w_exp_gate"][0].reshape(NEXP, 16, 128, DFF).transpose(0, 2, 1, 3).reshape(NEXP * 128 * 4, 2048), f)
    sh["weu"] = np.ascontiguousarray(
        inp["w_exp_up"][0].reshape(NEXP, 16, 128, DFF).transpose(0, 2, 1, 3).reshape(NEXP * 128 * 4, 2048), f)
    sh["wed"] = np.ascontiguousarray(
        inp["w_exp_down"][0].reshape(NEXP, 4, 128, D).transpose(0, 2, 1, 3).reshape(NEXP * 128 * 4, 2048), f)
    ea = np.zeros((16, 16 * 128), np.float32)
    for kb in range(16):
        ea[kb, kb * 128:(kb + 1) * 128] = 1.0
    sh["eall"] = ea
    sh["cpack2"] = _const_pack2()
    return sh


def _prep_core(inp, sh, c):
    b, j = c // 2, c % 2
    order = _local_order(j)
    xb = np.asarray(inp["x"][b], np.float32).reshape(16, 256, D)
    pb = np.asarray(inp["positions"][b], np.int32).reshape(16, 256)
    m = dict(sh)
    m["x"] = np.ascontiguousarray(xb[order].reshape(SEQ, D))
    m["pos"] = np.ascontiguousarray(pb[order].reshape(1, SEQ))
    m["cvec"] = np.ascontiguousarray(np.asarray(inp["c"][b], np.float32).reshape(16, 128).T)
    m["cpack"] = _const_pack(j)
    return m


_NC_CACHE = {}


def kernel(**inputs):
    inp = {k: np.asarray(v) for k, v in inputs.items()}
    sh = _prep_shared(inp)
    in_maps = [_prep_core(inp, sh, c) for c in range(8)]
    if "nc" not in _NC_CACHE:
        _NC_CACHE["nc"] = build()
    res = run_bass_kernel_spmd(_NC_CACHE["nc"], in_maps, core_ids=list(range(8)))
    outp = np.zeros((4, 16, 256, D), np.float32)
    for c in range(8):
        b, j = c // 2, c % 2
        o = np.asarray(res.results[c]["out"]).reshape(8, 256, D)
        for i in range(8):
            outp[b, 2 * i + j] = o[i]
    return outp.reshape(4, SEQ, D)
```

```python
import numpy as np
from contextlib import ExitStack
import concourse.bass as bass
import concourse.mybir as mybir
from concourse.bass_utils import run_bass_kernel_spmd
from concourse.alu_op_type import AluOpType as ALU

AF = mybir.ActivationFunctionType
AX = mybir.AxisListType
F32 = mybir.dt.float32
BF16 = mybir.dt.bfloat16
I32 = mybir.dt.int32
U32 = mybir.dt.uint32

D = 2048
SEQ = 4096
NOWN = 2048
NH = 16
DH = 128
EPS = 1e-6
BIG = 30000.0
NEXP = 32
DFF = 512
RB = 256
NBLK = 48
CAPROWS = NBLK * RB
PI = float(np.pi)
RELAX = False

CP_IDENT, CP_PM, CP_TRI01, CP_TRIPEN, CP_LSTRICT, CP_ONES = 0, 128, 256, 384, 512, 640
CP_INVF, CP_IOTAP, CP_SIGN = 768, 769, 770
CP_PASTPEN = 771
CP_PAST01 = CP_PASTPEN + 128
CP_W = CP_PAST01 + 128
C2_THR = 0
C2_BLKI = 1024
C2_W = 1024 + 2048


class Res:
    __slots__ = ("name", "w", "rs", "rd")

    def __init__(self, name):
        self.name = name
        self.w = None
        self.rs = {}
        self.rd = []


class Sched:
    ENGS = ("pe", "act", "dve", "pool", "sp")

    def __init__(self, nc, es, ndsem=12):
        self.nc = nc
        self.eng = {"pe": nc.tensor, "act": nc.scalar, "dve": nc.vector, "pool": nc.gpsimd, "sp": nc.sync}
        self.sem = {e: es.enter_context(nc.semaphore("sem_" + e)) for e in self.ENGS}
        self.cnt = {e: 0 for e in self.ENGS}
        self.seen = {e: {f: 0 for f in self.ENGS} for e in self.ENGS}
        self.dq = {}
        for q, nd in (("sp", 16), ("pool", 14)):
            sems = [es.enter_context(nc.semaphore(f"dsem_{q}{i}")) for i in range(nd)]
            self.dq[q] = {"sems": sems, "val": [0] * nd, "nxt": 0}
        self.seen_d = {e: {} for e in self.ENGS}

    def _wait(self, e, t):
        if t is None:
            return
        if t[0] == "dma":
            _, q, k, v = t
            key = (q, k)
            if self.seen_d[e].get(key, 0) >= v:
                return
            self.eng[e].wait_ge(self.dq[q]["sems"][k], v)
            self.seen_d[e][key] = v
        else:
            src, v = t
            if self.seen[e][src] >= v:
                return
            self.eng[e].wait_ge(self.sem[src], v)
            self.seen[e][src] = v

    def _deps(self, e, reads, writes):
        for r in reads:
            self._wait(e, r.w)
        for w in writes:
            if w.w is not None and not (w.w[0] == e and (RELAX or e == "pe")):
                self._wait(e, w.w)
            for src, v in w.rs.items():
                if src != e or not (RELAX or e == "pe"):
                    self._wait(e, (src, v))
            for t in w.rd:
                self._wait(e, t)

    def _mark(self, t, reads, writes):
        for r in reads:
            if t[0] == "dma":
                r.rd.append(t)
            else:
                if r.rs.get(t[0], 0) < t[1]:
                    r.rs[t[0]] = t[1]
        for w in writes:
            w.w = t
            w.rs = {}
            w.rd = []

    def op(self, e, fn, reads=(), writes=()):
        self._deps(e, reads, writes)
        inst = fn(self.eng[e])
        self.cnt[e] += 1
        inst.then_inc(self.sem[e], 1)
        t = (e, self.cnt[e])
        self.seen[e][e] = max(self.seen[e][e], 0)
        self._mark(t, reads, writes)
        return t

    def dma(self, q, out, in_, reads=(), writes=(), after=(), **kw):
        self._deps(q, reads, writes)
        for t_ in after:
            self._wait(q, t_)
        d = self.dq[q]
        k = d["nxt"]
        d["nxt"] = (k + 1) % len(d["sems"])
        if d["val"][k]:
            self._wait(q, ("dma", q, k, d["val"][k]))
        inst = self.eng[q].dma_start(out=out, in_=in_, **kw)
        d["val"][k] += 16
        inst.then_inc(d["sems"][k], 16)
        t = ("dma", q, k, d["val"][k])
        self._mark(t, reads, writes)
        return t

    def idma(self, fn, reads=(), writes=()):
        q = "pool"
        self._deps(q, reads, writes)
        d = self.dq[q]
        k = d["nxt"]
        d["nxt"] = (k + 1) % len(d["sems"])
        if d["val"][k]:
            self._wait(q, ("dma", q, k, d["val"][k]))
        inst = fn(self.eng[q])
        d["val"][k] += 16
        inst.then_inc(d["sems"][k], 16)
        t = ("dma", q, k, d["val"][k])
        self._mark(t, reads, writes)
        return t

    def barrier(self, engines=None):
        for e in (engines or self.ENGS):
            for f in self.ENGS:
                if f != e and self.cnt[f]:
                    self._wait(e, (f, self.cnt[f]))
            for q, d in self.dq.items():
                for k, v in enumerate(d["val"]):
                    if v:
                        self._wait(e, ("dma", q, k, v))


def dram_bcast(ap, n):
    return bass.AP(ap.tensor, ap.offset, [[0, 128], [1, n]])


def build(dbg=(), stop_after=None):
    nc = bass.Bass("TRN2", target_bir_lowering=False)
    dbg = set(dbg)

    def din(name, shape, dt=F32):
        return nc.dram_tensor(name, list(shape), dt, kind="ExternalInput").ap()

    def dscr(name, shape, dt):
        if name in dbg:
            return nc.dram_tensor(name, list(shape), dt, kind="ExternalOutput").ap()
        return nc.dram_tensor(name, list(shape), dt).ap()

    x = din("x", [SEQ, D])
    cvec = din("cvec", [128, 16])
    pos = din("pos", [1, SEQ], I32)
    w_ada = din("w_ada", [D, 6 * D])
    b_ada = din("b_ada", [1, 6 * D])
    gmix = din("gmix", [128, 16])
    w_in = din("w_in", [D, 6 * D])
    ln_g = din("sgu_ln_g", [1, 1024])
    ln_b = din("sgu_ln_b", [1, 1024])
    wsT_d = din("wsT", [128, 8 * 128])
    bs_d = din("sgu_b_s", [1, 8 * 128])
    w_sgu_out = din("w_sgu_out", [1024, D])
    w_moba_out = din("w_moba_out", [D, D])
    w_out = din("w_out", [D, D])
    nffn_g = din("norm_ffn_g", [1, D])
    nfin_g = din("norm_final_g", [1, D])
    wr_d = din("wr", [128, 16 * 36])
    br_d = din("br", [1, 36])
    weg = din("weg", [NEXP * 128 * 4, 2048])
    weu = din("weu", [NEXP * 128 * 4, 2048])
    wed = din("wed", [NEXP * 128 * 4, 2048])
    cpack = din("cpack", [128, CP_W])
    eall_d = din("eall", [16, 16 * 128])
    cpack2 = din("cpack2", [128, C2_W])

    out = nc.dram_tensor("out", [NOWN, D], F32, kind="ExternalOutput").ap()

    UT = dscr("UT", [16, 128, 8 * 128], BF16)
    VS = dscr("VS", [NOWN, 1024], BF16)
    QT = dscr("QT", [NH, 128, NOWN], BF16)
    KT = dscr("KT", [NH, 128, SEQ], BF16)
    VA = dscr("VA", [NH, 128, 32 * 128], BF16)
    GA = dscr("GA", [16, 128, NOWN], BF16)
    GB = dscr("GB", [16, 128, NOWN], BF16)
    MG = dscr("MG", [16, 128, NOWN], BF16)
    H1 = dscr("H1", [NOWN, D], F32)
    N2 = dscr("N2", [NOWN, D], BF16)
    XS = dscr("XS", [CAPROWS, D], BF16)
    YS = dscr("YS", [CAPROWS, D], BF16)
    DBG = dscr("DBG", [128, 4096], F32)
    WGB = dscr("WGB", [NEXP * 128 * 4, 2048], BF16)
    WUB = dscr("WUB", [NEXP * 128 * 4, 2048], BF16)
    WDB = dscr("WDB", [NEXP * 128 * 4, 2048], BF16)

    def dscr_out(name, shape, dt):
        return nc.dram_tensor(name, list(shape), dt, kind="ExternalOutput").ap()

    with ExitStack() as es:
        S = Sched(nc, es)

        uniq = [0]

        def sb(name, shape, dt, stack=es):
            uniq[0] += 1
            return stack.enter_context(nc.sbuf_tensor(f"{name}_{uniq[0]}", list(shape), dt))

        PB = [es.enter_context(nc.psum_tensor(f"pb{i}", [128, 512], F32)) for i in range(6)]
        PBr = [Res(f"pb{i}") for i in range(6)]
        PT = [es.enter_context(nc.psum_tensor(f"pt{i}", [128, 1024], BF16)) for i in range(2)]
        PTr = [Res(f"pt{i}") for i in range(2)]

        cp = sb("cp", [128, CP_W], F32)
        cpr = Res("cp")
        S.dma("sp", cp[:], cpack[:, :], writes=[cpr])
        identb = sb("identb", [128, 128], BF16)
        pmb = sb("pmb", [128, 128], BF16)
        tripenb = sb("tripenb", [128, 128], BF16)
        lstrb = sb("lstrb", [128, 128], BF16)
        onesb = sb("onesb", [128, 128], BF16)
        epsc = sb("epsc", [128, 1], F32)
        constr = Res("constb")
        for dst, off in ((identb, CP_IDENT), (pmb, CP_PM), (tripenb, CP_TRIPEN), (lstrb, CP_LSTRICT), (onesb, CP_ONES)):
            S.op("dve", lambda v, dst=dst, off=off: v.tensor_copy(out=dst[:], in_=cp[:, off:off + 128]),
                 reads=[cpr], writes=[constr])
        S.op("dve", lambda v: v.memset(epsc[:], EPS), writes=[constr])
        eallb = sb("eallb", [16, 2048], BF16)
        with ExitStack() as s0:
            eall_f = sb("eall_f", [16, 2048], F32, s0)
            er = Res("eall")
            S.dma("sp", eall_f[:], eall_d[:, :], writes=[er])
            S.op("dve", lambda v: v.tensor_copy(out=eallb[:], in_=eall_f[:]), reads=[er], writes=[constr])
            S.barrier()

        adaT = sb("adaT", [128, 96], F32)
        adaTr = Res("adaT")
        g1 = sb("g1", [128, 16], F32)
        g1r = Res("g1")
        gm_bc = sb("gm_bc", [128, D], F32)
        G2_bc = sb("G2_bc", [128, D], F32)
        shf_bc = sb("shf_bc", [128, D], F32)
        gf_bc = sb("gf_bc", [128, D], F32)
        bcr = Res("bc")

        with ExitStack() as sa:
            c_sb = sb("c_sb", [128, 16], F32, sa)
            c_act = sb("c_act", [128, 16], BF16, sa)
            ada_row = sb("ada_row", [1, 6 * D], F32, sa)
            bada = [sb(f"bada{i}", [1, 512], F32, sa) for i in range(2)]
            badar = [Res(f"bada{i}") for i in range(2)]
            wblk = [sb(f"wa{i}", [128, 16, 512], BF16, sa) for i in range(3)]
            wblr = [Res(f"wa{i}") for i in range(3)]
            one1 = sb("one1", [1, 128], F32, sa)
            cr, ar, br_ = Res("c"), Res("adarow"), Res("bada")
            S.dma("sp", c_sb[:], cvec[:, :], writes=[cr])
            S.op("act", lambda a: a.activation(out=c_act[:], in_=c_sb[:], func=AF.Silu), reads=[cr], writes=[cr])
            S.op("dve", lambda v: v.memset(one1[:], 1.0), writes=[ar])
            wav = w_ada.rearrange("(kc p) n -> p kc n", p=128)
            for nt in range(24):
                k = nt % 3
                S.dma("pool", wblk[k][:], wav[:, :, nt * 512:(nt + 1) * 512], writes=[wblr[k]])
                pbi = nt % 2
                S.dma("sp", bada[pbi][:], b_ada[0:1, nt * 512:(nt + 1) * 512], writes=[badar[pbi]])

                def mm(pe, k=k, pbi=pbi):
                    for kc in range(16):
                        i = pe.matmul(PB[pbi][0:1, :], lhsT=c_act[:, kc:kc + 1], rhs=wblk[k][:, kc, :],
                                      start=(kc == 0), stop=(kc == 15))
                    return i
                S.op("pe", mm, reads=[cr, wblr[k]], writes=[PBr[pbi]])
                S.op("dve", lambda v, nt=nt, pbi=pbi: v.tensor_tensor(
                    out=ada_row[0:1, nt * 512:(nt + 1) * 512], in0=PB[pbi][0:1, :],
                    in1=bada[pbi][0:1, :], op=ALU.add), reads=[PBr[pbi], badar[pbi]], writes=[ar])
            def mmT(pe):
                for j in range(96):
                    i = pe.matmul(PB[2][:, j:j + 1], lhsT=ada_row[0:1, j * 128:(j + 1) * 128], rhs=one1[0:1, 0:1],
                                  start=True, stop=True)
                return i
            S.op("pe", mmT, reads=[ar], writes=[PBr[2]])
            S.op("dve", lambda v: v.tensor_copy(out=adaT[:], in_=PB[2][:, 0:96]), reads=[PBr[2]], writes=[adaTr])
            gmx = sb("gmx", [128, 16], F32, sa)
            gr_ = Res("gmx")
            S.dma("sp", gmx[:], gmix[:, :], writes=[gr_])
            S.op("dve", lambda v: v.scalar_tensor_tensor(out=g1[:], in0=adaT[:, 16:32], scalar=1.0, in1=gmx[:],
                                                         op0=ALU.add, op1=ALU.mult), reads=[adaTr, gr_], writes=[g1r])
            S.dma("sp", G2_bc[:], dram_bcast(nffn_g[0:1, :], D), writes=[bcr])
            for which, dst in ((2, gm_bc), (3, shf_bc), (4, None), (5, gf_bc)):
                for q4 in range(4):
                    c0 = which * D + q4 * 512
                    pbi = 3 + (q4 % 2)
                    S.op("pe", lambda pe, c0=c0, pbi=pbi: pe.matmul(PB[pbi][:, :], lhsT=one1[0:1, :],
                                                                    rhs=ada_row[0:1, c0:c0 + 512], start=True, stop=True),
                         reads=[ar], writes=[PBr[pbi]])
                    if dst is not None:
                        S.op("dve", lambda v, dst=dst, q4=q4, pbi=pbi: v.tensor_copy(
                            out=dst[:, q4 * 512:(q4 + 1) * 512], in_=PB[pbi][:, :]), reads=[PBr[pbi]], writes=[bcr])
                    else:
                        S.op("dve", lambda v, q4=q4, pbi=pbi: v.scalar_tensor_tensor(
                            out=G2_bc[:, q4 * 512:(q4 + 1) * 512], in0=PB[pbi][:, :], scalar=1.0,
                            in1=G2_bc[:, q4 * 512:(q4 + 1) * 512], op0=ALU.add, op1=ALU.mult),
                            reads=[PBr[pbi], bcr], writes=[bcr])
            S.barrier()
        if "adaT" in dbg:
            S.dma("sp", DBG[:, 0:96], adaT[:], reads=[adaTr])
            S.dma("sp", DBG[:, 128:128 + 2048], G2_bc[:], reads=[bcr])
        if stop_after == "A":
            S.barrier()
            return nc

        kmT = sb("kmT", [128, 16, 16], F32)
        kmr = Res("kmT")
        d1f = sb("d1f", [128, 16], F32)
        d2f = sb("d2f", [128, 16], F32)
        w1_all = sb("w1_all", [128, 16], F32)
        w2_all = sb("w2_all", [128, 16], F32)
        idx4 = sb("idx4", [128, 256], I32)
        wallr, idxr, XSr, YSr = Res("wall"), Res("idx4"), Res("XS"), Res("YS")
        sNT = es.enter_context(ExitStack())
        NT = sb("NT", [128, 16, NOWN], BF16, sNT)
        NTr = Res("NT")
        NTr2 = Res("NT2")
        w_inv = w_in.rearrange("(kc p) n -> p kc n", p=128)

        def norm_stage(tok0):
            with ExitStack() as sb_:
                NB_ = 3
                xt = [sb(f"xt{i}", [128, D], F32, sb_) for i in range(NB_)]
                xs = [sb(f"xs{i}", [128, D], BF16, sb_) for i in range(2)]
                junk = sb("junk", [128, D], BF16, sb_)
                ssq = [sb(f"ssq{i}", [128, 1], F32, sb_) for i in range(NB_)]
                xr = [Res(f"xt{i}") for i in range(NB_)]
                xsr = [Res(f"xs{i}") for i in range(2)]
                sr = [Res(f"ssq{i}") for i in range(NB_)]
                jr = Res("junk")
                ntmp = [sb(f"ntmp{i}", [128, 8, 128], F32, sb_) for i in range(2)]
                ntr = [Res(f"ntmp{i}") for i in range(2)]

                def p1(tt):
                    k = tt % NB_
                    S.dma("sp", xt[k][:], x[tok0 + tt * 128: tok0 + (tt + 1) * 128, :], writes=[xr[k]])
                    S.op("act", lambda a, k=k: a.activation(out=junk[:], in_=xt[k][:], func=AF.Square, accum_out=ssq[k][:]),
                         reads=[xr[k]], writes=[jr, sr[k]])

                def p2(tt):
                    k = tt % NB_
                    k2 = tt % 2
                    S.op("act", lambda a, k=k: a.activation(out=ssq[k][:], in_=ssq[k][:], func=AF.Sqrt, scale=1.0 / D, bias=epsc[:, 0:1]),
                         reads=[sr[k], constr], writes=[sr[k]])
                    S.op("dve", lambda v, k=k: v.reciprocal(out=ssq[k][:], in_=ssq[k][:]), reads=[sr[k]], writes=[sr[k]])
                    S.op("act", lambda a, k=k, k2=k2: a.activation(out=xs[k2][:], in_=xt[k][:], func=AF.Copy, scale=ssq[k][:, 0:1]),
                         reads=[xr[k], sr[k]], writes=[xsr[k2]])

                def p3(tt):
                    k2 = tt % 2
                    for half in range(2):
                        def tr(pe, k2=k2, half=half):
                            for jj in range(8):
                                kc = half * 8 + jj
                                i = pe.transpose(out=PT[half][:, jj * 128:(jj + 1) * 128], in_=xs[k2][:, kc * 128:(kc + 1) * 128],
                                                 identity=identb[:])
                            return i
                        S.op("pe", tr, reads=[xsr[k2], constr], writes=[PTr[half]])
                        tb = (2 * tt + half) % 2
                        S.op("dve", lambda v, half=half, tb=tb: v.tensor_tensor(
                            out=ntmp[tb][:], in0=PT[half][:, :].rearrange("p (a b) -> p a b", b=128),
                            in1=g1[:, half * 8:(half + 1) * 8].unsqueeze(2).broadcast_to([128, 8, 128]), op=ALU.mult),
                            reads=[PTr[half], g1r], writes=[ntr[tb]])
                        S.op("pool", lambda g_, half=half, tb=tb, tt=tt: g_.tensor_tensor(
                            out=NT[:, half * 8:(half + 1) * 8, tt * 128:(tt + 1) * 128], in0=ntmp[tb][:],
                            in1=adaT[:, half * 8:(half + 1) * 8].unsqueeze(2).broadcast_to([128, 8, 128]), op=ALU.add),
                            reads=[ntr[tb], adaTr], writes=[NTr])

                p1(0)
                p1(1)
                p2(0)
                for tt in range(16):
                    if tt + 2 < 16:
                        p1(tt + 2)
                    if tt + 1 < 16:
                        p2(tt + 1)
                    p3(tt)
                S.barrier()

        def proj_stage(tok0, blocks):
            ntok = NOWN
            with ExitStack() as sc:
                wb = [sb(f"wb{i}", [128, 16, 512], BF16, sc) for i in range(3)]
                wbr = [Res(f"wb{i}") for i in range(3)]
                cosT = sb("cosT", [128, NOWN], F32, sc)
                sinT = sb("sinT", [128, NOWN], F32, sc)
                tabr = Res("tab")
                import os
                KN = os.environ.get("KNOB", "")
                need_tab = any(kd in ("q", "k") for kd, _ in blocks) or "forcetab" in KN
                with ExitStack() as st:
                    pi_ = sb("pos_i", [128, 512], I32, st)
                    ang = sb("ang", [128, 512], F32, st)
                    kf = sb("kf", [128, 512], F32, st)
                    ki = sb("ki", [128, 512], I32, st)
                    msk = sb("msk", [128, 512], F32, st)
                    r1, r2, r3, r4, r5 = Res("pos_i"), Res("ang"), Res("kf"), Res("ki"), Res("msk")
                    for tg in (range(4) if need_tab else ()):
                        t0 = tok0 + tg * 512
                        S.dma("sp", pi_[:], dram_bcast(pos[0:1, t0:t0 + 512], 512), writes=[r1])
                        for which, dst in ((0, sinT), (1, cosT)):
                            S.op("dve", lambda v: v.tensor_copy(out=ang[:], in_=pi_[:]), reads=[r1], writes=[r2])
                            S.op("dve", lambda v, which=which: v.tensor_scalar(
                                out=ang[:], in0=ang[:], scalar1=cp[:, CP_INVF:CP_INVF + 1], scalar2=which * PI / 2,
                                op0=ALU.mult, op1=ALU.add), reads=[r2, cpr], writes=[r2])
                            S.op("dve", lambda v: v.tensor_scalar(out=kf[:], in0=ang[:], scalar1=1.0 / (2 * PI), scalar2=None,
                                                                  op0=ALU.mult), reads=[r2], writes=[r3])
                            S.op("dve", lambda v: v.tensor_copy(out=ki[:], in_=kf[:]), reads=[r3], writes=[r4])
                            S.op("dve", lambda v: v.tensor_copy(out=kf[:], in_=ki[:]), reads=[r4], writes=[r3])
                            S.op("dve", lambda v: v.scalar_tensor_tensor(out=ang[:], in0=kf[:], scalar=-2 * PI, in1=ang[:],
                                                                         op0=ALU.mult, op1=ALU.add), reads=[r3, r2], writes=[r2])
                            S.op("dve", lambda v: v.tensor_scalar(out=msk[:], in0=ang[:], scalar1=PI, scalar2=-2 * PI,
                                                                  op0=ALU.is_gt, op1=ALU.mult), reads=[r2], writes=[r5])
                            S.op("dve", lambda v: v.tensor_tensor(out=ang[:], in0=ang[:], in1=msk[:], op=ALU.add),
                                 reads=[r2, r5], writes=[r2])
                            S.op("dve", lambda v: v.tensor_scalar(out=msk[:], in0=ang[:], scalar1=-PI, scalar2=2 * PI,
                                                                  op0=ALU.is_lt, op1=ALU.mult), reads=[r2], writes=[r5])
                            S.op("dve", lambda v: v.tensor_tensor(out=ang[:], in0=ang[:], in1=msk[:], op=ALU.add),
                                 reads=[r2, r5], writes=[r2])
                            S.op("dve", lambda v: v.tensor_scalar(out=ang[:], in0=ang[:], scalar1=-3.1415925, scalar2=3.1415925,
                                                                  op0=ALU.max, op1=ALU.min), reads=[r2], writes=[r2])
                            if which == 0:
                                S.op("act", lambda a, dst=dst, tg=tg: a.activation(out=dst[:, tg * 512:(tg + 1) * 512], in_=ang[:],
                                                                                 func=AF.Sin, scale=cp[:, CP_SIGN:CP_SIGN + 1]),
                                     reads=[r2, cpr], writes=[tabr])
                            else:
                                S.op("act", lambda a, dst=dst, tg=tg: a.activation(out=dst[:, tg * 512:(tg + 1) * 512], in_=ang[:],
                                                                                 func=AF.Sin), reads=[r2], writes=[tabr])
                    S.barrier()
                ev = [sb(f"ev{i}", [128, 512], BF16, sc) for i in range(3)]
                evr = [Res(f"ev{i}") for i in range(3)]
                qb = [sb(f"qb{i}", [128, 512], BF16, sc) for i in range(2)]
                qbr = [Res(f"qb{i}") for i in range(2)]
                t1 = [sb(f"t1{i}", [128, 512], F32, sc) for i in range(2)]
                t1r = [Res(f"t1{i}") for i in range(2)]
                t2 = [sb(f"t2{i}", [128, 512], F32, sc) for i in range(2)]
                t2r = [Res(f"t2{i}") for i in range(2)]
                evi = [0]
                rpi = [0]
                pbi = [0]
                def issue_w(bj):
                    S.dma("pool", wb[bj % 3][:], w_inv[:, :, blocks[bj][1]:blocks[bj][1] + 512], writes=[wbr[bj % 3]])

                issue_w(0)
                for bi, (kind, col0) in enumerate(blocks):
                    k = bi % 3
                    if bi + 1 < len(blocks):
                        issue_w(bi + 1)
                    if kind in ("vs", "va"):
                        for tt in range(16):
                            pb = pbi[0] % 6
                            pbi[0] += 1

                            def mm(pe, k=k, tt=tt, pb=pb):
                                for kc in range(16):
                                    i = pe.matmul(PB[pb][:, :], lhsT=NT[:, kc, tt * 128:(tt + 1) * 128], rhs=wb[k][:, kc, :],
                                                  start=(kc == 0), stop=(kc == 15))
                                return i
                            S.op("pe", mm, reads=[NTr, NTr2, wbr[k]], writes=[PBr[pb]])
                            e = evi[0] % 3
                            evi[0] += 1
                            if kind == "vs":
                                S.op("act", lambda a, e=e, pb=pb: a.activation(out=ev[e][:], in_=PB[pb][:, :], func=AF.Gelu_apprx_tanh),
                                     reads=[PBr[pb]], writes=[evr[e]])
                                c0 = col0 - 1024
                                S.dma("sp", VS[tt * 128:(tt + 1) * 128, c0:c0 + 512], ev[e][:], reads=[evr[e]])
                            else:
                                S.op("act", lambda a, e=e, pb=pb: a.activation(out=ev[e][:], in_=PB[pb][:, :], func=AF.Copy),
                                     reads=[PBr[pb]], writes=[evr[e]])
                                h0 = (col0 - 6144) // 128
                                tile_g = tok0 // 128 + tt
                                dst = VA.rearrange("h p (t d) -> p h t d", d=128)[:, h0:h0 + 4, tile_g, :]
                                S.dma("sp", dst, ev[e][:].rearrange("p (h d) -> p h d", d=128), reads=[evr[e]])
                        continue
                    for tg in range(4):
                        for nch in range(4):
                            pb = pbi[0] % 6
                            pbi[0] += 1

                            def mm(pe, k=k, tg=tg, nch=nch, pb=pb):
                                for kc in range(16):
                                    i = pe.matmul(PB[pb][:, :], lhsT=wb[k][:, kc, nch * 128:(nch + 1) * 128],
                                                  rhs=NT[:, kc, tg * 512:(tg + 1) * 512], start=(kc == 0), stop=(kc == 15))
                                return i
                            S.op("pe", mm, reads=[NTr, NTr2, wbr[k]], writes=[PBr[pb]])
                            e = evi[0] % 3
                            evi[0] += 1
                            tsl = slice(tg * 512, (tg + 1) * 512)
                            if kind == "u":
                                ch = col0 // 128 + nch
                                S.op("act", lambda a, e=e, pb=pb: a.activation(out=ev[e][:], in_=PB[pb][:, :], func=AF.Gelu_apprx_tanh),
                                     reads=[PBr[pb]], writes=[evr[e]])
                                dst = UT.rearrange("t p (c k) -> p t c k", k=128)[:, tg * 4:(tg + 1) * 4, ch, :]
                                S.dma("sp", dst, ev[e][:].rearrange("p (t k) -> p t k", k=128), reads=[evr[e]])
                            elif kind in ("ga", "gb"):
                                ch = (col0 - (8192 if kind == "ga" else 10240)) // 128 + nch
                                S.op("act", lambda a, e=e, pb=pb: a.activation(out=ev[e][:], in_=PB[pb][:, :], func=AF.Sigmoid),
                                     reads=[PBr[pb]], writes=[evr[e]])
                                S.dma("sp", (GA if kind == "ga" else GB)[ch, :, tsl], ev[e][:], reads=[evr[e]])
                            else:
                                h = (col0 - (2048 if kind == "q" else 4096)) // 128 + nch
                                r = rpi[0] % 2
                                rpi[0] += 1
                                S.op("dve", lambda v, r=r, pb=pb, tsl=tsl: v.tensor_tensor(out=t1[r][:], in0=PB[pb][:, :], in1=cosT[:, tsl],
                                                                                          op=ALU.mult), reads=[PBr[pb], tabr], writes=[t1r[r]])
                                S.op("dve", lambda v, r=r, pb=pb, tsl=tsl: v.tensor_tensor(out=t2[r][0:64, :], in0=PB[pb][64:128, :],
                                                                                          in1=sinT[0:64, tsl], op=ALU.mult),
                                     reads=[PBr[pb], tabr], writes=[t2r[r]])
                                S.op("dve", lambda v, r=r, pb=pb, tsl=tsl: v.tensor_tensor(out=t2[r][64:128, :], in0=PB[pb][0:64, :],
                                                                                          in1=sinT[64:128, tsl], op=ALU.mult),
                                     reads=[PBr[pb], tabr], writes=[t2r[r]])
                                S.op("pool", lambda g, r=r, e=e: g.tensor_tensor(out=ev[e][:], in0=t1[r][:], in1=t2[r][:], op=ALU.add),
                                     reads=[t1r[r], t2r[r]], writes=[evr[e]])
                                if kind == "q":
                                    S.dma("sp", QT[h, :, tsl], ev[e][:], reads=[evr[e]])
                                else:
                                    S.dma("sp", KT[h, :, tok0 + tg * 512: tok0 + (tg + 1) * 512], ev[e][:], reads=[evr[e]])
                                    kb0 = (tok0 + tg * 512) // 256
                                    S.op("dve", lambda v, e=e, h=h, kb0=kb0: v.tensor_reduce(
                                        out=kmT[:, h, kb0:kb0 + 2], in_=ev[e][:].rearrange("p (b k) -> p b k", k=256),
                                        axis=AX.X, op=ALU.add), reads=[evr[e]], writes=[kmr])
                S.barrier()

        own_blocks = ([("u", 0), ("u", 512), ("vs", 1024), ("vs", 1536)]
                      + [("q", 2048 + 512 * i) for i in range(4)] + [("k", 4096 + 512 * i) for i in range(4)]
                      + [("va", 6144 + 512 * i) for i in range(4)] + [("ga", 8192 + 512 * i) for i in range(4)]
                      + [("gb", 10240 + 512 * i) for i in range(4)])
        oth_blocks = [("k", 4096 + 512 * i) for i in range(4)] + [("va", 6144 + 512 * i) for i in range(4)]
        if stop_after and stop_after.startswith("C0"):
            allb = dict(u=("u", 0), vs=("vs", 1024), q=("q", 2048), k=("k", 4096), va=("va", 6144), ga=("ga", 8192))
            sel = stop_after.split(":")[1].split(",") if ":" in stop_after else list(allb)
            own_blocks = [allb[z] for z in sel]
            stop_after = "C0"
        norm_stage(0)
        if "NT" in dbg:
            NTd = dscr_out("NTd", [128, 16 * NOWN], BF16)
            S.dma("sp", NTd[:, :], NT[:].rearrange("p a b -> p (a b)"), reads=[NTr, NTr2])
        if stop_after == "B":
            S.barrier()
            return nc
        proj_stage(0, own_blocks)
        if stop_after in ("C0", "C1"):
            if "kmT" in dbg:
                S.dma("sp", DBG[:, 0:256], kmT[:].rearrange("p a b -> p (a b)"), reads=[kmr])
            S.barrier()
            return nc
        norm_stage(NOWN)
        proj_stage(NOWN, oth_blocks)
        if "kmT" in dbg:
            S.dma("sp", DBG[:, 0:256], kmT[:].rearrange("p a b -> p (a b)"), reads=[kmr])

        ST = sb("ST", [128, 8, NOWN], BF16, sNT)
        STr = Res("ST")
        with ExitStack() as sd:
            lng = sb("lng", [128, 1024], F32, sd)
            lnb = sb("lnb", [128, 1024], F32, sd)
            wsf = sb("wsf", [128, 1024], F32, sd)
            wsb = sb("wsb", [128, 8, 128], BF16, sd)
            bsf = sb("bsf", [1, 1024], F32, sd)
            bsh = sb("bsh", [1, 1024], BF16, sd)
            bsl = sb("bsl", [1, 1024], BF16, sd)
            bst = sb("bst", [1, 1024], F32, sd)
            dcr = Res("dconst")
            S.dma("sp", lng[:], dram_bcast(ln_g[0:1, :], 1024), writes=[dcr])
            S.dma("sp", lnb[:], dram_bcast(ln_b[0:1, :], 1024), writes=[dcr])
            S.dma("sp", wsf[:], wsT_d[:, :], writes=[dcr])
            S.dma("sp", bsf[:], bs_d[:, :], writes=[dcr])
            for g in range(8):
                S.op("dve", lambda v, g=g: v.tensor_tensor(out=wsb[:, g, :], in0=wsf[:, g * 128:(g + 1) * 128],
                                                           in1=cp[:, CP_TRI01:CP_TRI01 + 128], op=ALU.mult),
                     reads=[dcr, cpr], writes=[dcr])
            S.op("dve", lambda v: v.tensor_copy(out=bsh[:], in_=bsf[:]), reads=[dcr], writes=[dcr])
            S.op("dve", lambda v: v.tensor_tensor(out=bst[:], in0=bsf[:], in1=bsh[:], op=ALU.subtract), reads=[dcr], writes=[dcr])
            S.op("dve", lambda v: v.tensor_copy(out=bsl[:], in_=bst[:]), reads=[dcr], writes=[dcr])
            vt = [sb(f"vt{i}", [128, 1024], BF16, sd) for i in range(2)]
            ut = [sb(f"ut{i}", [128, 8, 128], BF16, sd) for i in range(2)]
            vtr = [Res(f"vt{i}") for i in range(2)]
            utr = [Res(f"ut{i}") for i in range(2)]
            st6 = sb("st6", [128, 12], F32, sd)
            mv = sb("mv", [128, 2], F32, sd)
            rs_ = sb("rs_", [128, 1], F32, sd)
            vn = sb("vn", [128, 1024], F32, sd)
            vln = [sb(f"vln{i}", [128, 1024], BF16, sd) for i in range(2)]
            smr, vnr = Res("dsmall"), Res("vn")
            vlr = [Res(f"vln{i}") for i in range(2)]
            for tt in range(16):
                k = tt % 2
                S.dma("sp", vt[k][:], VS[tt * 128:(tt + 1) * 128, :], writes=[vtr[k]])
                S.dma("sp", ut[k][:].rearrange("p c k -> p (c k)"), UT[tt, :, :], writes=[utr[k]])
                S.op("dve", lambda v, k=k: v.bn_stats(out=st6[:, 0:6], in_=vt[k][:, 0:512]), reads=[vtr[k]], writes=[smr])
                S.op("dve", lambda v, k=k: v.bn_stats(out=st6[:, 6:12], in_=vt[k][:, 512:1024]), reads=[vtr[k]], writes=[smr])
                S.op("dve", lambda v: v.bn_aggr(out=mv[:], in_=st6[:]), reads=[smr], writes=[smr])
                S.op("dve", lambda v: v.tensor_scalar(out=rs_[:], in0=mv[:, 1:2], scalar1=EPS, scalar2=None, op0=ALU.add),
                     reads=[smr], writes=[smr])
                S.op("act", lambda a: a.activation(out=rs_[:], in_=rs_[:], func=AF.Sqrt), reads=[smr], writes=[smr])
                S.op("dve", lambda v: v.reciprocal(out=rs_[:], in_=rs_[:]), reads=[smr], writes=[smr])
                S.op("dve", lambda v, k=k: v.tensor_scalar(out=vn[:], in0=vt[k][:], scalar1=mv[:, 0:1], scalar2=rs_[:, 0:1],
                                                            op0=ALU.subtract, op1=ALU.mult), reads=[vtr[k], smr], writes=[vnr])
                S.op("dve", lambda v: v.tensor_tensor(out=vn[:], in0=vn[:], in1=lng[:], op=ALU.mult), reads=[vnr, dcr], writes=[vnr])
                S.op("pool", lambda g_, k=k: g_.tensor_tensor(out=vln[k][:], in0=vn[:], in1=lnb[:], op=ALU.add),
                     reads=[vnr, dcr], writes=[vlr[k]])
                for half in range(2):
                    pb = half

                    def mm(pe, k=k, half=half, pb=pb):
                        for gg in range(4):
                            g = half * 4 + gg
                            o = PB[pb][:, gg * 128:(gg + 1) * 128]
                            pe.matmul(o, lhsT=vln[k][:, g * 128:(g + 1) * 128], rhs=wsb[:, g, :], start=True, stop=False)
                            pe.matmul(o, lhsT=onesb[0:1, :], rhs=bsh[0:1, g * 128:(g + 1) * 128], start=False, stop=False)
                            i = pe.matmul(o, lhsT=onesb[0:1, :], rhs=bsl[0:1, g * 128:(g + 1) * 128], start=False, stop=True)
                        return i
                    S.op("pe", mm, reads=[vlr[k], dcr, constr], writes=[PBr[pb]])
                    S.op("dve", lambda v, k=k, half=half, pb=pb, tt=tt: v.tensor_tensor(
                        out=ST[:, half * 4:half * 4 + 4, tt * 128:(tt + 1) * 128],
                        in0=PB[pb][:, :].rearrange("p (g i) -> p g i", i=128), in1=ut[k][:, half * 4:half * 4 + 4, :], op=ALU.mult),
                        reads=[PBr[pb], utr[k]], writes=[STr])
            S.barrier()
        if "ST" in dbg:
            STd = dscr_out("STd", [128, 8 * NOWN], BF16)
            S.dma("sp", STd[:, :], ST[:].rearrange("p a b -> p (a b)"), reads=[STr])
        if stop_after == "D":
            S.barrier()
            return nc

        AT, ATr = NT, Res("AT")
        SCALE = float(DH) ** -0.5
        with ExitStack() as se:
            kt = [sb(f"kt{i}", [128, SEQ], BF16, se) for i in range(2)]
            va = [sb(f"va{i}", [128, 32, 132], BF16, se) for i in range(2)]
            qt = [sb(f"qt{i}", [128, NOWN], BF16, se) for i in range(2)]
            hr = [Res(f"hd{i}") for i in range(2)]
            hkr = [Res(f"hk{i}") for i in range(2)]
            hqr = [Res(f"hq{i}") for i in range(2)]
            kmb = [sb(f"kmb{i}", [128, 16], BF16, se) for i in range(2)]
            kmbr = [Res(f"kmb{i}") for i in range(2)]
            for i in range(2):
                S.op("dve", lambda v, i=i: v.memset(va[i][:, :, 128:129], 1.0), writes=[hr[i]])
            gm = sb("gm", [128, 2, 16], F32, se)
            m8 = sb("m8", [128, 2, 8], F32, se)
            selt = sb("selt", [128, 2, 16], F32, se)
            Mq = sb("Mq", [128, 2, 16], BF16, se)
            MT = [sb(f"MT{i}", [16, 256], BF16, se) for i in range(2)]
            gsr = Res("gsmall")
            Mqr = Res("Mq")
            MTr = [Res(f"MT{i}") for i in range(2)]
            pt_ = [sb(f"pexp{i}", [128, 256], BF16, se) for i in range(4)]
            ptr = [Res(f"pexp{i}") for i in range(4)]
            rden = sb("rden", [128, 2], F32, se)
            atn = [sb(f"atn{i}", [128, 128], BF16, se) for i in range(2)]
            atr = [Res(f"atn{i}") for i in range(2)]
            rdr = Res("rden")
            sidx = [0]
            pidx = [0]
            units = [(h, i) for h in range(NH) for i in range(8)]

            def load_head(h):
                hb = h % 2
                S.dma("sp", qt[hb][:], QT[h, :, :], writes=[hqr[hb]])
                S.dma("sp", kt[hb][:], KT[h, :, :], writes=[hkr[hb]])
                S.dma("sp", va[hb][:, :, 0:128], VA[h, :, :].rearrange("p (t d) -> p t d", d=128), writes=[hr[hb]])
                S.op("dve", lambda v, hb=hb, h=h: v.tensor_copy(out=kmb[hb][:], in_=kmT[:, h, :]), reads=[kmr], writes=[kmbr[hb]])

            def prep_a(u):
                h, i = units[u]
                hb = h % 2

                def gmm(pe):
                    for q2 in range(2):
                        q0 = i * 256 + q2 * 128
                        ins = pe.matmul(PB[5][:, q2 * 16:(q2 + 1) * 16], lhsT=qt[hb][:, q0:q0 + 128], rhs=kmb[hb][:, :],
                                        start=True, stop=True)
                    return ins
                S.op("pe", gmm, reads=[hqr[hb], kmbr[hb]], writes=[PBr[5]])
                for q2 in range(2):
                    S.op("dve", lambda v, q2=q2: v.tensor_tensor(
                        out=gm[:, q2, :], in0=PB[5][:, q2 * 16:(q2 + 1) * 16],
                        in1=cp[:, CP_PASTPEN + i * 16:CP_PASTPEN + (i + 1) * 16], op=ALU.add), reads=[PBr[5], cpr], writes=[gsr])
                    S.op("dve", lambda v, q2=q2: v.max(out=m8[:, q2, :], in_=gm[:, q2, :]), reads=[gsr], writes=[gsr])
                    S.op("dve", lambda v, q2=q2: v.tensor_scalar(out=selt[:, q2, :], in0=gm[:, q2, :], scalar1=m8[:, q2, 2:3],
                                                                  scalar2=None, op0=ALU.is_ge), reads=[gsr], writes=[gsr])
                    S.op("dve", lambda v, q2=q2: v.tensor_tensor(
                        out=selt[:, q2, :], in0=selt[:, q2, :], in1=cp[:, CP_PAST01 + i * 16:CP_PAST01 + (i + 1) * 16],
                        op=ALU.mult), reads=[gsr, cpr], writes=[gsr])
                    S.op("dve", lambda v, q2=q2: v.tensor_scalar(out=Mq[:, q2, :], in0=selt[:, q2, :], scalar1=-1.0, scalar2=BIG,
                                                                  op0=ALU.add, op1=ALU.mult), reads=[gsr], writes=[Mqr])

            def prep_b(u):
                mb = u % 2

                def mtr(pe):
                    for q2 in range(2):
                        ins = pe.transpose(out=PT[0][0:16, q2 * 128:(q2 + 1) * 128], in_=Mq[:, q2, :], identity=identb[:])
                    return ins
                S.op("pe", mtr, reads=[Mqr, constr], writes=[PTr[0]])
                S.op("act", lambda a: a.activation(out=MT[mb][:], in_=PT[0][0:16, 0:256], func=AF.Copy),
                     reads=[PTr[0]], writes=[MTr[mb]])

            load_head(0)
            prep_a(0)
            prep_b(0)
            zt_ = sb("zfill", [128, 8192], BF16, se)
            zr_ = Res("zfill")
            S.op("pool", lambda g_: g_.memset(zt_[:], 0.0), writes=[zr_])
            XSz = XS.rearrange("(c p r) d -> c p (r d)", p=128, r=4)
            bg = []
            for (dst_, src_) in ((WGB, weg), (WUB, weu), (WDB, wed)):
                for c_ in range(32):
                    bg.append((dst_[c_ * 512:(c_ + 1) * 512, :], src_[c_ * 512:(c_ + 1) * 512, :], []))
            for c_ in range(CAPROWS // 512):
                bg.append((XSz[c_, :, :], zt_[:], [zr_]))
            LAG = 2
            for u, (h, i) in enumerate(units):
                hb = h % 2
                mb = u % 2
                if i == 0 and h + 1 < NH:
                    load_head(h + 1)
                if u + 1 < len(units):
                    prep_a(u + 1)
                tiles = []
                for kbl in list(range(0, i)) + list(range(8, 8 + i + 1)):
                    for c in range(2):
                        tiles.append(("past", kbl, c))
                tiles += [("d00", i, 0), ("d01", i, 0), ("d11", i, 1)]
                nt0 = sum(1 for t_ in tiles if t_[0] in ("past", "d00"))
                nt1 = sum(1 for t_ in tiles if t_[0] in ("past", "d01", "d11"))
                c0 = [0]
                c1 = [0]
                pend = []

                def emit_pv(pk, ktile, qlist):
                    def pvm(pe):
                        for (qq_, off) in qlist:
                            cnt_, tot = (c0, nt0) if qq_ == 0 else (c1, nt1)
                            ins = pe.matmul(PB[3 + qq_][:, 0:129], lhsT=pt_[pk][:, off:off + 128], rhs=va[hb][:, ktile, 0:129],
                                            start=(cnt_[0] == 0), stop=(cnt_[0] == tot - 1))
                            cnt_[0] += 1
                        return ins
                    S.op("pe", pvm, reads=[ptr[pk], hr[hb]], writes=[PBr[3], PBr[4]])

                prepb_at = min(len(tiles) - 1, max(2, len(tiles) // 2))
                for ti, (typ, kbl, c) in enumerate(tiles):
                    sbk = sidx[0] % 3
                    sidx[0] += 1
                    pk = pidx[0] % 4
                    pidx[0] += 1
                    k0 = kbl * 256 + c * 128
                    ktile = kbl * 2 + c
                    if typ == "past":
                        def smm(pe, k0=k0, kbl=kbl, sbk=sbk):
                            pe.matmul(PB[sbk][:, 0:256], lhsT=kt[hb][:, k0:k0 + 128], rhs=qt[hb][:, i * 256:(i + 1) * 256],
                                      start=True, stop=False)
                            return pe.matmul(PB[sbk][:, 0:256], lhsT=eallb[0:16, kbl * 128:(kbl + 1) * 128], rhs=MT[mb][0:16, :],
                                             start=False, stop=True)
                        S.op("pe", smm, reads=[hkr[hb], hqr[hb], MTr[mb], constr], writes=[PBr[sbk]])
                        S.op("act", lambda a, sbk=sbk, pk=pk: a.activation(out=pt_[pk][:, :], in_=PB[sbk][:, 0:256], func=AF.Exp,
                                                                          scale=SCALE), reads=[PBr[sbk]], writes=[ptr[pk]])
                        qlist = ((0, 0), (1, 128))
                    else:
                        qq = 0 if typ == "d00" else 1
                        q0 = i * 256 + qq * 128
                        tri = typ in ("d00", "d11")

                        def smm(pe, k0=k0, q0=q0, sbk=sbk, tri=tri):
                            ins = pe.matmul(PB[sbk][:, 0:128], lhsT=kt[hb][:, k0:k0 + 128], rhs=qt[hb][:, q0:q0 + 128],
                                            start=True, stop=not tri)
                            if tri:
                                ins = pe.matmul(PB[sbk][:, 0:128], lhsT=identb[:], rhs=tripenb[:], start=False, stop=True)
                            return ins
                        S.op("pe", smm, reads=[hkr[hb], hqr[hb], constr], writes=[PBr[sbk]])
                        S.op("act", lambda a, sbk=sbk, pk=pk: a.activation(out=pt_[pk][:, 0:128], in_=PB[sbk][:, 0:128], func=AF.Exp,
                                                                          scale=SCALE), reads=[PBr[sbk]], writes=[ptr[pk]])
                        qlist = ((qq, 0),)
                    pend.append((pk, ktile, qlist))
                    if len(pend) > LAG:
                        emit_pv(*pend.pop(0))
                    if ti == prepb_at and u + 1 < len(units):
                        prep_b(u + 1)
                while pend:
                    emit_pv(*pend.pop(0))
                for q2 in range(2):
                    S.op("dve", lambda v, q2=q2: v.reciprocal(out=rden[:, q2:q2 + 1], in_=PB[3 + q2][:, 128:129]),
                         reads=[PBr[3 + q2]], writes=[rdr])
                    S.op("dve", lambda v, q2=q2: v.tensor_scalar(out=atn[q2][:], in0=PB[3 + q2][:, 0:128], scalar1=rden[:, q2:q2 + 1],
                                                                  scalar2=None, op0=ALU.mult), reads=[PBr[3 + q2], rdr], writes=[atr[q2]])
                    S.op("pe", lambda pe, q2=q2: pe.transpose(out=PT[1][:, q2 * 128:(q2 + 1) * 128], in_=atn[q2][:], identity=identb[:]),
                         reads=[atr[q2], constr], writes=[PTr[1]])
                S.op("act", lambda a, h=h, i=i: a.activation(out=AT[:, h, i * 256:(i + 1) * 256], in_=PT[1][:, 0:256], func=AF.Copy),
                     reads=[PTr[1]], writes=[ATr])
                if bg:
                    o_, i_, rd_ = bg.pop(0)
                    S.dma("pool", o_, i_, reads=rd_, after=[ATr.w])
            while bg:
                o_, i_, rd_ = bg.pop(0)
                S.dma("pool", o_, i_, reads=rd_)
            S.barrier()
        if "AT" in dbg:
            ATd = dscr_out("ATd", [128, 16 * NOWN], BF16)
            S.dma("sp", ATd[:, :], AT[:].rearrange("p a b -> p (a b)"), reads=[ATr])
        if stop_after == "E":
            S.barrier()
            return nc
        wso_v = w_sgu_out.rearrange("(kc p) n -> p kc n", p=128)
        wmo_v = w_moba_out.rearrange("(kc p) n -> p kc n", p=128)
        wo_v = w_out.rearrange("(kc p) n -> p kc n", p=128)
        with ExitStack() as sf:
            wso = [sb(f"wso{i}", [128, 8, 512], BF16, sf) for i in range(2)]
            wmo = [sb(f"wmo{i}", [128, 16, 512], BF16, sf) for i in range(2)]
            wfr = [Res(f"wf{i}") for i in range(2)]
            wfr2 = [Res(f"wfb{i}") for i in range(2)]
            gat = [sb(f"gat{i}", [128, 512], BF16, sf) for i in range(2)]
            gbt = [sb(f"gbt{i}", [128, 512], BF16, sf) for i in range(2)]
            ggr = [Res(f"gg{i}") for i in range(2)]
            m1 = [sb(f"m1{i}", [128, 512], F32, sf) for i in range(2)]
            m2 = [sb(f"m2{i}", [128, 512], F32, sf) for i in range(2)]
            mr = [Res(f"m{i}") for i in range(2)]
            mgt = [sb(f"mgt{i}", [128, 512], BF16, sf) for i in range(2)]
            mgr = [Res(f"mgt{i}") for i in range(2)]
            it = 0
            def issue_f(nb_):
                S.dma("pool", wso[nb_ % 2][:], wso_v[:, :, nb_ * 512:(nb_ + 1) * 512], writes=[wfr[nb_ % 2]])
                S.dma("pool", wmo[nb_ % 2][:], wmo_v[:, :, nb_ * 512:(nb_ + 1) * 512], writes=[wfr2[nb_ % 2]])

            issue_f(0)
            for nb in range(4):
                wk = nb % 2
                if nb + 1 < 4:
                    issue_f(nb + 1)
                for tg in range(4):
                    tsl = slice(tg * 512, (tg + 1) * 512)
                    for nch in range(4):
                        k = it % 2
                        it += 1
                        ch = nb * 4 + nch
                        pa, pb2 = 2 * ((it - 1) % 3), 2 * ((it - 1) % 3) + 1
                        S.dma("sp", gat[k][:], GA[ch, :, tsl], writes=[ggr[k]])
                        S.dma("sp", gbt[k][:], GB[ch, :, tsl], writes=[ggr[k]])

                        def mma(pe, wk=wk, nch=nch, tsl=tsl, pa=pa):
                            for kc in range(8):
                                ins = pe.matmul(PB[pa][:, :], lhsT=wso[wk][:, kc, nch * 128:(nch + 1) * 128], rhs=ST[:, kc, tsl],
                                                start=(kc == 0), stop=(kc == 7))
                            return ins

                        def mmb(pe, wk=wk, nch=nch, tsl=tsl, pb2=pb2):
                            for kc in range(16):
                                ins = pe.matmul(PB[pb2][:, :], lhsT=wmo[wk][:, kc, nch * 128:(nch + 1) * 128], rhs=AT[:, kc, tsl],
                                                start=(kc == 0), stop=(kc == 15))
                            return ins
                        S.op("pe", mma, reads=[wfr[wk], STr], writes=[PBr[pa]])
                        S.op("pe", mmb, reads=[wfr2[wk], ATr], writes=[PBr[pb2]])
                        S.op("dve", lambda v, k=k, pa=pa: v.tensor_tensor(out=m1[k][:], in0=PB[pa][:, :], in1=gat[k][:], op=ALU.mult),
                             reads=[PBr[pa], ggr[k]], writes=[mr[k]])
                        S.op("dve", lambda v, k=k, pb2=pb2: v.tensor_tensor(out=m2[k][:], in0=PB[pb2][:, :], in1=gbt[k][:], op=ALU.mult),
                             reads=[PBr[pb2], ggr[k]], writes=[mr[k]])
                        S.op("pool", lambda g_, k=k: g_.tensor_tensor(out=mgt[k][:], in0=m1[k][:], in1=m2[k][:], op=ALU.add),
                             reads=[mr[k]], writes=[mgr[k]])
                        S.dma("pool", MG[ch, :, tsl], mgt[k][:], reads=[mgr[k]])
            S.barrier()
        MGs, MGr = NT, [Res(f"MGs{i}") for i in range(16)]
        with ExitStack() as sg:
            for ch in range(16):
                S.dma("sp", MGs[:, ch, :], MG[ch, :, :], writes=[MGr[ch]])
            wo = [sb(f"wo{i}", [128, 16, 512], BF16, sg) for i in range(2)]
            wor = [Res(f"wo{i}") for i in range(2)]
            xp = [sb(f"xp{i}", [128, 512], F32, sg) for i in range(2)]
            xpr = [Res(f"xp{i}") for i in range(2)]
            hp = [sb(f"hp{i}", [128, 512], F32, sg) for i in range(2)]
            hpr = [Res(f"hp{i}") for i in range(2)]
            ho = [sb(f"ho{i}", [128, 512], F32, sg) for i in range(2)]
            hor = [Res(f"ho{i}") for i in range(2)]
            it = 0
            def issue_g(nb_):
                S.dma("pool", wo[nb_ % 2][:], wo_v[:, :, nb_ * 512:(nb_ + 1) * 512], writes=[wor[nb_ % 2]])

            issue_g(0)
            for nb in range(4):
                wk = nb % 2
                csl = slice(nb * 512, (nb + 1) * 512)
                if nb + 1 < 4:
                    issue_g(nb + 1)
                for tt in range(16):
                    k = it % 2
                    pb = it % 6
                    it += 1
                    S.dma("sp", xp[k][:], x[tt * 128:(tt + 1) * 128, csl], writes=[xpr[k]])

                    def mmo(pe, wk=wk, tt=tt, pb=pb):
                        for kc in range(16):
                            ins = pe.matmul(PB[pb][:, :], lhsT=MGs[:, kc, tt * 128:(tt + 1) * 128], rhs=wo[wk][:, kc, :],
                                            start=(kc == 0), stop=(kc == 15))
                        return ins
                    S.op("pe", mmo, reads=MGr + [wor[wk]], writes=[PBr[pb]])
                    S.op("dve", lambda v, k=k, pb=pb, csl=csl: v.tensor_tensor(out=hp[k][:], in0=PB[pb][:, :], in1=gm_bc[:, csl], op=ALU.mult),
                         reads=[PBr[pb], bcr], writes=[hpr[k]])
                    S.op("pool", lambda g_, k=k: g_.tensor_tensor(out=ho[k][:], in0=hp[k][:], in1=xp[k][:], op=ALU.add),
                         reads=[hpr[k], xpr[k]], writes=[hor[k]])
                    S.dma("pool", H1[tt * 128:(tt + 1) * 128, csl], ho[k][:], reads=[hor[k]])
            S.barrier()
        if stop_after == "G":
            return nc
        n2b_all, n2r = NT, Res("n2b")
        weg_v, weu_v, wed_v = WGB, WUB, WDB
        with ExitStack() as sh:
            c2 = sb("c2", [128, C2_W], F32, sh)
            c2r = Res("c2")
            S.dma("sp", c2[:], cpack2[:, :], writes=[c2r])
            wrf = sb("wrf", [128, 16, 36], F32, sh)
            brb = sb("brb", [128, 36], F32, sh)
            S.dma("sp", wrf[:].rearrange("p a b -> p (a b)"), wr_d[:, :], writes=[c2r])
            S.dma("sp", brb[:], dram_bcast(br_d[0:1, :], 36), writes=[c2r])
            oh1_all = sb("oh1", [128, 16, 32], F32, sh)
            oh2_all = sb("oh2", [128, 16, 32], F32, sh)
            cnt_all = sb("cnta", [128, 16, 32], BF16, sh)
            allr = Res("routeall")
            lg_all = sb("lg_all", [128, 16, 36], F32, sh)
            gmx16 = sb("gmx16", [128, 16], F32, sh)
            ohg16 = sb("ohg16", [128, 16, 4], F32, sh)
            d416 = sb("d416", [128, 16, 4], F32, sh)
            pg16 = sb("pg16", [128, 16], F32, sh)
            elm16 = sb("elm16", [128, 16, 32], F32, sh)
            m816 = sb("m816", [128, 16, 8], F32, sh)
            e16 = sb("e16", [128, 16], F32, sh)
            sm = sb("sm", [128, 64], F32, sh)
            elm = sb("elm", [128, 32], F32, sh)
            m8r = sb("m8r", [128, 8], F32, sh)
            ex4 = sb("ex4", [128, 4], F32, sh)
            rr = Res("rsmall")
            sh1 = sh.enter_context(ExitStack())
            h1t = [sb(f"h1t{i}", [128, D], F32, sh1) for i in range(2)]
            h1r = [Res(f"h1t{i}") for i in range(2)]
            n2f = sb("n2f", [128, D], F32, sh1)
            n2fr = Res("n2f")
            junk = sb("hjunk", [128, D], BF16, sh1)
            jr = Res("hjunk")
            ssq = sb("hssq", [128, 1], F32, sh1)
            sr = Res("hssq")
            n2T = sb("n2T", [128, 16, 128], F32, sh1)
            n2Tr = Res("n2T")
            for tt in range(16):
                k = tt % 2
                S.dma("sp", h1t[k][:], H1[tt * 128:(tt + 1) * 128, :], writes=[h1r[k]])
                S.op("act", lambda a, k=k: a.activation(out=junk[:], in_=h1t[k][:], func=AF.Square, accum_out=ssq[:]),
                     reads=[h1r[k]], writes=[jr, sr])
                S.op("act", lambda a: a.activation(out=ssq[:], in_=ssq[:], func=AF.Sqrt, scale=1.0 / D, bias=epsc[:, 0:1]),
                     reads=[sr, constr], writes=[sr])
                S.op("dve", lambda v: v.reciprocal(out=ssq[:], in_=ssq[:]), reads=[sr], writes=[sr])
                S.op("dve", lambda v, k=k: v.scalar_tensor_tensor(out=n2f[:], in0=h1t[k][:], scalar=ssq[:, 0:1], in1=G2_bc[:],
                                                                   op0=ALU.mult, op1=ALU.mult), reads=[h1r[k], sr, bcr], writes=[n2fr])
                S.op("pool", lambda g_: g_.tensor_tensor(out=n2f[:], in0=n2f[:], in1=shf_bc[:], op=ALU.add), reads=[n2fr, bcr], writes=[n2fr])
                S.op("act", lambda a, tt=tt: a.activation(out=n2b_all[:, tt, :], in_=n2f[:], func=AF.Copy), reads=[n2fr], writes=[n2r])
                for b4 in range(4):
                    def trf(pe, b4=b4):
                        for jj in range(4):
                            kc = b4 * 4 + jj
                            ins = pe.transpose(out=PB[b4][:, jj * 128:(jj + 1) * 128], in_=n2f[:, kc * 128:(kc + 1) * 128],
                                               identity=cp[:, CP_IDENT:CP_IDENT + 128])
                        return ins
                    S.op("pe", trf, reads=[n2fr, cpr], writes=[PBr[b4]])
                    if b4 % 2 == 0:
                        S.op("dve", lambda v, b4=b4: v.tensor_copy(out=n2T[:, b4 * 4:(b4 + 1) * 4, :].rearrange("p a b -> p (a b)"),
                                                                   in_=PB[b4][:, :]), reads=[PBr[b4]], writes=[n2Tr])
                    else:
                        S.op("act", lambda a, b4=b4: a.activation(out=n2T[:, b4 * 4:(b4 + 1) * 4, :].rearrange("p a b -> p (a b)"),
                                                                  in_=PB[b4][:, :], func=AF.Copy), reads=[PBr[b4]], writes=[n2Tr])

                def lgm(pe):
                    for kc in range(16):
                        ins = pe.matmul(PB[4][:, 0:36], lhsT=n2T[:, kc, :], rhs=wrf[:, kc, :], start=(kc == 0), stop=(kc == 15))
                    return ins
                S.op("pe", lgm, reads=[n2Tr, c2r], writes=[PBr[4]])
                S.op("dve", lambda v, tt=tt: v.tensor_tensor(out=lg_all[:, tt, :], in0=PB[4][:, 0:36], in1=brb[:], op=ALU.add),
                     reads=[PBr[4], c2r], writes=[rr])
            gl = lg_all[:, :, 0:4]
            S.op("dve", lambda v: v.tensor_reduce(out=gmx16[:], in_=gl, axis=AX.X, op=ALU.max), reads=[rr], writes=[rr])
            S.op("dve", lambda v: v.tensor_tensor(out=ohg16[:], in0=gl, in1=gmx16[:].unsqueeze(2).broadcast_to([128, 16, 4]), op=ALU.is_ge),
                 reads=[rr], writes=[rr])
            S.op("dve", lambda v: v.tensor_tensor(out=d416[:], in0=gl, in1=gmx16[:].unsqueeze(2).broadcast_to([128, 16, 4]), op=ALU.subtract),
                 reads=[rr], writes=[rr])
            S.op("act", lambda a: a.activation(out=d416[:], in_=d416[:], func=AF.Exp), reads=[rr], writes=[rr])
            S.op("dve", lambda v: v.tensor_reduce(out=pg16[:], in_=d416[:], axis=AX.X, op=ALU.add), reads=[rr], writes=[rr])
            S.op("dve", lambda v: v.reciprocal(out=pg16[:], in_=pg16[:]), reads=[rr], writes=[rr])
            S.op("dve", lambda v: v.tensor_scalar(out=ohg16[:], in0=ohg16[:], scalar1=-1.0, scalar2=1e9, op0=ALU.add, op1=ALU.mult),
                 reads=[rr], writes=[rr])
            S.op("dve", lambda v: v.tensor_tensor(out=elm16[:].rearrange("p t (g e) -> p t g e", e=8),
                                                  in0=lg_all[:, :, 4:36].rearrange("p t (g e) -> p t g e", e=8),
                                                  in1=ohg16[:].unsqueeze(3).broadcast_to([128, 16, 4, 8]), op=ALU.add), reads=[rr], writes=[rr])
            for tt in range(16):
                S.op("dve", lambda v, tt=tt: v.max(out=m816[:, tt, :], in_=elm16[:, tt, :]), reads=[rr], writes=[rr])
            S.op("dve", lambda v: v.tensor_tensor(out=oh1_all[:], in0=elm16[:], in1=m816[:, :, 0:1].broadcast_to([128, 16, 32]), op=ALU.is_equal),
                 reads=[rr], writes=[allr])
            S.op("dve", lambda v: v.tensor_tensor(out=oh2_all[:], in0=elm16[:], in1=m816[:, :, 1:2].broadcast_to([128, 16, 32]), op=ALU.is_equal),
                 reads=[rr], writes=[allr])
            S.op("dve", lambda v: v.tensor_tensor(out=cnt_all[:], in0=oh1_all[:], in1=oh2_all[:], op=ALU.add), reads=[allr], writes=[allr])
            S.op("dve", lambda v: v.tensor_tensor(out=e16[:], in0=m816[:, :, 1], in1=m816[:, :, 0], op=ALU.subtract), reads=[rr], writes=[rr])
            S.op("act", lambda a: a.activation(out=e16[:], in_=e16[:], func=AF.Exp), reads=[rr], writes=[rr])
            S.op("dve", lambda v: v.tensor_scalar(out=w1_all[:], in0=e16[:], scalar1=1.0, scalar2=None, op0=ALU.add), reads=[rr], writes=[wallr])
            S.op("dve", lambda v: v.reciprocal(out=w1_all[:], in_=w1_all[:]), reads=[wallr], writes=[wallr])
            S.op("dve", lambda v: v.tensor_tensor(out=w1_all[:], in0=w1_all[:], in1=pg16[:], op=ALU.mult), reads=[wallr, rr], writes=[wallr])
            S.op("dve", lambda v: v.tensor_tensor(out=w2_all[:], in0=w1_all[:], in1=e16[:], op=ALU.mult), reads=[wallr, rr], writes=[wallr])
            S.barrier()
            sh1.close()
            cntf = sb("cntf", [128, 32], F32, sh)
            nblk = sb("nblk", [128, 32], F32, sh)
            endb = sb("endb", [128, 32], F32, sh)
            endt = sb("endt", [128, 32], F32, sh)
            startp = sb("startp", [128, 32], F32, sh)
            cmp1 = sb("cmp1", [128, 32, 32], F32, sh)
            cmp2 = sb("cmp2", [128, 64, 32], F32, sh)
            bef = sb("bef", [128, 64], F32, sh)
            idxf = sb("idxf", [128, 64, 4], F32, sh)
            p2r = Res("pass2")

            def cmm(pe):
                for tt in range(16):
                    ins = pe.matmul(PB[5][:, 0:32], lhsT=onesb[:], rhs=cnt_all[:, tt, :], start=(tt == 0), stop=(tt == 15))
                return ins
            S.op("pe", cmm, reads=[allr, constr], writes=[PBr[5]])
            S.op("dve", lambda v: v.tensor_copy(out=cntf[:], in_=PB[5][:, 0:32]), reads=[PBr[5]], writes=[p2r])
            S.op("dve", lambda v: v.tensor_tensor(out=cmp1[:], in0=cntf[:].unsqueeze(2).broadcast_to([128, 32, 32]),
                                                  in1=c2[:, C2_THR:C2_THR + 1024].rearrange("p (a b) -> p a b", b=32), op=ALU.is_gt),
                 reads=[p2r, c2r], writes=[p2r])
            S.op("dve", lambda v: v.tensor_reduce(out=nblk[:], in_=cmp1[:], axis=AX.X, op=ALU.add), reads=[p2r], writes=[p2r])
            S.op("dve", lambda v: v.tensor_copy(out=endb[:], in_=nblk[:]), reads=[p2r], writes=[p2r])
            for sft in (1, 2, 4, 8, 16):
                S.op("dve", lambda v: v.tensor_copy(out=endt[:], in_=endb[:]), reads=[p2r], writes=[p2r])
                S.op("dve", lambda v, sft=sft: v.tensor_tensor(out=endb[:, sft:32], in0=endt[:, sft:32], in1=endt[:, 0:32 - sft], op=ALU.add),
                     reads=[p2r], writes=[p2r])
            S.op("dve", lambda v: v.tensor_tensor(out=startp[:], in0=endb[:], in1=nblk[:], op=ALU.subtract), reads=[p2r], writes=[p2r])
            S.op("dve", lambda v: v.tensor_scalar(out=startp[:], in0=startp[:], scalar1=float(RB), scalar2=None, op0=ALU.mult), reads=[p2r], writes=[p2r])
            S.op("dve", lambda v: v.tensor_tensor(out=cmp2[:], in0=endb[:].unsqueeze(1).broadcast_to([128, 64, 32]),
                                                  in1=c2[:, C2_BLKI:C2_BLKI + 2048].rearrange("p (a b) -> p a b", b=32), op=ALU.is_le),
                 reads=[p2r, c2r], writes=[p2r])
            S.op("dve", lambda v: v.tensor_reduce(out=bef[:], in_=cmp2[:], axis=AX.X, op=ALU.add), reads=[p2r], writes=[p2r])
            S.op("dve", lambda v: v.tensor_scalar(out=bef[:], in0=bef[:], scalar1=31.0, scalar2=512.0, op0=ALU.min, op1=ALU.mult),
                 reads=[p2r], writes=[p2r])
            S.op("dve", lambda v: v.tensor_scalar(out=sm[:, 32:33], in0=cp[:, CP_IOTAP:CP_IOTAP + 1], scalar1=4.0, scalar2=None, op0=ALU.mult),
                 reads=[cpr, rr], writes=[rr])
            for q in range(4):
                S.op("dve", lambda v, q=q: v.tensor_scalar(out=idxf[:, :, q], in0=bef[:], scalar1=sm[:, 32:33], scalar2=float(q),
                                                           op0=ALU.add, op1=ALU.add), reads=[p2r, rr], writes=[p2r])
            S.op("dve", lambda v: v.tensor_scalar(out=bef[:], in0=c2[:, C2_BLKI:C2_BLKI + 2048].rearrange("p (a b) -> p a b", b=32)[:, :, 0],
                                                  scalar1=endb[:, 31:32], scalar2=1.0e6, op0=ALU.is_ge, op1=ALU.mult), reads=[p2r, c2r], writes=[p2r])
            for q in range(4):
                S.op("dve", lambda v, q=q: v.tensor_tensor(out=idxf[:, :, q], in0=idxf[:, :, q], in1=bef[:], op=ALU.add), reads=[p2r], writes=[p2r])
            S.op("dve", lambda v: v.tensor_copy(out=idx4[:], in_=idxf[:].rearrange("p a b -> p (a b)")), reads=[p2r], writes=[idxr])
            if "ROUTE" in dbg:
                S.dma("sp", DBG[:, 0:64], bef[:], reads=[p2r])
                S.dma("sp", DBG[:, 64:96], cntf[:], reads=[p2r])
                S.dma("sp", DBG[:, 96:128], startp[:], reads=[p2r])
            dest = sb("dest", [128, 32], F32, sh)
            tmp32 = sb("tmp32", [128, 32], F32, sh)
            dr = Res("dest")
            cap_reg = nc.gpsimd.to_reg(CAPROWS - 1)
            dcol = [sb(f"dcol{i}", [128, 1], I32, sh) for i in range(4)]
            dcr2 = [Res(f"dcol{i}") for i in range(4)]
            di = 0
            for tt in range(16):
                def rmm(pe, tt=tt):
                    for t2_ in range(tt):
                        pe.matmul(PB[5][:, 0:32], lhsT=onesb[:], rhs=cnt_all[:, t2_, :], start=(t2_ == 0), stop=False)
                    return pe.matmul(PB[5][:, 0:32], lhsT=lstrb[:], rhs=cnt_all[:, tt, :], start=(tt == 0), stop=True)
                S.op("pe", rmm, reads=[allr, constr], writes=[PBr[5]])
                S.op("dve", lambda v: v.tensor_tensor(out=dest[:], in0=PB[5][:, 0:32], in1=startp[:], op=ALU.add), reads=[PBr[5], p2r], writes=[dr])
                for kk, (oh, dall) in enumerate(((oh1_all, d1f), (oh2_all, d2f))):
                    S.op("dve", lambda v, oh=oh, tt=tt: v.tensor_tensor(out=tmp32[:], in0=oh[:, tt, :], in1=dest[:], op=ALU.mult),
                         reads=[allr, dr], writes=[dr])
                    S.op("dve", lambda v, dall=dall, tt=tt: v.tensor_reduce(out=dall[:, tt:tt + 1], in_=tmp32[:], axis=AX.X, op=ALU.add),
                         reads=[dr], writes=[wallr])
                    dc = di % 4
                    di += 1
                    S.op("dve", lambda v, dall=dall, tt=tt, dc=dc: v.tensor_copy(out=dcol[dc][:], in_=dall[:, tt:tt + 1]),
                         reads=[wallr], writes=[dcr2[dc]])
                    S.idma(lambda g_, dc=dc, tt=tt: g_.indirect_dma_start(
                        out=XS[:, :], out_offset=bass.IndirectOffsetOnAxis(ap=dcol[dc][:, 0:1], axis=0),
                        in_=n2b_all[:, tt, :], in_offset=None, bounds_check=cap_reg, oob_is_err=False), reads=[dcr2[dc], n2r])
            if "ROUTE" in dbg:
                S.dma("sp", DBG[:, 128:144], d1f[:], reads=[wallr])
                S.dma("sp", DBG[:, 144:160], d2f[:], reads=[wallr])
                S.dma("sp", DBG[:, 160:176], w1_all[:], reads=[wallr])
                S.dma("sp", DBG[:, 176:192], w2_all[:], reads=[wallr])
            S.barrier()
        if stop_after == "H":
            return nc
        sNT.close()

        with ExitStack() as si:
            wg = [sb(f"wg{i}", [128, 16, 512], BF16, si) for i in range(2)]
            wu = [sb(f"wu{i}", [128, 16, 512], BF16, si) for i in range(2)]
            wd = [sb(f"wd{i}", [128, 4, 2048], BF16, si) for i in range(2)]
            wgr = [[Res(f"wg{i}_{q}") for q in range(4)] for i in range(2)]
            wur = [[Res(f"wu{i}_{q}") for q in range(4)] for i in range(2)]
            wdr = [[Res(f"wd{i}_{q}") for q in range(4)] for i in range(2)]
            iq = [[sb(f"iq{i}_{q}", [128, 1], I32, si) for q in range(4)] for i in range(NBLK)]
            iqr = [Res(f"iq{i}") for i in range(NBLK)]
            for blk in range(NBLK):
                for q in range(4):
                    S.op("dve", lambda v, q=q, blk=blk: v.tensor_copy(out=iq[blk][q][:], in_=idx4[:, blk * 4 + q:blk * 4 + q + 1]),
                         reads=[idxr], writes=[iqr[blk]])
            xrow = [sb(f"xrow{i}", [128, D], BF16, si) for i in range(4)]
            xrr = [Res(f"xrow{i}") for i in range(4)]
            xT = [sb(f"xT{i}", [128, 16, RB], BF16, si) for i in range(2)]
            xTr = [[Res(f"xT{i}_{r}") for r in range(2)] for i in range(2)]
            sg = [sb(f"sg{i}", [128, RB], F32, si) for i in range(2)]
            sgr = [Res(f"sg{i}") for i in range(2)]
            hT = [sb(f"hT{i}", [128, 4, RB], BF16, si) for i in range(2)]
            hTr = [Res(f"hT{i}") for i in range(2)]
            yt = [sb(f"yt{i}", [128, D], BF16, si) for i in range(2)]
            ytr = [Res(f"yt{i}") for i in range(2)]
            fi = 0
            xi = 0
            yi = 0
            bc_reg = nc.gpsimd.to_reg(NEXP * 128 * 4 - 1)
            xks = {}

            def emit_gathers(blk):
                k = blk % 2
                for (wt_, src, nkc, wrs) in ((wg, weg_v, 4, wgr), (wu, weu_v, 4, wur), (wd, wed_v, 1, wdr)):
                    for q in range(4):
                        if nkc == 4:
                            o = wt_[k][:, 4 * q:4 * q + 4, :].rearrange("p a b -> p (a b)")
                        else:
                            o = wt_[k][:, q, :]
                        S.idma(lambda g_, o=o, src=src, blk=blk, q=q: g_.indirect_dma_start(
                            out=o, out_offset=None, in_=src[:, :], in_offset=bass.IndirectOffsetOnAxis(ap=iq[blk][q][:, 0:1], axis=0),
                            bounds_check=bc_reg, oob_is_err=False),
                            reads=[iqr[blk]], writes=[wrs[k][q]])

            def emit_xload(blk):
                for r in range(RB // 128):
                    xk = (blk * 2 + r) % 4
                    r0 = blk * RB + r * 128
                    S.dma("sp", xrow[xk][:], XS[r0:r0 + 128, :], reads=[XSr], writes=[xrr[xk]])

            def emit_xT(blk):
                k = blk % 2
                for r in range(RB // 128):
                    xk = (blk * 2 + r) % 4
                    for half in range(2):
                        def trx(pe, xk=xk, half=half):
                            for jj in range(8):
                                kc = half * 8 + jj
                                ins = pe.transpose(out=PT[half][:, jj * 128:(jj + 1) * 128], in_=xrow[xk][:, kc * 128:(kc + 1) * 128],
                                                   identity=identb[:])
                            return ins
                        S.op("pe", trx, reads=[xrr[xk], constr], writes=[PTr[half]])
                        if half == 0:
                            S.op("act", lambda a, k=k, r=r: a.activation(out=xT[k][:, 0:8, r * 128:(r + 1) * 128],
                                                                         in_=PT[0][:, :].rearrange("p (a b) -> p a b", b=128), func=AF.Copy),
                                 reads=[PTr[0]], writes=[xTr[k][r]])
                        else:
                            S.op("dve", lambda v, k=k, r=r: v.tensor_copy(out=xT[k][:, 8:16, r * 128:(r + 1) * 128],
                                                                          in_=PT[1][:, :].rearrange("p (a b) -> p a b", b=128)),
                                 reads=[PTr[1]], writes=[xTr[k][r]])

            def emit_gateup(blk):
                nonlocal_fi = fi_box
                k = blk % 2
                for fc in range(4):
                    f2 = nonlocal_fi[0] % 2
                    nonlocal_fi[0] += 1
                    pg, pu = 2 * f2, 2 * f2 + 1

                    def gmm_(pe, k=k, fc=fc, pg=pg):
                        for kc in range(16):
                            ins = pe.matmul(PB[pg][:, 0:RB], lhsT=wg[k][:, kc, fc * 128:(fc + 1) * 128], rhs=xT[k][:, kc, :],
                                            start=(kc == 0), stop=(kc == 15))
                        return ins

                    def umm_(pe, k=k, fc=fc, pu=pu):
                        for kc in range(16):
                            ins = pe.matmul(PB[pu][:, 0:RB], lhsT=wu[k][:, kc, fc * 128:(fc + 1) * 128], rhs=xT[k][:, kc, :],
                                            start=(kc == 0), stop=(kc == 15))
                        return ins
                    S.op("pe", gmm_, reads=wgr[k] + xTr[k], writes=[PBr[pg]])
                    S.op("pe", umm_, reads=wur[k] + xTr[k], writes=[PBr[pu]])
                    S.op("act", lambda a, f2=f2, pg=pg: a.activation(out=sg[f2][:], in_=PB[pg][:, 0:RB], func=AF.Silu),
                         reads=[PBr[pg]], writes=[sgr[f2]])
                    S.op("dve", lambda v, k=k, fc=fc, f2=f2, pu=pu: v.tensor_tensor(out=hT[k][:, fc, :], in0=PB[pu][:, 0:RB], in1=sg[f2][:],
                                                                                   op=ALU.mult), reads=[PBr[pu], sgr[f2]], writes=[hTr[k]])

            def emit_down(blk):
                k = blk % 2
                for r in range(RB // 128):
                    yk = (blk * 2 + r) % 2
                    for nt in range(4):
                        py = 4 + nt % 2

                        def dmm(pe, k=k, nt=nt, py=py, r=r):
                            for fc in range(4):
                                ins = pe.matmul(PB[py][:, :], lhsT=hT[k][:, fc, r * 128:(r + 1) * 128], rhs=wd[k][:, fc, nt * 512:(nt + 1) * 512],
                                                start=(fc == 0), stop=(fc == 3))
                            return ins
                        S.op("pe", dmm, reads=[hTr[k]] + wdr[k], writes=[PBr[py]])
                        if nt % 2 == 0:
                            S.op("act", lambda a, yk=yk, nt=nt, py=py: a.activation(out=yt[yk][:, nt * 512:(nt + 1) * 512], in_=PB[py][:, :],
                                                                                   func=AF.Copy), reads=[PBr[py]], writes=[ytr[yk]])
                        else:
                            S.op("dve", lambda v, yk=yk, nt=nt, py=py: v.tensor_copy(out=yt[yk][:, nt * 512:(nt + 1) * 512], in_=PB[py][:, :]),
                                 reads=[PBr[py]], writes=[ytr[yk]])
                    r0 = blk * RB + r * 128
                    S.dma("sp", YS[r0:r0 + 128, :], yt[yk][:], reads=[ytr[yk]])

            fi_box = [0]
            emit_gathers(0)
            emit_xload(0)
            emit_xT(0)
            emit_gathers(1)
            emit_xload(1)
            for blk in range(NBLK):
                emit_gateup(blk)
                if blk + 1 < NBLK:
                    emit_xT(blk + 1)
                if blk + 2 < NBLK:
                    emit_xload(blk + 2)
                emit_down(blk)
                if blk + 2 < NBLK:
                    emit_gathers(blk + 2)
            S.barrier()

        with ExitStack() as sj:
            nfin_bc = sb("nfin_bc", [128, D], F32, sj)
            nfr = Res("nfin")
            S.dma("sp", nfin_bc[:], dram_bcast(nfin_g[0:1, :], D), writes=[nfr])
            y1 = [sb(f"y1{i}", [128, D], BF16, sj) for i in range(2)]
            y2 = [sb(f"y2{i}", [128, D], BF16, sj) for i in range(2)]
            acc = [sb(f"jacc{i}", [128, D], F32, sj) for i in range(2)]
            accr = [Res(f"jacc{i}") for i in range(2)]
            yr = [Res(f"y{i}") for i in range(2)]
            yr2 = [Res(f"yb{i}") for i in range(2)]
            h1j = [sb(f"h1j{i}", [128, D], F32, sj) for i in range(2)]
            h1jr = [Res(f"h1j{i}") for i in range(2)]
            ot = [sb(f"jo{i}", [128, D], F32, sj) for i in range(2)]
            orr = [Res(f"jo{i}") for i in range(2)]
            junk = sb("jjunk", [128, D], BF16, sj)
            jr = Res("jjunk")
            ssq = [sb(f"jssq{i}", [128, 1], F32, sj) for i in range(2)]
            sr = [Res(f"jssq{i}") for i in range(2)]
            jc = [[sb(f"jc{i}_{z}", [128, 1], I32, sj) for z in range(2)] for i in range(16)]
            jcr = [Res(f"jc{i}") for i in range(16)]
            jcr2 = [Res(f"jcb{i}") for i in range(16)]
            for tt in range(16):
                S.op("dve", lambda v, tt=tt: v.tensor_copy(out=jc[tt][0][:], in_=d1f[:, tt:tt + 1]), reads=[wallr], writes=[jcr[tt]])
                S.op("dve", lambda v, tt=tt: v.tensor_copy(out=jc[tt][1][:], in_=d2f[:, tt:tt + 1]), reads=[wallr], writes=[jcr2[tt]])
            def j_loads(tt):
                k = tt % 2
                S.idma(lambda g_, k=k: g_.indirect_dma_start(out=y1[k][:], out_offset=None, in_=YS[:, :],
                                                            in_offset=bass.IndirectOffsetOnAxis(ap=jc[tt][0][:, 0:1], axis=0),
                                                            bounds_check=cap_reg, oob_is_err=False),
                       reads=[jcr[tt], YSr], writes=[yr[k]])
                S.idma(lambda g_, k=k: g_.indirect_dma_start(out=y2[k][:], out_offset=None, in_=YS[:, :],
                                                            in_offset=bass.IndirectOffsetOnAxis(ap=jc[tt][1][:, 0:1], axis=0),
                                                            bounds_check=cap_reg, oob_is_err=False),
                       reads=[jcr2[tt], YSr], writes=[yr2[k]])
                S.dma("sp", h1j[k][:], H1[tt * 128:(tt + 1) * 128, :], writes=[h1jr[k]])

            j_loads(0)
            for tt in range(16):
                k = tt % 2
                if tt + 1 < 16:
                    j_loads(tt + 1)
                S.op("dve", lambda v, k=k, tt=tt: v.tensor_scalar(out=acc[k][:], in0=y1[k][:], scalar1=w1_all[:, tt:tt + 1], scalar2=None,
                                                                  op0=ALU.mult), reads=[yr[k], wallr], writes=[accr[k]])
                S.op("dve", lambda v, k=k, tt=tt: v.scalar_tensor_tensor(out=acc[k][:], in0=y2[k][:], scalar=w2_all[:, tt:tt + 1], in1=acc[k][:],
                                                                         op0=ALU.mult, op1=ALU.add), reads=[accr[k], yr2[k], wallr], writes=[accr[k]])
                S.op("pool", lambda g_, k=k: g_.tensor_tensor(out=acc[k][:], in0=acc[k][:], in1=gf_bc[:], op=ALU.mult), reads=[accr[k], bcr], writes=[accr[k]])
                S.op("dve", lambda v, k=k: v.tensor_tensor(out=h1j[k][:], in0=h1j[k][:], in1=acc[k][:], op=ALU.add), reads=[accr[k], h1jr[k]], writes=[h1jr[k]])
                S.op("act", lambda a, k=k: a.activation(out=junk[:], in_=h1j[k][:], func=AF.Square, accum_out=ssq[k][:]),
                     reads=[h1jr[k]], writes=[jr, sr[k]])
                S.op("act", lambda a, k=k: a.activation(out=ssq[k][:], in_=ssq[k][:], func=AF.Sqrt, scale=1.0 / D, bias=epsc[:, 0:1]),
                     reads=[sr[k], constr], writes=[sr[k]])
                S.op("dve", lambda v, k=k: v.reciprocal(out=ssq[k][:], in_=ssq[k][:]), reads=[sr[k]], writes=[sr[k]])
                S.op("dve", lambda v, k=k: v.scalar_tensor_tensor(out=ot[k][:], in0=h1j[k][:], scalar=ssq[k][:, 0:1], in1=nfin_bc[:],
                                                                   op0=ALU.mult, op1=ALU.mult), reads=[h1jr[k], sr[k], nfr], writes=[orr[k]])
                S.dma("sp", out[tt * 128:(tt + 1) * 128, :], ot[k][:], reads=[orr[k]])
            S.barrier()
    return nc


def _local_order(j):
    own = [2 * i + j for i in range(8)]
    oth = [2 * i + 1 - j for i in range(8)]
    return own + oth


def _const_pack(j):
    cpk = np.zeros((128, CP_W), np.float32)
    p = np.arange(128)
    cpk[:, CP_IDENT:CP_IDENT + 128] = np.eye(128)
    pm = np.zeros((128, 128), np.float32)
    for pp in range(64):
        pm[pp + 64, pp] = -1.0
        pm[pp, pp + 64] = 1.0
    cpk[:, CP_PM:CP_PM + 128] = pm
    cpk[:, CP_TRI01:CP_TRI01 + 128] = (p[:, None] <= p[None, :])
    cpk[:, CP_TRIPEN:CP_TRIPEN + 128] = np.where(p[:, None] <= p[None, :], 0.0, -BIG)
    cpk[:, CP_LSTRICT:CP_LSTRICT + 128] = (p[:, None] < p[None, :])
    cpk[:, CP_ONES:CP_ONES + 128] = 1.0
    invf = (10000.0 ** (-np.arange(0, 128, 2, dtype=np.float32) / 128)).astype(np.float32)
    cpk[:, CP_INVF] = np.concatenate([invf, invf])
    cpk[:, CP_IOTAP] = p
    cpk[:, CP_SIGN] = np.where(p < 64, -1.0, 1.0)
    order = _local_order(j)
    pp_ = np.zeros((8, 16), np.float32)
    p01 = np.zeros((8, 16), np.float32)
    for i in range(8):
        gq = 2 * i + j
        for kb in range(16):
            past = order[kb] < gq
            pp_[i, kb] = 0.0 if past else -1e30
            p01[i, kb] = 1.0 if past else 0.0
    cpk[:, CP_PASTPEN:CP_PASTPEN + 128] = pp_.reshape(1, 128)
    cpk[:, CP_PAST01:CP_PAST01 + 128] = p01.reshape(1, 128)
    return cpk


def _const_pack2():
    c2 = np.zeros((128, C2_W), np.float32)
    c2[:, C2_THR:C2_THR + 1024] = np.tile(float(RB) * np.arange(32, dtype=np.float32), 32)[None, :]
    c2[:, C2_BLKI:C2_BLKI + 2048] = np.repeat(np.arange(64, dtype=np.float32), 32)[None, :]
    return c2


def _prep_shared(inp):
    f = np.float32
    sh = {}
    sh["w_ada"] = np.ascontiguousarray(inp["w_ada"][0], f)
    sh["b_ada"] = np.ascontiguousarray(inp["b_ada"][0].reshape(1, -1), f)
    sh["gmix"] = np.ascontiguousarray(inp["norm_mix_g"][0].reshape(16, 128).T, f)
    sh["w_in"] = np.ascontiguousarray(inp["w_in"][0], f)
    sh["sgu_ln_g"] = np.ascontiguousarray(inp["sgu_ln_g"][0].reshape(1, -1), f)
    sh["sgu_ln_b"] = np.ascontiguousarray(inp["sgu_ln_b"][0].reshape(1, -1), f)
    sh["wsT"] = np.ascontiguousarray(np.transpose(inp["sgu_w_s"][0], (2, 0, 1)).reshape(128, 1024), f)
    sh["sgu_b_s"] = np.ascontiguousarray(inp["sgu_b_s"][0].reshape(1, -1), f)
    sh["w_sgu_out"] = np.ascontiguousarray(inp["w_sgu_out"][0], f)
    sh["w_moba_out"] = np.ascontiguousarray(inp["w_moba_out"][0], f)
    sh["w_out"] = np.ascontiguousarray(inp["w_out"][0], f)
    sh["norm_ffn_g"] = np.ascontiguousarray(inp["norm_ffn_g"][0].reshape(1, -1), f)
    sh["norm_final_g"] = np.ascontiguousarray(inp["norm_final_g"].reshape(1, -1), f)
    wr = np.concatenate([inp["w_route_group"][0], inp["w_route_expert"][0]], axis=1)
    sh["wr"] = np.ascontiguousarray(wr.reshape(16, 128, 36).transpose(1, 0, 2).reshape(128, 16 * 36), f)
    sh["br"] = np.ascontiguousarray(
        np.concatenate([inp["b_route_group"][0].reshape(-1), inp["b_route_expert"][0].reshape(-1)]).reshape(1, 36), f)
    sh["weg"] = np.ascontiguousarray(
        inp["w_exp_gate"][0].reshape(NEXP, 16, 128, DFF).transpose(0, 2, 1, 3).reshape(NEXP * 128 * 4, 2048), f)
    sh["weu"] = np.ascontiguousarray(
        inp["w_exp_up"][0].reshape(NEXP, 16, 128, DFF).transpose(0, 2, 1, 3).reshape(NEXP * 128 * 4, 2048), f)
    sh["wed"] = np.ascontiguousarray(
        inp["w_exp_down"][0].reshape(NEXP, 4, 128, D).transpose(0, 2, 1, 3).reshape(NEXP * 128 * 4, 2048), f)
    ea = np.zeros((16, 16 * 128), np.float32)
    for kb in range(16):
        ea[kb, kb * 128:(kb + 1) * 128] = 1.0
    sh["eall"] = ea
    sh["cpack2"] = _const_pack2()
    return sh


def _prep_core(inp, sh, c):
    b, j = c // 2, c % 2
    order = _local_order(j)
    xb = np.asarray(inp["x"][b], np.float32).reshape(16, 256, D)
    pb = np.asarray(inp["positions"][b], np.int32).reshape(16, 256)
    m = dict(sh)
    m["x"] = np.ascontiguousarray(xb[order].reshape(SEQ, D))
    m["pos"] = np.ascontiguousarray(pb[order].reshape(1, SEQ))
    m["cvec"] = np.ascontiguousarray(np.asarray(inp["c"][b], np.float32).reshape(16, 128).T)
    m["cpack"] = _const_pack(j)
    return m


_NC_CACHE = {}


def kernel(**inputs):
    inp = {k: np.asarray(v) for k, v in inputs.items()}
    sh = _prep_shared(inp)
    in_maps = [_prep_core(inp, sh, c) for c in range(8)]
    if "nc" not in _NC_CACHE:
        _NC_CACHE["nc"] = build()
    res = run_bass_kernel_spmd(_NC_CACHE["nc"], in_maps, core_ids=list(range(8)))
    outp = np.zeros((4, 16, 256, D), np.float32)
    for c in range(8):
        b, j = c // 2, c % 2
        o = np.asarray(res.results[c]["out"]).reshape(8, 256, D)
        for i in range(8):
            outp[b, 2 * i + j] = o[i]
    return outp.reshape(4, SEQ, D)
```
